# Optimizing a Trainium2 kernel written in Bass

```python
import math
import jax, jax.numpy as jnp
from jax import lax
import numpy as np

D_MODEL = 1024
BATCH = 8
SEQ = 4096
DEPTH = 2

CHUNK = 64
QBLK = 128
MIX_WIDTH = D_MODEL
A_HEADS = 8
A_HEAD_DIM = 64
A_WIDTH = A_HEADS * A_HEAD_DIM
KV_LORA = 128
IDX_HEADS = 8
IDX_DIM = 64
INDEX_TOPK_MAX = 256
B_HEADS = 4
B_QK_DIM = 64
B_V_DIM = 2 * B_QK_DIM
B_WIDTH = B_HEADS * B_V_DIM
MEM_TOKENS = 256
MEM_HEADS = 4
MEM_HEAD_DIM = D_MODEL // MEM_HEADS
N_EXPERTS = 64
TOP_K = 8
EXPERT_DIM = 256
SHARED_DIM = 256
ROUTED_SCALE = 2.5
MOE_BLOCK = 128
ALPHA = (2 * DEPTH) ** 0.25
BETA = (8 * DEPTH) ** -0.25
LN_EPS = 1e-5
RMS_EPS = 1e-6
IN_SIZES = (A_WIDTH, KV_LORA, IDX_HEADS * IDX_DIM, IDX_DIM, IDX_HEADS,
            2 * B_HEADS * B_QK_DIM, 2 * B_HEADS * B_QK_DIM, B_WIDTH)

kernel_name = "hybrid_dsa_diffattn_mem_moe_deepnorm"


def layer_norm(x, g, b):
    xf = x.astype(jnp.float32)
    mu = jnp.mean(xf, axis=-1, keepdims=True)
    var = jnp.mean(jnp.square(xf - mu), axis=-1, keepdims=True)
    return ((xf - mu) * lax.rsqrt(var + LN_EPS) * g + b).astype(x.dtype)


def rms_norm(x, g):
    xf = x.astype(jnp.float32)
    return (xf * lax.rsqrt(jnp.mean(xf * xf, axis=-1, keepdims=True) + RMS_EPS) * g).astype(x.dtype)


def alibi_slopes(n):
    return jnp.exp2(-8.0 * jnp.arange(1, n + 1, dtype=jnp.float32) / n)


def to_qblocks(a):
    b, s = a.shape[:2]
    return jnp.moveaxis(a.reshape(b, s // QBLK, QBLK, *a.shape[2:]), 1, 0)


def from_qblocks(a):
    a = jnp.moveaxis(a, 0, 1)
    return a.reshape(a.shape[0], -1, *a.shape[3:])


def dsa_attention(q, q_idx, w_idx, c_kv, k_idx, w_uk, w_uv):
    b, s = q.shape[:2]
    topk = min(INDEX_TOPK_MAX, s // 4)
    slopes = alibi_slopes(A_HEADS)
    key_chunk = jnp.arange(s) // CHUNK
    bidx = jnp.arange(b)[:, None, None]
    scale = A_HEAD_DIM ** -0.5

    def one_block(args):
        qb, qib, wib, blk = args
        tq = blk * QBLK + jnp.arange(QBLK)
        rel = jax.nn.relu(jnp.einsum('bqhd,bsd->bqhs', qib, k_idx) * IDX_DIM ** -0.5)
        iscore = jnp.einsum('bqh,bqhs->bqs', wib, rel).astype(jnp.float32)
        admissible = key_chunk[None, :] <= (tq // CHUNK)[:, None]
        iscore = jnp.where(admissible[None], iscore, -jnp.inf)
        _, sel = lax.top_k(iscore, topk)
        valid = (sel // CHUNK) <= (tq // CHUNK)[None, :, None]
        c_sel = c_kv[bidx, sel]
        q_lat = jnp.einsum('bqhd,hcd->bqhc', qb, w_uk)
        sc = jnp.einsum('bqhc,bqkc->bqhk', q_lat, c_sel).astype(jnp.float32) * scale
        dist = jnp.abs(tq[None, :, None] - sel).astype(jnp.float32)
        sc = sc - slopes[None, None, :, None] * dist[:, :, None, :]
        sc = jnp.where(valid[:, :, None, :], sc, -jnp.inf)
        p = jax.nn.softmax(sc, axis=-1).astype(c_kv.dtype)
        o_lat = jnp.einsum('bqhk,bqkc->bqhc', p, c_sel)
        return jnp.einsum('bqhc,hcd->bqhd', o_lat, w_uv)

    nb = s // QBLK
    out = lax.map(one_block, (to_qblocks(q), to_qblocks(q_idx), to_qblocks(w_idx), jnp.arange(nb)))
    return from_qblocks(out).reshape(b, s, A_WIDTH)


def diff_attention(q1, q2, k1, k2, v, lam):
    s = q1.shape[1]
    slopes = alibi_slopes(B_HEADS)
    ts = jnp.arange(s)
    scale = B_QK_DIM ** -0.5

    def one_block(args):
        q1b, q2b, blk = args
        tq = blk * QBLK + jnp.arange(QBLK)
        mask = (ts // CHUNK)[None, :] <= (tq // CHUNK)[:, None]
        dist = jnp.abs(tq[:, None] - ts[None, :]).astype(jnp.float32)
        bias = jnp.where(mask[None], -slopes[:, None, None] * dist[None], -jnp.inf)
        s1 = jnp.einsum('bqhd,bshd->bhqs', q1b, k1).astype(jnp.float32) * scale + bias
        s2 = jnp.einsum('bqhd,bshd->bhqs', q2b, k2).astype(jnp.float32) * scale + bias
        attn = (jax.nn.softmax(s1, axis=-1) - lam * jax.nn.softmax(s2, axis=-1)).astype(v.dtype)
        return jnp.einsum('bhqs,bshd->bqhd', attn, v)

    out = lax.map(one_block, (to_qblocks(q1), to_qblocks(q2), jnp.arange(s // QBLK)))
    return from_qblocks(out)


def hybrid_mixer(x, w_in, a_kv_norm, a_w_uk, a_w_uv, b_lq1, b_lk1, b_lq2, b_lk2, b_subln, w_o, lambda_init):
    b, s, _ = x.shape
    proj = x @ w_in
    splits = np.cumsum(IN_SIZES)[:-1].tolist()
    q_a, c_kv, q_idx, k_idx, w_idx, q_b, k_b, v_b = jnp.split(proj, splits, axis=-1)
    q_a = q_a.reshape(b, s, A_HEADS, A_HEAD_DIM)
    c_kv = rms_norm(c_kv, a_kv_norm)
    q_idx = q_idx.reshape(b, s, IDX_HEADS, IDX_DIM)
    w_idx = w_idx * IDX_HEADS ** -0.5
    out_a = dsa_attention(q_a, q_idx, w_idx, c_kv, k_idx, a_w_uk, a_w_uv)
    q_b = q_b.reshape(b, s, B_HEADS, 2, B_QK_DIM)
    k_b = k_b.reshape(b, s, B_HEADS, 2, B_QK_DIM)
    v_b = v_b.reshape(b, s, B_HEADS, B_V_DIM)
    lam = (jnp.exp(jnp.sum(b_lq1.astype(jnp.float32) * b_lk1.astype(jnp.float32)))
           - jnp.exp(jnp.sum(b_lq2.astype(jnp.float32) * b_lk2.astype(jnp.float32))) + lambda_init)
    o_b = diff_attention(q_b[..., 0, :], q_b[..., 1, :], k_b[..., 0, :], k_b[..., 1, :], v_b, lam)
    out_b = (rms_norm(o_b, b_subln) * (1.0 - lambda_init)).reshape(b, s, B_WIDTH)
    return jnp.concatenate([out_a, out_b], axis=-1) @ w_o


def memory_attention(x, mem, wq, wkv, wo):
    b, s, d = x.shape
    m = mem.shape[1]
    q = (x @ wq).reshape(b, s, MEM_HEADS, MEM_HEAD_DIM)
    kv = (mem @ wkv).reshape(b, m, 2, MEM_HEADS, MEM_HEAD_DIM)
    k, v = kv[:, :, 0], kv[:, :, 1]
    sc = jnp.einsum('bqhd,bmhd->bhqm', q, k).astype(jnp.float32) * MEM_HEAD_DIM ** -0.5
    p = jax.nn.softmax(sc, axis=-1).astype(v.dtype)
    o = jnp.einsum('bhqm,bmhd->bqhd', p, v).reshape(b, s, d)
    return o @ wo


def swiglu(x, wg, wu, wd):
    return (jax.nn.silu(x @ wg) * (x @ wu)) @ wd


def moe(x, router_w, router_bias, e_w_gate, e_w_up, e_w_down, s_w_gate, s_w_up, s_w_down):
    b, s, d = x.shape
    n = b * s
    xf = x.reshape(n, d)
    scores = jax.nn.sigmoid((xf @ router_w).astype(jnp.float32))
    _, top_e = lax.top_k(scores + router_bias.astype(jnp.float32), TOP_K)
    top_s = jnp.take_along_axis(scores, top_e, axis=-1)
    gates = top_s / jnp.sum(top_s, axis=-1, keepdims=True) * ROUTED_SCALE
    m = n * TOP_K
    flat_e = top_e.reshape(m)
    flat_tok = jnp.repeat(jnp.arange(n, dtype=jnp.int32), TOP_K)
    flat_g = gates.reshape(m)
    order = jnp.argsort(flat_e)
    se = flat_e[order]
    counts = jnp.bincount(flat_e, length=N_EXPERTS)
    padded = (counts + MOE_BLOCK - 1) // MOE_BLOCK * MOE_BLOCK
    start = jnp.cumsum(counts) - counts
    pend = jnp.cumsum(padded)
    pstart = pend - padded
    dest = pstart[se] + (jnp.arange(m) - start[se])
    rows_total = m + N_EXPERTS * MOE_BLOCK
    nblk = rows_total // MOE_BLOCK
    row_tok = jnp.full((rows_total,), n, dtype=jnp.int32).at[dest].set(flat_tok[order])
    row_gate = jnp.zeros((rows_total,), jnp.float32).at[dest].set(flat_g[order])
    blk_e = jnp.minimum(jnp.searchsorted(pend, jnp.arange(nblk) * MOE_BLOCK, side='right'), N_EXPERTS - 1)
    x_pad = jnp.concatenate([xf, jnp.zeros((1, d), xf.dtype)], axis=0)

    def body(acc, blk):
        rows, g, e = blk
        xb = x_pad[rows]
        yb = swiglu(xb, e_w_gate[e], e_w_up[e], e_w_down[e]) * g[:, None]
        return acc.at[rows].add(yb), None

    acc, _ = lax.scan(body, jnp.zeros((n + 1, d), x.dtype),
                      (row_tok.reshape(nblk, MOE_BLOCK),
                       row_gate.reshape(nblk, MOE_BLOCK).astype(x.dtype), blk_e))
    out = acc[:n] + swiglu(xf, s_w_gate, s_w_up, s_w_down)
    return out.reshape(b, s, d)


def setup_inputs(seed: int = 0) -> dict:
    key = jax.random.key(seed)
    keys = list(jax.random.split(key, 48))

    def nrm(shape, scale):
        return scale * jax.random.normal(keys.pop(), shape, jnp.float32)

    D = D_MODEL
    x = nrm((BATCH, SEQ, D), 1.0)
    mem = nrm((BATCH, MEM_TOKENS, D), 1.0)
    ln_in_g = 1.0 + nrm((D,), 0.02)
    ln_in_b = nrm((D,), 0.02)
    w_in = jnp.concatenate(
        [nrm((DEPTH, D, sz), D ** -0.5 * (BETA if i == len(IN_SIZES) - 1 else 1.0))
         for i, sz in enumerate(IN_SIZES)], axis=-1)
    a_kv_norm = 1.0 + nrm((DEPTH, KV_LORA), 0.02)
    a_w_uk = nrm((DEPTH, A_HEADS, KV_LORA, A_HEAD_DIM), KV_LORA ** -0.5)
    a_w_uv = nrm((DEPTH, A_HEADS, KV_LORA, A_HEAD_DIM), KV_LORA ** -0.5 * BETA)
    b_lq1 = nrm((DEPTH, B_QK_DIM), 0.1)
    b_lk1 = nrm((DEPTH, B_QK_DIM), 0.1)
    b_lq2 = nrm((DEPTH, B_QK_DIM), 0.1)
    b_lk2 = nrm((DEPTH, B_QK_DIM), 0.1)
    b_subln = 1.0 + nrm((DEPTH, B_V_DIM), 0.02)
    w_o = nrm((DEPTH, MIX_WIDTH, D), MIX_WIDTH ** -0.5 * BETA)
    ln1_g = 1.0 + nrm((DEPTH, D), 0.02)
    ln1_b = nrm((DEPTH, D), 0.02)
    m_wq = nrm((DEPTH, D, D), D ** -0.5)
    m_wkv = jnp.concatenate([nrm((DEPTH, D, D), D ** -0.5), nrm((DEPTH, D, D), D ** -0.5 * BETA)], axis=-1)
    m_wo = nrm((DEPTH, D, D), D ** -0.5 * BETA)
    ln2_g = 1.0 + nrm((DEPTH, D), 0.02)
    ln2_b = nrm((DEPTH, D), 0.02)
    router_w = nrm((DEPTH, D, N_EXPERTS), D ** -0.5)
    router_bias = nrm((DEPTH, N_EXPERTS), 0.01)
    e_w_gate = nrm((DEPTH, N_EXPERTS, D, EXPERT_DIM), D ** -0.5)
    e_w_up = nrm((DEPTH, N_EXPERTS, D, EXPERT_DIM), D ** -0.5 * BETA)
    e_w_down = nrm((DEPTH, N_EXPERTS, EXPERT_DIM, D), EXPERT_DIM ** -0.5 * BETA)
    s_w_gate = nrm((DEPTH, D, SHARED_DIM), D ** -0.5)
    s_w_up = nrm((DEPTH, D, SHARED_DIM), D ** -0.5 * BETA)
    s_w_down = nrm((DEPTH, SHARED_DIM, D), SHARED_DIM ** -0.5 * BETA)
    ln3_g = 1.0 + nrm((DEPTH, D), 0.02)
    ln3_b = nrm((DEPTH, D), 0.02)
    return {"x": x, "mem": mem, "ln_in_g": ln_in_g, "ln_in_b": ln_in_b, "w_in": w_in,
            "a_kv_norm": a_kv_norm, "a_w_uk": a_w_uk, "a_w_uv": a_w_uv,
            "b_lq1": b_lq1, "b_lk1": b_lk1, "b_lq2": b_lq2, "b_lk2": b_lk2, "b_subln": b_subln,
            "w_o": w_o, "ln1_g": ln1_g, "ln1_b": ln1_b,
            "m_wq": m_wq, "m_wkv": m_wkv, "m_wo": m_wo, "ln2_g": ln2_g, "ln2_b": ln2_b,
            "router_w": router_w, "router_bias": router_bias,
            "e_w_gate": e_w_gate, "e_w_up": e_w_up, "e_w_down": e_w_down,
            "s_w_gate": s_w_gate, "s_w_up": s_w_up, "s_w_down": s_w_down,
            "ln3_g": ln3_g, "ln3_b": ln3_b}


def reference(x, mem, ln_in_g, ln_in_b, w_in, a_kv_norm, a_w_uk, a_w_uv,
              b_lq1, b_lk1, b_lq2, b_lk2, b_subln, w_o, ln1_g, ln1_b,
              m_wq, m_wkv, m_wo, ln2_g, ln2_b,
              router_w, router_bias, e_w_gate, e_w_up, e_w_down,
              s_w_gate, s_w_up, s_w_down, ln3_g, ln3_b):
    h = layer_norm(x, ln_in_g, ln_in_b)
    for l in range(DEPTH):
        lambda_init = 0.8 - 0.6 * math.exp(-0.3 * l)
        y = hybrid_mixer(h, w_in[l], a_kv_norm[l], a_w_uk[l], a_w_uv[l],
                         b_lq1[l], b_lk1[l], b_lq2[l], b_lk2[l], b_subln[l], w_o[l], lambda_init)
        h = layer_norm(ALPHA * h + y, ln1_g[l], ln1_b[l])
        y = memory_attention(h, mem, m_wq[l], m_wkv[l], m_wo[l])
        h = layer_norm(ALPHA * h + y, ln2_g[l], ln2_b[l])
        y = moe(h, router_w[l], router_bias[l], e_w_gate[l], e_w_up[l], e_w_down[l],
                s_w_gate[l], s_w_up[l], s_w_down[l])
        h = layer_norm(ALPHA * h + y, ln3_g[l], ln3_b[l])
    return h
```

```python
import math
from contextlib import ExitStack

import numpy as np
import ml_dtypes

import concourse.bass as bass
import concourse.mybir as mybir
from concourse.bass_utils import run_bass_kernel_spmd

F32 = mybir.dt.float32
BF16 = mybir.dt.bfloat16
ALU = mybir.AluOpType
AF = mybir.ActivationFunctionType
AX = mybir.AxisListType

D = 1024
DEPTH = 2
NE = 64
ALPHA = (2 * DEPTH) ** 0.25
LN_EPS = 1e-5
RMS_EPS = 1e-6
WIN = 2760
NBIS = 14
NEG = -1.0e30


class H:
    __slots__ = ("name", "last_w", "rd_eng", "rd_dma")

    def __init__(self, name=""):
        self.name = name
        self.last_w = None
        self.rd_eng = {}
        self.rd_dma = []


class V:
    def __init__(self, hs, ap):
        self.hs = hs
        self.ap = ap

    def __getitem__(self, k):
        return V(self.hs, self.ap[k])

    def re(self, s, **kw):
        return V(self.hs, self.ap.rearrange(s, **kw))

    def bc(self, dt):
        return V(self.hs, self.ap.bitcast(dt))


class Op:
    __slots__ = ("eng", "fn", "dma", "deps", "needs_inc", "sem", "val", "prev_val")

    def __init__(self, eng, fn, dma):
        self.eng = eng
        self.fn = fn
        self.dma = dma
        self.deps = ()
        self.needs_inc = False
        self.sem = None
        self.val = 0
        self.prev_val = 0


ENGS = ("pe", "act", "dve", "pool", "sp")
NDMASEM = 24


class Prog:
    def __init__(self, nc):
        self.nc = nc
        self.ops = {e: [] for e in ENGS}
        self.stack = ExitStack()
        self.nalloc = 0
        self.dma_scan = {}

    ARENA = 190 * 1024

    def sb(self, shape, dt, name=None):
        self.nalloc += 1
        name = name or f"sb{self.nalloc}"
        if not hasattr(self, "arena"):
            self.arena = self.stack.enter_context(
                self.nc.sbuf_tensor("arena", [128, self.ARENA], mybir.dt.uint8)).ap()
            self.off = 0
            self.barrier_deps = {e: None for e in ENGS}
        esz = 2 if dt == BF16 else 4
        n = int(np.prod(shape[1:]))
        nbytes = (n * esz + 63) // 64 * 64
        assert self.off + nbytes <= self.ARENA, f"SBUF arena overflow at {name}: {self.off}+{nbytes}"
        ap = self.arena[0:shape[0], self.off:self.off + n * esz].bitcast(dt)
        self.off += nbytes
        if len(shape) == 3:
            ap = ap.rearrange("p (a b) -> p a b", a=shape[1])
        elif len(shape) == 4:
            ap = ap.rearrange("p (a b c) -> p a b c", a=shape[1], b=shape[2])
        return V([H(name)], ap)

    def mark(self):
        return self.off

    def release(self, mark):
        deps = set()
        for e in ENGS:
            last_c = None
            for op in reversed(self.ops[e]):
                if not op.dma:
                    last_c = op
                    break
            if last_c is not None:
                deps.add(last_c)
            for op in self.ops[e][self.dma_scan.get(e, 0):]:
                if op.dma:
                    deps.add(op)
            self.dma_scan[e] = len(self.ops[e])
        for d in deps:
            d.needs_inc = True
        for e in ENGS:
            prev = self.barrier_deps[e] or set()
            self.barrier_deps[e] = prev | deps
        self.off = mark

    def ps(self, shape, dt, name=None):
        self.nalloc += 1
        name = name or f"ps{self.nalloc}"
        t = self.stack.enter_context(self.nc.psum_tensor(name, list(shape), dt))
        return V([H(name)], t.ap())

    def dram(self, name, shape, dt, kind="Internal"):
        t = self.nc.dram_tensor(name, list(shape), dt, kind=kind)
        return V([H(name)], t.ap())

    def add(self, eng, fn, reads=(), writes=(), dma=False):
        op = Op(eng, fn, dma)
        deps = set()
        for v in reads:
            for h in v.hs:
                w = h.last_w
                if w is not None:
                    deps.add(w)
        for v in writes:
            for h in v.hs:
                w = h.last_w
                if w is not None and (w.dma or dma or w.eng != eng):
                    deps.add(w)
                for e, r in h.rd_eng.items():
                    if dma or e != eng:
                        deps.add(r)
                for r in h.rd_dma:
                    deps.add(r)
        if getattr(self, "barrier_deps", None) and self.barrier_deps[eng]:
            deps |= self.barrier_deps[eng]
            self.barrier_deps[eng] = None
        deps.discard(op)
        op.deps = tuple(deps)
        for d in deps:
            d.needs_inc = True
        for v in reads:
            for h in v.hs:
                if dma:
                    h.rd_dma.append(op)
                else:
                    h.rd_eng[eng] = op
        for v in writes:
            for h in v.hs:
                h.last_w = op
                h.rd_eng = {}
                h.rd_dma = []
        self.ops[eng].append(op)
        return op

    def op(self, eng, meth, **kw):
        reads, writes, real = [], [], {}
        kw.pop("_w", None)
        for k, v in kw.items():
            if isinstance(v, V):
                (writes if (k.startswith("out") or k in ("accum_out", "ap")) else reads).append(v)
                real[k] = v.ap
            else:
                real[k] = v
        return self.add(eng, lambda e: getattr(e, meth)(**real), reads, writes)

    def mm(self, out, lhsT, rhs, start, stop):
        return self.add("pe", lambda e: e.matmul(out.ap, lhsT.ap, rhs.ap, start=start, stop=stop),
                        [lhsT, rhs], [out])

    def tr(self, out, in_, ident):
        return self.add("pe", lambda e: e.transpose(out.ap, in_.ap, ident.ap), [in_, ident], [out])

    def dma(self, out, in_, q="sp"):
        return self.add(q, lambda e: e.dma_start(out=out.ap, in_=in_.ap), [in_], [out], dma=True)

    def emit(self):
        nc = self.nc
        st = self.stack
        esem = {e: st.enter_context(nc.semaphore(f"es_{e}")) for e in ENGS}
        dsem = {e: [st.enter_context(nc.semaphore(f"ds_{e}_{i}")) for i in range(NDMASEM)]
                for e in ("sp", "pool", "act")}
        for e in ENGS:
            cnt = 0
            duse = [0] * NDMASEM
            di = 0
            for op in self.ops[e]:
                if op.dma:
                    k = di % NDMASEM
                    di += 1
                    op.sem = dsem[e][k]
                    op.prev_val = duse[k]
                    duse[k] += 16
                    op.val = duse[k]
                elif op.needs_inc:
                    cnt += 1
                    op.sem = esem[e]
                    op.val = cnt
            if e == "sp":
                self.final_dma = [(dsem[e][k], duse[k]) for k in range(NDMASEM) if duse[k] > 0]
            if e == "pool":
                self.final_dma_pool = [(dsem[e][k], duse[k]) for k in range(NDMASEM) if duse[k] > 0]

        def run(ename, eng):
            waited = {}
            for op in self.ops[ename]:
                need = {}
                for d in op.deps:
                    key = id(d.sem)
                    if need.get(key, (None, -1))[1] < d.val:
                        need[key] = (d.sem, d.val)
                if op.dma and op.prev_val > 0:
                    key = id(op.sem)
                    if need.get(key, (None, -1))[1] < op.prev_val:
                        need[key] = (op.sem, op.prev_val)
                for key, (sem, val) in need.items():
                    if waited.get(key, 0) < val:
                        eng.wait_ge(sem, val)
                        waited[key] = val
                ins = op.fn(eng)
                if op.dma:
                    ins.then_inc(op.sem, 16)
                elif op.needs_inc:
                    ins.then_inc(op.sem, 1)
            if ename == "sp":
                for sem, val in self.final_dma:
                    eng.wait_ge(sem, val)
            if ename == "pool":
                for sem, val in self.final_dma_pool:
                    eng.wait_ge(sem, val)

        with nc.Block() as block:
            @block.tensor
            def _(eng):
                run("pe", eng)

            @block.scalar
            def _(eng):
                run("act", eng)

            @block.vector
            def _(eng):
                run("dve", eng)

            @block.gpsimd
            def _(eng):
                run("pool", eng)

            @block.sync
            def _(eng):
                run("sp", eng)
        st.close()


def bf16(a):
    return np.asarray(a, dtype=np.float32).astype(ml_dtypes.bfloat16)


def make_consts(S):
    pos = np.arange(S)
    a = (pos // 128).astype(np.float32)
    b = (pos % 128).astype(np.float32)
    c = {}
    c["ident"] = bf16(np.eye(128))
    c["kaug"] = bf16(np.stack([128 * a, b, np.ones(S), np.ones(S)]))
    sl_a = 2.0 ** (-8.0 * np.arange(1, 9) / 8)
    sl_b = 2.0 ** (-8.0 * np.arange(1, 5) / 4)
    def qaug(sl):
        return bf16(np.stack([np.stack([8 * s * np.ones(S), 8 * s * np.ones(S), -8 * s * 128 * a, -8 * s * b])
                              for s in sl]))
    c["qaug_a"] = qaug(sl_a)
    c["qaug_b"] = qaug(sl_b)
    i = np.arange(128)
    adm = (i[:, None] // 64) <= (i[None, :] // 64)
    fut = np.maximum(i[:, None] - i[None, :], 0).astype(np.float32)
    c["dfix_a"] = bf16(np.stack([-16 * s * fut for s in sl_a], 1))
    c["dfix_b"] = bf16(np.stack([np.where(adm, -16 * s * fut, -30000.0) for s in sl_b], 1))
    c["dmask"] = np.where(adm.T, 0.0, NEG).astype(np.float32)
    c["posrow"] = np.broadcast_to(pos.astype(np.float32)[None, :], (128, S)).copy()
    c["qpos"] = pos.astype(np.float32).reshape(S // 128, 128).T.copy()
    c["slope8"] = np.broadcast_to((8 * sl_a).astype(np.float32)[None, :], (128, 8)).copy()
    c["pow2"] = np.broadcast_to((2.0 ** -(np.arange(NBIS) + 1.0)).astype(np.float32)[None, :], (128, NBIS)).copy()
    c["ones_bf"] = bf16(np.ones((128, 128)))
    c["ones_f"] = np.ones((128, 128), np.float32)
    return c


CONST_SPECS = None


def build_program(S, stop_after=None, nlayers=DEPTH, debug_out=False):
    nc = bass.Bass("TRN2", target_bir_lowering=False)
    P = Prog(nc)
    NT = S // 512
    NS = S // 128
    TOPK = min(256, S // 4)

    def ext(name, shape, dt=F32):
        return P.dram(name, shape, dt, kind="ExternalInput")

    x = ext("x", [S, D])
    mem = ext("mem", [256, D])
    ln_in_g = ext("ln_in_g", [1, D]); ln_in_b = ext("ln_in_b", [1, D])
    w_in = ext("w_in", [DEPTH, D, WIN])
    a_kv_norm = ext("a_kv_norm", [DEPTH, 128])
    a_w_uk = ext("a_w_uk", [DEPTH, 8, 128, 64]); a_w_uv = ext("a_w_uv", [DEPTH, 8, 128, 64])
    b_l = [ext(n, [DEPTH, 64]) for n in ("b_lq1", "b_lk1", "b_lq2", "b_lk2")]
    b_subln = ext("b_subln", [DEPTH, 128])
    w_o = ext("w_o", [DEPTH, D, D])
    ln_g = [ext(f"ln{i}_g", [DEPTH, D]) for i in (1, 2, 3)]
    ln_b = [ext(f"ln{i}_b", [DEPTH, D]) for i in (1, 2, 3)]
    m_wq = ext("m_wq", [DEPTH, D, D]); m_wkv = ext("m_wkv", [DEPTH, D, 2 * D]); m_wo = ext("m_wo", [DEPTH, D, D])
    router_w = ext("router_w", [DEPTH, D, NE]); router_bias = ext("router_bias", [DEPTH, NE])
    need_moe = stop_after is None or stop_after.startswith("p6")
    if need_moe:
        e_w_gate = ext("e_w_gate", [DEPTH, NE, D, 256]); e_w_up = ext("e_w_up", [DEPTH, NE, D, 256])
        e_w_down = ext("e_w_down", [DEPTH, NE, 256, D])
    s_w_gate = ext("s_w_gate", [DEPTH, D, 256]); s_w_up = ext("s_w_up", [DEPTH, D, 256])
    s_w_down = ext("s_w_down", [DEPTH, 256, D])
    cst = make_consts(S)
    cd = {k: ext("c_" + k, list(v.shape), BF16 if v.dtype == ml_dtypes.bfloat16 else F32) for k, v in cst.items()}
    out = P.dram("out", [S, D], F32, kind="ExternalOutput")

    def scratch(name, shape, dt, ntile):
        v = P.dram(name, shape, dt, kind="ExternalOutput" if debug_out else "Internal")
        hs = [H(f"{name}_{i}") for i in range(ntile)]
        return v.ap, hs
    h_tm, h_tm_h = scratch("h_tm", [S, D], F32, NT)
    hT, hT_h = scratch("hT", [D, S], BF16, NT)
    projT, projT_h = scratch("projT", [33, 64, S], BF16, NT)
    kaT, kaT_h = scratch("kaT", [8, 64, S], BF16, NT)
    va, va_h = scratch("va", [S, 512], BF16, NT)
    vb, vb_h = scratch("vb", [S, 512], BF16, NT)
    widx, widx_h = scratch("widx", [S, 8], F32, NT)
    catT, catT_h = scratch("catT", [D, S], BF16, NT)
    dbg = {}

    ident = P.sb([128, 128], BF16, "ident"); P.dma(ident, cd["ident"])
    ones_bf = P.sb([128, 128], BF16, "ones_bf"); P.dma(ones_bf, cd["ones_bf"])
    ones_f = P.sb([128, 128], F32, "ones_f"); P.dma(ones_f, cd["ones_f"])

    pbank = [P.ps([128, 512], F32, f"pb{i}") for i in range(8)]

    cnt = {"ln": 0}
    eps_ln = P.sb([128, 2], F32, "eps_ln")
    P.op("pool", "memset", ap=eps_ln[:, 0:1], constant=LN_EPS)
    P.op("pool", "memset", ap=eps_ln[:, 1:2], constant=RMS_EPS)
    LB = {}

    def ln_alloc():
        LB["g_bc"] = P.sb([128, D], F32, "g_bc"); LB["b_bc"] = P.sb([128, D], F32, "b_bc")
        LB["z"] = [P.sb([128, D], F32, f"ln_z{i}") for i in range(2)]
        LB["hb"] = [P.sb([128, D], BF16, f"ln_hb{i}") for i in range(2)]
        LB["st"] = [P.sb([128, 8], F32, f"ln_st{i}") for i in range(2)]
        LB["hTs"] = [P.sb([128, 8, 512], BF16, f"hTs{i}") for i in range(2)]

    def load_ln_params(g_ap, b_ap):
        P.dma(LB["g_bc"], V(g_ap.hs, g_ap.ap.partition_broadcast(128)))
        P.dma(LB["b_bc"], V(b_ap.hs, b_ap.ap.partition_broadcast(128)))

    def ln_tile(z, s128, final):
        i = cnt["ln"]; cnt["ln"] += 1
        stt = LB["st"][i % 2]
        hb = LB["hb"][i % 2]
        ln_junk = hb; g_bc = LB["g_bc"]; b_bc = LB["b_bc"]; hTs = LB["hTs"]
        P.op("act", "activation", out=ln_junk, in_=z, func=AF.Identity, accum_out=stt[:, 0:1])
        P.op("act", "activation", out=ln_junk, in_=z, func=AF.Square, accum_out=stt[:, 1:2])
        P.op("dve", "tensor_scalar", out=stt[:, 2:3], in0=stt[:, 0:1], scalar1=1.0 / D, scalar2=None, op0=ALU.mult)
        P.op("dve", "tensor_tensor", out=stt[:, 3:4], in0=stt[:, 2:3], in1=stt[:, 2:3], op=ALU.mult)
        P.op("dve", "scalar_tensor_tensor", out=stt[:, 4:5], in0=stt[:, 1:2], scalar=1.0 / D, in1=stt[:, 3:4],
             op0=ALU.mult, op1=ALU.subtract)
        P.op("act", "activation", out=stt[:, 6:7], in_=stt[:, 4:5], func=AF.Sqrt, bias=eps_ln[:, 0:1])
        P.op("dve", "reciprocal", out=stt[:, 5:6], in_=stt[:, 6:7])
        P.op("dve", "tensor_scalar", out=z, in0=z, scalar1=stt[:, 2:3], scalar2=stt[:, 5:6],
             op0=ALU.subtract, op1=ALU.mult)
        P.op("pool", "tensor_tensor", out=z, in0=z, in1=g_bc, op=ALU.mult)
        P.op("pool", "tensor_tensor", out=z, in0=z, in1=b_bc, op=ALU.add)
        tt = s128 // 4
        if final:
            P.dma(out[s128 * 128:(s128 + 1) * 128, :], z, q="pool")
        else:
            P.dma(V([h_tm_h[tt]], h_tm[s128 * 128:(s128 + 1) * 128, :]), z, q="pool")
        if final:
            return
        P.op("act", "activation", out=hb, in_=z, func=AF.Copy)
        pst = pbank[7].bc(BF16)
        hs = hTs[tt % 2]
        for k in range(8):
            P.tr(pst[:, k * 128:(k + 1) * 128], hb[:, k * 128:(k + 1) * 128], ident)
        P.op("act", "activation", out=hs[:, :, (s128 % 4) * 128:(s128 % 4 + 1) * 128],
             in_=pst.re("p (k t) -> p k t", k=8), func=AF.Copy)
        if s128 % 4 == 3:
            P.dma(V([hT_h[tt]], hT.rearrange("(k p) s -> p k s", p=128)[:, :, tt * 512:(tt + 1) * 512]), hs, q="pool")

    p0_mark = P.mark()
    ln_alloc()
    load_ln_params(ln_in_g, ln_in_b)
    for s128 in range(NS):
        z = LB["z"][s128 % 2]
        P.dma(z, x[s128 * 128:(s128 + 1) * 128, :])
        ln_tile(z, s128, False)
    P.release(p0_mark)
    if stop_after == "p0":
        return finish(P, nc)

    bank_i = [0]

    rot = [[0, 1, 2, 3, 4, 5, 6]]

    def nb():
        r = rot[0]
        b = pbank[r[bank_i[0] % len(r)]]
        bank_i[0] += 1
        return b

    ev_i = [0]

    def evac(out, in_, act_only=False):
        ev_i[0] += 1
        if ev_i[0] % 2 or act_only:
            P.op("act", "activation", out=out, in_=in_, func=AF.Copy)
        else:
            P.op("dve", "tensor_copy", out=out, in_=in_)

    def wload(dst, src_ap, nk):
        srcv = src_ap.rearrange("(k p) c -> p k c", p=128)
        for k in range(nk):
            P.dma(dst[:, k, :], V([wH], srcv[:, k, :]), q="pool")
    wH = H("weights")

    def residual_ln(s128, ysrc, final):
        z = LB["z"][cnt["ln"] % 2]
        tt = s128 // 4
        P.dma(z, V([h_tm_h[tt]], h_tm[s128 * 128:(s128 + 1) * 128, :]))
        for hf in range(2):
            P.op("dve", "scalar_tensor_tensor", out=z[:, hf * 512:(hf + 1) * 512], in0=z[:, hf * 512:(hf + 1) * 512],
                 scalar=ALPHA, in1=ysrc[hf], op0=ALU.mult, op1=ALU.add)
        ln_tile(z, s128, final)

    base_mark = P.mark()
    OFF = dict(qa=0, ckv=512, qidx=640, kidx=1152, widx=1216, qb=1224, kb=1736, vb=2248)
    gcols = ([OFF["qa"] + 64 * h for h in range(8)] + [OFF["qidx"] + 64 * h for h in range(8)] + [OFF["kidx"]]
             + [OFF["qb"] + 64 * i for i in range(8)] + [OFF["kb"] + 64 * i for i in range(8)])
    projT_gh = [[H(f"projT_{b}_{t}") for t in range(NT)] for b in range(9)]
    kaT_gh = [[H(f"kaT_{b}_{t}") for t in range(NT)] for b in range(2)]
    catT_gh = [[H(f"catT_{b}_{t}") for t in range(NT)] for b in range(6)]

    for l in range(nlayers):
        lam_init = 0.8 - 0.6 * math.exp(-0.3 * l)
        w_in_sb = P.sb([128, 8, WIN], BF16, "w_in_sb"); wload(w_in_sb, w_in.ap[l], 8)
        wuk_sb = P.sb([128, 8, 64], BF16, "wuk_sb")
        P.dma(wuk_sb, V([wH], a_w_uk.ap[l].rearrange("h c d -> c h d")), q="pool")
        wuv_sb = P.sb([128, 8, 64], BF16, "wuv_sb")
        P.dma(wuv_sb, V([wH], a_w_uv.ap[l].rearrange("h c d -> c h d")), q="pool")
        kvn = P.sb([128, 1], F32, "kvn")
        P.dma(kvn, V([wH], a_kv_norm.ap[l:l + 1, :].rearrange("o c -> c o")))
        hTt = [P.sb([128, 8, 512], BF16, f"hTt{i}") for i in range(2)]
        stg64 = [P.sb([64, 4, 512], BF16, f"stg64_{i}") for i in range(3)]
        stg128 = [P.sb([128, 4, 512], BF16, f"stg128_{i}") for i in range(2)]
        stgw = [P.sb([128, 4, 8], F32, f"stgw_{i}") for i in range(2)]
        csq = P.sb([128, 512], F32, "csq"); rs = P.sb([128, 512], F32, "rs")
        cnT = P.sb([128, 512], BF16, "cnT")
        si = 0
        for tt in range(NT):
            ts = slice(tt * 512, (tt + 1) * 512)
            ht = hTt[tt % 2]
            P.dma(ht, V([hT_h[tt]], hT.rearrange("(k p) s -> p k s", p=128)[:, :, ts]))
            for b in range(9):
                gs = list(range(b * 4, min(b * 4 + 4, 33)))
                stg = stg64[si % 3]; si += 1
                for j, g in enumerate(gs):
                    ps = nb()
                    for k in range(8):
                        P.mm(ps[0:64, :], w_in_sb[:, k, gcols[g]:gcols[g] + 64], ht[:, k, :], k == 0, k == 7)
                    evac(stg[:, j, :], ps[0:64, :])
                P.dma(V([projT_gh[b][tt]], projT[gs[0]:gs[-1] + 1, :, ts].rearrange("g p s -> p g s")),
                      stg[:, 0:len(gs), :], q="pool")
            pc = nb()
            for k in range(8):
                P.mm(pc, w_in_sb[:, k, OFF["ckv"]:OFF["ckv"] + 128], ht[:, k, :], k == 0, k == 7)
            P.op("act", "activation", out=csq, in_=pc, func=AF.Square)
            pss = nb()
            P.mm(pss, ones_f, csq, True, True)
            P.op("act", "activation", out=rs, in_=pss, func=AF.Sqrt, scale=1.0 / 128, bias=eps_ln[:, 1:2])
            P.op("dve", "reciprocal", out=rs, in_=rs)
            P.op("dve", "scalar_tensor_tensor", out=cnT, in0=pc, scalar=kvn[:, 0:1], in1=rs, op0=ALU.mult, op1=ALU.mult)
            for b in range(2):
                stg = stg64[si % 3]; si += 1
                for j in range(4):
                    ps = nb()
                    P.mm(ps[0:64, :], wuk_sb[:, b * 4 + j, :], cnT, True, True)
                    evac(stg[:, j, :], ps[0:64, :])
                P.dma(V([kaT_gh[b][tt]], kaT[b * 4:b * 4 + 4, :, ts].rearrange("g p s -> p g s")), stg, q="pool")
            sg = stg128[0]
            for st_ in range(4):
                ps = nb()
                P.mm(ps, cnT[:, st_ * 128:(st_ + 1) * 128], wuv_sb.re("p h d -> p (h d)"), True, True)
                evac(sg[:, st_, :], ps)
            P.dma(V([va_h[tt]], va[ts, :].rearrange("(t p) c -> p t c", p=128)), sg, q="pool")
            sg = stg128[1]; sw = stgw[tt % 2]
            for st_ in range(4):
                ps = nb()
                for k in range(8):
                    P.mm(ps, ht[:, k, st_ * 128:(st_ + 1) * 128], w_in_sb[:, k, OFF["vb"]:OFF["vb"] + 512], k == 0, k == 7)
                evac(sg[:, st_, :], ps)
                ps = nb()
                for k in range(8):
                    P.mm(ps[:, 0:8], ht[:, k, st_ * 128:(st_ + 1) * 128], w_in_sb[:, k, OFF["widx"]:OFF["widx"] + 8], k == 0, k == 7)
                evac(sw[:, st_, :], ps[:, 0:8])
            P.dma(V([vb_h[tt]], vb[ts, :].rearrange("(t p) c -> p t c", p=128)), sg, q="pool")
            P.dma(V([widx_h[tt]], widx[ts, :].rearrange("(t p) c -> p t c", p=128)), sw, q="pool")
        P.release(base_mark)
        if stop_after == f"p1_{l}":
            return finish(P, nc)
        cq = {}
        kidx_sb = P.sb([64, S], BF16, "kidx_sb")
        P.dma(kidx_sb, V(projT_gh[4], projT[16, :, :]))
        ka_ring = [P.sb([69, S], BF16, f"ka{i}") for i in range(2)]
        for kr in ka_ring:
            P.dma(kr[64:65, :], cd["kaug"][2:3, :])
            P.dma(kr[65:69, :], cd["kaug"][0:4, :])
        va_sb = P.sb([128, NS, 8, 65], BF16, "va_sb")
        P.op("pool", "memset", ap=va_sb[:, :, :, 64:65], constant=1.0)
        for t in range(NS):
            P.dma(va_sb[:, t, :, 0:64], V([va_h[t // 4]], va[t * 128:(t + 1) * 128, :].rearrange("p (h d) -> p h d", h=8)))
        widx_sb = P.sb([128, NS, 8], F32, "widx_sb")
        P.dma(widx_sb, V(widx_h, widx.rearrange("(t p) c -> p t c", p=128)))
        isc = P.sb([128, S], F32, "isc")
        junk = P.sb([128, S], BF16, "junkb")
        maskq = [P.sb([128, S], BF16, f"maskq{i}") for i in range(1)]
        rot[0] = [0, 1, 2, 3, 4]
        maskT = P.sb([128, NS, 512], BF16, "maskT")
        posrow = P.sb([128, S], BF16, "posrow"); P.dma(posrow, cd["posrow"], q="pool")
        qpos = P.sb([128, NS], F32, "qpos"); P.dma(qpos, cd["qpos"])
        slope8 = P.sb([128, 8], F32, "slope8"); P.dma(slope8, cd["slope8"])
        pow2 = P.sb([128, NBIS], F32, "pow2"); P.dma(pow2, cd["pow2"])
        dmask = P.sb([128, 128], F32, "dmask"); P.dma(dmask, cd["dmask"])
        dfix_a = P.sb([128, 8, 128], BF16, "dfix_a"); P.dma(dfix_a, cd["dfix_a"])
        zero_c = P.sb([128, 1], F32, "zero_c"); P.op("pool", "memset", ap=zero_c, constant=0.0)
        negc = P.sb([128, 1], F32, "negc"); P.op("pool", "memset", ap=negc, constant=-30000.0)
        qi_ring = [P.sb([64, 8, 512], BF16, f"qi{i}") for i in range(1)]
        q_ring = [P.sb([69, 8, 512], BF16, f"qa{i}") for i in range(1)]
        rl_ring = [P.sb([128, 512], F32, f"rl{i}") for i in range(2)]
        e_ring = [P.sb([128, 512], BF16, f"e{i}") for i in range(4)]
        bst = [P.sb([128, 8], F32, f"bst{i}") for i in range(2)]
        steps = P.sb([128, NBIS], F32, "steps")
        shq = P.sb([128, 8], BF16, "shq")
        shT = P.sb([8, 512], BF16, "shT")
        osb = P.sb([65, 512], F32, "osb")
        oa_stg = [P.sb([64, 4, 512], BF16, f"oa_stg{i}") for i in range(2)]
        ei = 0; hh = 0; sti = 0
        for qt in range(NT):
            ts = slice(qt * 512, (qt + 1) * 512)
            qi = qi_ring[0]; qs = q_ring[0]
            P.dma(qi, V(projT_gh[2] + projT_gh[3], projT[8:16, :, ts].rearrange("g p s -> p g s")))
            P.dma(qs[0:64, :, :], V(projT_gh[0] + projT_gh[1], projT[0:8, :, ts].rearrange("g p s -> p g s")))
            P.dma(qs[65:69, :, :], V(cd["qaug_a"].hs, cd["qaug_a"].ap[:, :, ts].rearrange("h r s -> r h s")))
            for st_ in range(4):
                blk = qt * 4 + st_
                q0 = blk * 128
                nkeys = q0 + 128
                mq = maskq[0]
                b_ = bst[blk % 2]
                for h in range(8):
                    for kc in range((nkeys + 511) // 512):
                        w = min(512, nkeys - kc * 512)
                        ps = nb()
                        P.mm(ps[:, 0:w], qi[:, h, st_ * 128:(st_ + 1) * 128], kidx_sb[:, kc * 512:kc * 512 + w], True, True)
                        rl = rl_ring[ei % 2]; ei += 1
                        P.op("act", "activation", out=rl[:, 0:w], in_=ps[:, 0:w], func=AF.Relu)
                        dst = isc[:, kc * 512:kc * 512 + w]
                        if h == 0:
                            P.op("dve", "tensor_scalar", out=dst, in0=rl[:, 0:w], scalar1=widx_sb[:, blk, 0:1], scalar2=None, op0=ALU.mult)
                        else:
                            P.op("dve", "scalar_tensor_tensor", out=dst, in0=rl[:, 0:w], scalar=widx_sb[:, blk, h:h + 1], in1=dst,
                                 op0=ALU.mult, op1=ALU.add)
                P.op("dve", "tensor_tensor", out=isc[:, q0:q0 + 128], in0=isc[:, q0:q0 + 128], in1=dmask, op=ALU.add)
                tcol = b_[:, 4:5]
                if q0 < TOPK:
                    P.op("dve", "memset", ap=tcol, constant=-1.0e29)
                else:
                    P.op("dve", "tensor_reduce", out=b_[:, 0:1], in_=isc[:, 0:nkeys], axis=AX.X, op=ALU.max)
                    P.op("dve", "tensor_reduce", out=b_[:, 1:2], in_=isc[:, 0:q0], axis=AX.X, op=ALU.min)
                    P.op("dve", "tensor_tensor", out=b_[:, 2:3], in0=b_[:, 0:1], in1=b_[:, 1:2], op=ALU.subtract)
                    P.op("dve", "tensor_tensor", out=b_[:, 3:4], in0=b_[:, 0:1], in1=b_[:, 1:2], op=ALU.add)
                    P.op("dve", "tensor_scalar", out=tcol, in0=b_[:, 3:4], scalar1=0.5, scalar2=None, op0=ALU.mult)
                    P.op("dve", "tensor_scalar", out=steps, in0=pow2, scalar1=b_[:, 2:3], scalar2=None, op0=ALU.mult)
                    for n in range(NBIS):
                        P.op("dve", "tensor_scalar", out=junk[:, 0:nkeys], in0=isc[:, 0:nkeys], scalar1=tcol, scalar2=zero_c[:, 0:1],
                             op0=ALU.is_ge, op1=ALU.add, accum_out=b_[:, 5:6])
                        P.op("dve", "tensor_scalar", out=b_[:, 6:7], in0=b_[:, 5:6], scalar1=TOPK - 0.5, scalar2=0.5,
                             op0=ALU.is_gt, op1=ALU.subtract)
                        P.op("dve", "scalar_tensor_tensor", out=tcol, in0=b_[:, 6:7], scalar=steps[:, n:n + 1], in1=tcol,
                             op0=ALU.mult, op1=ALU.add)
                P.op("dve", "tensor_scalar", out=mq[:, 0:nkeys], in0=isc[:, 0:nkeys], scalar1=tcol, scalar2=None, op0=ALU.is_ge)
                P.op("dve", "tensor_tensor", out=junk[:, 0:nkeys], in0=mq[:, 0:nkeys], in1=posrow[:, 0:nkeys], op=ALU.mult)
                P.op("dve", "tensor_reduce", out=b_[:, 7:8], in_=junk[:, 0:nkeys], axis=AX.X, op=ALU.max)
                P.op("dve", "tensor_tensor", out=b_[:, 3:4], in0=qpos[:, blk:blk + 1], in1=b_[:, 7:8], op=ALU.subtract)
                P.op("dve", "tensor_scalar", out=shq, in0=slope8, scalar1=b_[:, 3:4], scalar2=None, op0=ALU.mult)
                pst = nb().bc(BF16)
                P.tr(pst[0:8, 0:128], shq, ident)
                evac(shT[:, st_ * 128:(st_ + 1) * 128], pst[0:8, 0:128], True)
                for kb0 in range(0, blk + 1, 8):
                    n_ = min(8, blk + 1 - kb0)
                    pst = nb().bc(BF16)
                    for j in range(n_):
                        P.tr(pst[:, j * 128:(j + 1) * 128], mq[:, (kb0 + j) * 128:(kb0 + j + 1) * 128], ident)
                    P.op("act", "activation", out=maskT[:, kb0:kb0 + n_, st_ * 128:(st_ + 1) * 128],
                         in_=pst[:, 0:n_ * 128].re("p (k t) -> p k t", k=n_), func=AF.Identity, scale=30000.0, bias=negc[:, 0:1])
            for h in range(8):
                P.dma(qs[64:65, h, :], shT[h:h + 1, :])
            nkb = 4 * (qt + 1)
            plist = []
            for h in range(8):
                for kb in range(nkb):
                    plist.append((h, kb))
            st8 = {}

            def s_part(h, kb, qt=qt, nkb=nkb, qs=qs, ts=ts):
                if kb == 0:
                    ka = ka_ring[st8["hh"] % 2]; st8["hh"] += 1
                    P.dma(ka[0:64, 0:nkb * 128], V(kaT_gh[h // 4], kaT[h, :, 0:nkb * 128]))
                    st8[("ka", h)] = ka
                ka = st8[("ka", h)]
                c0 = max(0, kb - 4 * qt) * 128
                ps = nb()
                kslc = ka[0:69, kb * 128:(kb + 1) * 128]
                if kb >= 4 * qt:
                    P.mm(ps[:, c0:c0 + 128], kslc, qs[0:69, h, c0:c0 + 128], True, False)
                    P.mm(ps[:, c0:c0 + 128], ident, dfix_a[:, h, :], False, False)
                    P.mm(ps[:, c0:c0 + 128], ident, maskT[:, kb, c0:c0 + 128], False, True)
                    if c0 + 128 < 512:
                        P.mm(ps[:, c0 + 128:512], kslc, qs[0:69, h, c0 + 128:512], True, False)
                        P.mm(ps[:, c0 + 128:512], ident, maskT[:, kb, c0 + 128:512], False, True)
                else:
                    P.mm(ps[:, c0:512], kslc, qs[0:69, h, c0:512], True, False)
                    P.mm(ps[:, c0:512], ident, maskT[:, kb, c0:512], False, True)
                E = e_ring[st8["sti"] % 4]; st8["sti"] += 1
                P.op("act", "activation", out=E[:, c0:512], in_=ps[:, c0:512], func=AF.Exp, scale=0.125)
                st8[("E", h, kb)] = E

            def r_part(h, kb, qt=qt, nkb=nkb, ts=ts):
                c0 = max(0, kb - 4 * qt) * 128
                E = st8.pop(("E", h, kb))
                po = pbank[5 + (h % 2)]
                P.mm(po[0:65, c0:512], va_sb[:, kb, h, :], E[:, c0:512], kb == 0, kb == nkb - 1)
                if kb == nkb - 1:
                    P.op("act", "activation", out=osb, in_=po[0:65, :], func=AF.Copy)
                    P.op("dve", "reciprocal", out=osb[64:65, :], in_=osb[64:65, :])
                    pb_ = pbank[7]
                    P.mm(pb_[0:64, :], ones_f[64:65, 0:64], osb[64:65, :], True, True)
                    og = oa_stg[(h // 4) % 2]
                    P.op("dve", "tensor_tensor", out=og[:, h % 4, :], in0=osb[0:64, :], in1=pb_[0:64, :], op=ALU.mult)
                    if h % 4 == 3:
                        h0 = h - 3
                        P.dma(V([catT_gh[h // 4][qt]], catT[h0 * 64:(h0 + 4) * 64, ts].rearrange("(g p) s -> p g s", p=64)), og, q="pool")
            st8["hh"] = hh; st8["sti"] = sti
            LA = 2
            for i in range(len(plist) + LA):
                if i < len(plist):
                    s_part(*plist[i])
                if i - LA >= 0:
                    r_part(*plist[i - LA])
            hh = st8["hh"]; sti = st8["sti"]
        P.release(base_mark)
        if stop_after == f"p2_{l}":
            return finish(P, nc)
        rot[0] = [0, 1, 2]
        kb_ring = [P.sb([68, S], BF16, f"kbr{i}") for i in range(4)]
        for kr in kb_ring:
            P.dma(kr[64:68, :], cd["kaug"][0:4, :])
        vb_sb = P.sb([128, NS, 512], BF16, "vb_sb")
        P.dma(vb_sb, V(vb_h, vb.rearrange("(t p) c -> p t c", p=128)))
        qb = P.sb([68, 8, 512], BF16, "qb")
        dfix_b = P.sb([128, 4, 128], BF16, "dfix_b"); P.dma(dfix_b, cd["dfix_b"])
        lt = [P.sb([128, 64], F32, f"lt{i}") for i in range(4)]
        for i in range(4):
            P.dma(lt[i], V(b_l[i].hs, b_l[i].ap[l:l + 1, :].partition_broadcast(128)))
        lsc = P.sb([128, 8], F32, "lsc")
        for j in range(2):
            P.op("dve", "tensor_tensor", out=lt[2 * j], in0=lt[2 * j], in1=lt[2 * j + 1], op=ALU.mult)
            P.op("dve", "tensor_reduce", out=lsc[:, j:j + 1], in_=lt[2 * j], axis=AX.X, op=ALU.add)
            P.op("act", "activation", out=lsc[:, 2 + j:3 + j], in_=lsc[:, j:j + 1], func=AF.Exp)
        P.op("dve", "tensor_tensor", out=lsc[:, 4:5], in0=lsc[:, 3:4], in1=lsc[:, 2:3], op=ALU.subtract)
        P.op("dve", "tensor_scalar", out=lsc[:, 5:6], in0=lsc[:, 4:5], scalar1=-lam_init, scalar2=None, op0=ALU.add)
        subc = P.sb([128, 1], F32, "subc")
        P.dma(subc, V([wH], b_subln.ap[l:l + 1, :].rearrange("o c -> c o")))
        P.op("dve", "tensor_scalar", out=subc, in0=subc, scalar1=1.0 - lam_init, scalar2=None, op0=ALU.mult)
        e_ring = [P.sb([128, 512], BF16, f"eb{i}") for i in range(4)]
        r_ = [P.sb([128, 512], F32, f"rr{i}") for i in range(4)]
        ob_stg = [P.sb([128, 512], BF16, f"ob_stg{i}") for i in range(2)]
        ki = 0; sti = 0
        for qt in range(NT):
            ts = slice(qt * 512, (qt + 1) * 512)
            nkb = 4 * (qt + 1)
            P.dma(qb[0:64, :, :], V(projT_gh[4] + projT_gh[5] + projT_gh[6], projT[17:25, :, ts].rearrange("g p s -> p g s")))
            for g in range(8):
                P.dma(qb[64:68, g, :], V(cd["qaug_b"].hs, cd["qaug_b"].ap[g // 2, :, ts]))
            steps = []
            for h in range(4):
                for kb in range(nkb):
                    for j in range(2):
                        steps.append((h, kb, j))
            st8 = {"ki": ki, "sti": sti}

            def s_part(h, kb, j, qt=qt, nkb=nkb):
                if kb == 0:
                    K_ = kb_ring[st8["ki"] % 4]; st8["ki"] += 1
                    P.dma(K_[0:64, 0:nkb * 128], V(projT_gh[6] + projT_gh[7] + projT_gh[8], projT[25 + 2 * h + j, :, 0:nkb * 128]))
                    st8[("K", h, j)] = K_
                K_ = st8[("K", h, j)]
                c0 = max(0, kb - 4 * qt) * 128
                ps = nb()
                kslc = K_[0:68, kb * 128:(kb + 1) * 128]
                if kb >= 4 * qt:
                    P.mm(ps[:, c0:c0 + 128], kslc, qb[0:68, 2 * h + j, c0:c0 + 128], True, False)
                    P.mm(ps[:, c0:c0 + 128], ident, dfix_b[:, h, :], False, True)
                    if c0 + 128 < 512:
                        P.mm(ps[:, c0 + 128:512], kslc, qb[0:68, 2 * h + j, c0 + 128:512], True, True)
                else:
                    P.mm(ps[:, c0:512], kslc, qb[0:68, 2 * h + j, c0:512], True, True)
                E = e_ring[st8["sti"] % 4]; st8["sti"] += 1
                P.op("act", "activation", out=E[:, c0:512], in_=ps[:, c0:512], func=AF.Exp, scale=0.125)
                st8[("E", h, kb, j)] = E

            def r_part(h, kb, j, qt=qt, nkb=nkb, ts=ts):
                c0 = max(0, kb - 4 * qt) * 128
                E = st8.pop(("E", h, kb, j))
                O_ = [pbank[3], pbank[4]]; Dn = [pbank[5], pbank[6]]
                P.mm(O_[j][:, c0:512], vb_sb[:, kb, h * 128:(h + 1) * 128], E[:, c0:512], kb == 0, kb == nkb - 1)
                P.mm(Dn[j][:, c0:512], ones_bf, E[:, c0:512], kb == 0, kb == nkb - 1)
                if kb == nkb - 1 and j == 1:
                    for jj in range(2):
                        P.op("dve", "reciprocal", out=r_[jj], in_=Dn[jj])
                        P.op("dve", "tensor_tensor", out=r_[jj], in0=O_[jj], in1=r_[jj], op=ALU.mult)
                    P.op("dve", "scalar_tensor_tensor", out=r_[2], in0=r_[1], scalar=lsc[:, 5:6], in1=r_[0], op0=ALU.mult, op1=ALU.add)
                    P.op("act", "activation", out=r_[3], in_=r_[2], func=AF.Square)
                    pss = pbank[7]
                    P.mm(pss, ones_f, r_[3], True, True)
                    P.op("act", "activation", out=r_[3], in_=pss, func=AF.Sqrt, scale=1.0 / 128, bias=eps_ln[:, 1:2])
                    P.op("dve", "reciprocal", out=r_[3], in_=r_[3])
                    og = ob_stg[h % 2]
                    P.op("dve", "scalar_tensor_tensor", out=og, in0=r_[2], scalar=subc[:, 0:1], in1=r_[3], op0=ALU.mult, op1=ALU.mult)
                    P.dma(V([catT_gh[2 + h][qt]], catT[512 + h * 128:512 + (h + 1) * 128, ts]), og, q="pool")
            LA = 2
            for i in range(len(steps) + LA):
                if i < len(steps):
                    s_part(*steps[i])
                if i - LA >= 0:
                    r_part(*steps[i - LA])
            ki = st8["ki"]; sti = st8["sti"]
        P.release(base_mark)
        if stop_after == f"p3_{l}":
            return finish(P, nc)
        rot[0] = [0, 1, 2, 3, 4, 5, 6]
        ln_alloc()
        load_ln_params(V(ln_g[0].hs, ln_g[0].ap[l:l + 1, :]), V(ln_b[0].hs, ln_b[0].ap[l:l + 1, :]))
        wo_sb = P.sb([128, 8, D], BF16, "wo_sb"); wload(wo_sb, w_o.ap[l], 8)
        ct_ring = [P.sb([128, 8, 512], BF16, f"ct{i}") for i in range(2)]
        for tt in range(NT):
            ts = slice(tt * 512, (tt + 1) * 512)
            ct = ct_ring[tt % 2]
            P.dma(ct, V([g_[tt] for g_ in catT_gh], catT.rearrange("(k p) s -> p k s", p=128)[:, :, ts]))
            for st_ in range(4):
                ys = [nb(), nb()]
                for hf in range(2):
                    for k in range(8):
                        P.mm(ys[hf], ct[:, k, st_ * 128:(st_ + 1) * 128], wo_sb[:, k, hf * 512:(hf + 1) * 512], k == 0, k == 7)
                residual_ln(tt * 4 + st_, ys, False)
        P.release(base_mark)
        if stop_after == f"p4_{l}":
            return finish(P, nc)
        rot[0] = [0, 1, 2, 3, 4]
        ln_alloc()
        load_ln_params(V(ln_g[1].hs, ln_g[1].ap[l:l + 1, :]), V(ln_b[1].hs, ln_b[1].ap[l:l + 1, :]))
        wq_sb = P.sb([128, 8, D], BF16, "wq_sb"); wload(wq_sb, m_wq.ap[l], 8)
        wkv_sb = P.sb([128, 8, 2 * D], BF16, "wkv_sb"); wload(wkv_sb, m_wkv.ap[l], 8)
        wo2_sb = P.sb([128, 8, D], BF16, "wo2_sb"); wload(wo2_sb, m_wo.ap[l], 8)
        mem_b = P.sb([128, 2, D], BF16, "mem_b")
        P.dma(mem_b, V(mem.hs, mem.ap.rearrange("(t p) d -> p t d", p=128)), q="pool")
        memT = P.sb([128, 8, 256], BF16, "memT")
        for t in range(2):
            pst = nb().bc(BF16)
            for k in range(8):
                P.tr(pst[:, k * 128:(k + 1) * 128], mem_b[:, t, k * 128:(k + 1) * 128], ident)
            evac(memT[:, :, t * 128:(t + 1) * 128], pst.re("p (k t) -> p k t", k=8))
        KT = P.sb([128, 8, 256], BF16, "KT")
        for dc in range(8):
            ps = nb()
            for k in range(8):
                P.mm(ps[:, 0:256], wkv_sb[:, k, dc * 128:(dc + 1) * 128], memT[:, k, :], k == 0, k == 7)
            evac(KT[:, dc, :], ps[:, 0:256])
        Vm = P.sb([128, 2, D], BF16, "Vm")
        for t in range(2):
            for hf in range(2):
                ps = nb()
                for k in range(8):
                    P.mm(ps, memT[:, k, t * 128:(t + 1) * 128], wkv_sb[:, k, D + hf * 512:D + (hf + 1) * 512], k == 0, k == 7)
                evac(Vm[:, t, hf * 512:(hf + 1) * 512], ps)
        ht_ring = [P.sb([128, 8, 512], BF16, f"ht5_{i}") for i in range(2)]
        QT = P.sb([128, 8, 512], BF16, "QT"); OT = P.sb([128, 8, 512], BF16, "OT")
        e5 = [P.sb([128, 2, 512], BF16, f"e5_{i}") for i in range(2)]
        rd = P.sb([128, 512], F32, "rd5")
        for tt in range(NT):
            ts = slice(tt * 512, (tt + 1) * 512)
            ht = ht_ring[tt % 2]
            P.dma(ht, V([hT_h[tt]], hT.rearrange("(k p) s -> p k s", p=128)[:, :, ts]))
            for dc in range(8):
                ps = nb()
                for k in range(8):
                    P.mm(ps, wq_sb[:, k, dc * 128:(dc + 1) * 128], ht[:, k, :], k == 0, k == 7)
                evac(QT[:, dc, :], ps)
            for h in range(4):
                E = e5[h % 2]
                for mb in range(2):
                    ps = nb()
                    for dc in range(2):
                        P.mm(ps, KT[:, 2 * h + dc, mb * 128:(mb + 1) * 128], QT[:, 2 * h + dc, :], dc == 0, dc == 1)
                    P.op("act", "activation", out=E[:, mb, :], in_=ps, func=AF.Exp, scale=1.0 / 16)
                den = pbank[5]
                for mb in range(2):
                    P.mm(den, ones_bf, E[:, mb, :], mb == 0, mb == 1)
                P.op("dve", "reciprocal", out=rd, in_=den)
                for dvc in range(2):
                    po = pbank[6] if dvc == 0 else nb()
                    for mb in range(2):
                        P.mm(po, Vm[:, mb, h * 256 + dvc * 128:h * 256 + (dvc + 1) * 128], E[:, mb, :], mb == 0, mb == 1)
                    P.op("dve", "tensor_tensor", out=OT[:, 2 * h + dvc, :], in0=po, in1=rd, op=ALU.mult)
            for st_ in range(4):
                ys = [nb(), nb()]
                for hf in range(2):
                    for k in range(8):
                        P.mm(ys[hf], OT[:, k, st_ * 128:(st_ + 1) * 128], wo2_sb[:, k, hf * 512:(hf + 1) * 512], k == 0, k == 7)
                residual_ln(tt * 4 + st_, ys, False)
        P.release(base_mark)
        if stop_after == f"p5_{l}":
            return finish(P, nc)
        rot[0] = [0, 1, 2, 3, 4, 5, 6]
        last = (l == nlayers - 1)
        ln_alloc()
        load_ln_params(V(ln_g[2].hs, ln_g[2].ap[l:l + 1, :]), V(ln_b[2].hs, ln_b[2].ap[l:l + 1, :]))
        rw_sb = P.sb([128, 8, NE], BF16, "rw_sb"); wload(rw_sb, router_w.ap[l], 8)
        rbias = P.sb([128, NE], F32, "rbias")
        P.dma(rbias, V(router_bias.hs, router_bias.ap[l:l + 1, :].partition_broadcast(128)))
        G = P.sb([128, NS, NE + 1], F32, "G")
        P.op("pool", "memset", ap=G[:, :, NE:NE + 1], constant=1.0)
        SH = S // 2
        NSH = NS // 2
        hTh = P.sb([128, 8, SH], BF16, "hTh")
        yacc = P.sb([128, NSH, D], F32, "yacc")
        wg_r = [P.sb([128, 8, 256], BF16, f"wg{i}") for i in range(2)]
        wu_r = [P.sb([128, 8, 256], BF16, f"wu{i}") for i in range(2)]
        wd_r = [P.sb([128, 2, D], BF16, f"wd{i}") for i in range(2)]
        sg_r = [P.sb([128, 512], F32, f"sg{i}") for i in range(2)]
        at_r = [P.sb([128, 2, 512], BF16, f"at{i}") for i in range(2)]
        rt = [P.sb([128, NE], F32, f"rt{i}") for i in range(3)]
        rsm = P.sb([128, 16], F32, "rsm")
        wi = 0; ai = 0
        for half in range(2):
            P.dma(hTh, V(hT_h, hT.rearrange("(k p) s -> p k s", p=128)[:, :, half * SH:(half + 1) * SH]))
            for s_ in range(NSH):
                sg_ = half * NSH + s_
                ps = nb()
                for k in range(8):
                    P.mm(ps[:, 0:NE], hTh[:, k, s_ * 128:(s_ + 1) * 128], rw_sb[:, k, :], k == 0, k == 7)
                P.op("act", "activation", out=rt[0], in_=ps[:, 0:NE], func=AF.Sigmoid)
                P.op("dve", "tensor_tensor", out=rt[1], in0=rt[0], in1=rbias, op=ALU.add)
                P.op("dve", "max", out=rsm[:, 0:8], in_=rt[1])
                P.op("dve", "tensor_scalar", out=rt[2], in0=rt[1], scalar1=rsm[:, 7:8], scalar2=None, op0=ALU.is_ge)
                P.op("dve", "tensor_tensor", out=rt[2], in0=rt[2], in1=rt[0], op=ALU.mult)
                P.op("dve", "tensor_reduce", out=rsm[:, 8:9], in_=rt[2], axis=AX.X, op=ALU.add)
                P.op("dve", "reciprocal", out=rsm[:, 9:10], in_=rsm[:, 8:9])
                P.op("dve", "tensor_scalar", out=rt[2], in0=rt[2], scalar1=rsm[:, 9:10], scalar2=None, op0=ALU.mult)
                P.op("dve", "tensor_scalar", out=G[:, sg_, 0:NE], in0=rt[2], scalar1=2.5, scalar2=None, op0=ALU.mult)
            steps = [(e, tt) for e in range(NE + 1) for tt in range(NSH // 4)]
            st8 = {"wi": 0, "ai": 0}

            def s_part(e, tt):
                if tt == 0:
                    wi_ = st8["wi"]; st8["wi"] += 1
                    wg = wg_r[wi_ % 2]; wu = wu_r[wi_ % 2]; wd = wd_r[wi_ % 2]
                    if e < NE:
                        wload(wg, e_w_gate.ap[l, e], 8); wload(wu, e_w_up.ap[l, e], 8); wload(wd, e_w_down.ap[l, e], 2)
                    else:
                        wload(wg, s_w_gate.ap[l], 8); wload(wu, s_w_up.ap[l], 8); wload(wd, s_w_down.ap[l], 2)
                    st8[("w", e)] = (wg, wu, wd)
                wg, wu, wd = st8[("w", e)]
                tsl = slice(tt * 512, (tt + 1) * 512)
                ai_ = st8["ai"]; st8["ai"] += 1
                gps = [pbank[0 + 2 * (ai_ % 2)], pbank[1 + 2 * (ai_ % 2)]]
                ups = [pbank[4], pbank[5]]
                for m in range(2):
                    for k in range(8):
                        P.mm(gps[m], wg[:, k, m * 128:(m + 1) * 128], hTh[:, k, tsl], k == 0, k == 7)
                for m in range(2):
                    for k in range(8):
                        P.mm(ups[m], wu[:, k, m * 128:(m + 1) * 128], hTh[:, k, tsl], k == 0, k == 7)
                at = at_r[ai_ % 2]
                for m in range(2):
                    sg = sg_r[m]
                    P.op("act", "activation", out=sg, in_=gps[m], func=AF.Silu)
                    P.op("dve", "tensor_tensor", out=at[:, m, :], in0=sg, in1=ups[m], op=ALU.mult)
                st8[("at", e, tt)] = at

            def r_part(e, tt, half=half):
                wg, wu, wd = st8[("w", e)]
                at = st8.pop(("at", e, tt))
                for st_ in range(4):
                    s_ = tt * 4 + st_
                    sg_ = half * NSH + s_
                    for hf in range(2):
                        py = pbank[6 + hf]
                        for m in range(2):
                            P.mm(py, at[:, m, st_ * 128:(st_ + 1) * 128], wd[:, m, hf * 512:(hf + 1) * 512], m == 0, m == 1)
                        dst = yacc[:, s_, hf * 512:(hf + 1) * 512]
                        if e == 0:
                            P.op("dve", "tensor_scalar", out=dst, in0=py, scalar1=G[:, sg_, e:e + 1], scalar2=None, op0=ALU.mult)
                        else:
                            P.op("dve", "scalar_tensor_tensor", out=dst, in0=py, scalar=G[:, sg_, e:e + 1], in1=dst,
                                 op0=ALU.mult, op1=ALU.add)
            LA = 1
            for i in range(len(steps) + LA):
                if i < len(steps):
                    s_part(*steps[i])
                if i - LA >= 0:
                    r_part(*steps[i - LA])
            for s_ in range(NSH):
                residual_ln(half * NSH + s_, [yacc[:, s_, 0:512], yacc[:, s_, 512:1024]], last)
        P.release(base_mark)
        if stop_after == f"p6_{l}":
            return finish(P, nc)
    return finish(P, nc)


def finish(P, nc):
    P.emit()
    return nc


def core_inputs(inp, core, S, need_moe=True):
    m = {}
    m["x"] = np.ascontiguousarray(inp["x"][core])
    m["mem"] = np.ascontiguousarray(inp["mem"][core])
    for k, v in inp.items():
        if k in ("x", "mem") or (not need_moe and k.startswith("e_w_")):
            continue
        if k in ("ln_in_g", "ln_in_b"):
            v = v.reshape(1, -1)
        m[k] = np.ascontiguousarray(v)
    for k, v in make_consts(S).items():
        m["c_" + k] = v
    return m


_CACHE = {}


def kernel(**inputs):
    S = inputs["x"].shape[1]
    nb = inputs["x"].shape[0]
    inp = {k: np.asarray(v) for k, v in inputs.items()}
    if S not in _CACHE:
        _CACHE[S] = build_program(S)
    nc = _CACHE[S]
    in_maps = [core_inputs(inp, c, S) for c in range(nb)]
    res = run_bass_kernel_spmd(nc, in_maps, core_ids=list(range(nb)))
    return np.stack([np.asarray(r["out"]) for r in res.results], axis=0).astype(np.float32)
```

```python
import math
from contextlib import ExitStack

import numpy as np
import ml_dtypes

import concourse.bass as bass
import concourse.mybir as mybir
from concourse.bass_utils import run_bass_kernel_spmd

F32 = mybir.dt.float32
BF16 = mybir.dt.bfloat16
ALU = mybir.AluOpType
AF = mybir.ActivationFunctionType
AX = mybir.AxisListType

D = 1024
DEPTH = 2
NE = 64
ALPHA = (2 * DEPTH) ** 0.25
LN_EPS = 1e-5
RMS_EPS = 1e-6
WIN = 2760
NBIS = 14
NEG = -1.0e30


class H:
    __slots__ = ("name", "last_w", "rd_eng", "rd_dma")

    def __init__(self, name=""):
        self.name = name
        self.last_w = None
        self.rd_eng = {}
        self.rd_dma = []


class V:
    def __init__(self, hs, ap):
        self.hs = hs
        self.ap = ap

    def __getitem__(self, k):
        return V(self.hs, self.ap[k])

    def re(self, s, **kw):
        return V(self.hs, self.ap.rearrange(s, **kw))

    def bc(self, dt):
        return V(self.hs, self.ap.bitcast(dt))


class Op:
    __slots__ = ("eng", "fn", "dma", "deps", "needs_inc", "sem", "val", "prev_val")

    def __init__(self, eng, fn, dma):
        self.eng = eng
        self.fn = fn
        self.dma = dma
        self.deps = ()
        self.needs_inc = False
        self.sem = None
        self.val = 0
        self.prev_val = 0


ENGS = ("pe", "act", "dve", "pool", "sp")
NDMASEM = 24


class Prog:
    def __init__(self, nc):
        self.nc = nc
        self.ops = {e: [] for e in ENGS}
        self.stack = ExitStack()
        self.nalloc = 0
        self.dma_scan = {}

    ARENA = 176 * 1024

    def sb(self, shape, dt, name=None):
        self.nalloc += 1
        name = name or f"sb{self.nalloc}"
        if not hasattr(self, "arena"):
            self.arena = self.stack.enter_context(
                self.nc.sbuf_tensor("arena", [128, self.ARENA], mybir.dt.uint8)).ap()
            self.off = 0
            self.barrier_deps = {e: None for e in ENGS}
        esz = 2 if dt == BF16 else 4
        n = int(np.prod(shape[1:]))
        nbytes = (n * esz + 63) // 64 * 64
        assert self.off + nbytes <= self.ARENA, f"SBUF arena overflow at {name}: {self.off}+{nbytes}"
        ap = self.arena[0:shape[0], self.off:self.off + n * esz].bitcast(dt)
        self.off += nbytes
        if len(shape) == 3:
            ap = ap.rearrange("p (a b) -> p a b", a=shape[1])
        elif len(shape) == 4:
            ap = ap.rearrange("p (a b c) -> p a b c", a=shape[1], b=shape[2])
        return V([H(name)], ap)

    def raw(self, shape, dt, name):
        t = self.stack.enter_context(self.nc.sbuf_tensor(name, list(shape), dt))
        return V([H(name)], t.ap())

    def mark(self):
        return self.off

    def release(self, mark):
        deps = set()
        for e in ENGS:
            last_c = None
            for op in reversed(self.ops[e]):
                if not op.dma:
                    last_c = op
                    break
            if last_c is not None:
                deps.add(last_c)
            for op in self.ops[e][self.dma_scan.get(e, 0):]:
                if op.dma:
                    deps.add(op)
            self.dma_scan[e] = len(self.ops[e])
        for d in deps:
            d.needs_inc = True
        for e in ENGS:
            prev = self.barrier_deps[e] or set()
            self.barrier_deps[e] = prev | deps
        self.off = mark

    def ps(self, shape, dt, name=None):
        self.nalloc += 1
        name = name or f"ps{self.nalloc}"
        t = self.stack.enter_context(self.nc.psum_tensor(name, list(shape), dt))
        return V([H(name)], t.ap())

    def dram(self, name, shape, dt, kind="Internal"):
        t = self.nc.dram_tensor(name, list(shape), dt, kind=kind)
        return V([H(name)], t.ap())

    def add(self, eng, fn, reads=(), writes=(), dma=False):
        op = Op(eng, fn, dma)
        deps = set()
        for v in reads:
            for h in v.hs:
                w = h.last_w
                if w is not None:
                    deps.add(w)
        for v in writes:
            for h in v.hs:
                w = h.last_w
                if w is not None and (w.dma or dma or w.eng != eng):
                    deps.add(w)
                for e, r in h.rd_eng.items():
                    if dma or e != eng:
                        deps.add(r)
                for r in h.rd_dma:
                    deps.add(r)
        if getattr(self, "barrier_deps", None) and self.barrier_deps[eng]:
            deps |= self.barrier_deps[eng]
            self.barrier_deps[eng] = None
        deps.discard(op)
        op.deps = tuple(deps)
        for d in deps:
            d.needs_inc = True
        for v in reads:
            for h in v.hs:
                if dma:
                    h.rd_dma.append(op)
                else:
                    h.rd_eng[eng] = op
        for v in writes:
            for h in v.hs:
                h.last_w = op
                h.rd_eng = {}
                h.rd_dma = []
        self.ops[eng].append(op)
        return op

    def op(self, eng, meth, **kw):
        reads, writes, real = [], [], {}
        kw.pop("_w", None)
        for k, v in kw.items():
            if isinstance(v, V):
                (writes if (k.startswith("out") or k in ("accum_out", "ap")) else reads).append(v)
                real[k] = v.ap
            else:
                real[k] = v
        return self.add(eng, lambda e: getattr(e, meth)(**real), reads, writes)

    def mm(self, out, lhsT, rhs, start, stop):
        return self.add("pe", lambda e: e.matmul(out.ap, lhsT.ap, rhs.ap, start=start, stop=stop),
                        [lhsT, rhs], [out])

    def tr(self, out, in_, ident):
        return self.add("pe", lambda e: e.transpose(out.ap, in_.ap, ident.ap), [in_, ident], [out])

    def dma(self, out, in_, q="sp"):
        return self.add(q, lambda e: e.dma_start(out=out.ap, in_=in_.ap), [in_], [out], dma=True)

    def emit(self):
        nc = self.nc
        st = self.stack
        esem = {e: st.enter_context(nc.semaphore(f"es_{e}")) for e in ENGS}
        dsem = {e: [st.enter_context(nc.semaphore(f"ds_{e}_{i}")) for i in range(NDMASEM)]
                for e in ("sp", "pool", "act")}
        for e in ENGS:
            cnt = 0
            duse = [0] * NDMASEM
            di = 0
            for op in self.ops[e]:
                if op.dma:
                    k = di % NDMASEM
                    di += 1
                    op.sem = dsem[e][k]
                    op.prev_val = duse[k]
                    duse[k] += 16
                    op.val = duse[k]
                elif op.needs_inc:
                    cnt += 1
                    op.sem = esem[e]
                    op.val = cnt
            if e == "sp":
                self.final_dma = [(dsem[e][k], duse[k]) for k in range(NDMASEM) if duse[k] > 0]
            if e == "pool":
                self.final_dma_pool = [(dsem[e][k], duse[k]) for k in range(NDMASEM) if duse[k] > 0]

        def run(ename, eng):
            waited = {}
            for op in self.ops[ename]:
                need = {}
                for d in op.deps:
                    key = id(d.sem)
                    if need.get(key, (None, -1))[1] < d.val:
                        need[key] = (d.sem, d.val)
                if op.dma and op.prev_val > 0:
                    key = id(op.sem)
                    if need.get(key, (None, -1))[1] < op.prev_val:
                        need[key] = (op.sem, op.prev_val)
                for key, (sem, val) in need.items():
                    if waited.get(key, 0) < val:
                        eng.wait_ge(sem, val)
                        waited[key] = val
                ins = op.fn(eng)
                if op.dma:
                    ins.then_inc(op.sem, 16)
                elif op.needs_inc:
                    ins.then_inc(op.sem, 1)
            if ename == "sp":
                for sem, val in self.final_dma:
                    eng.wait_ge(sem, val)
            if ename == "pool":
                for sem, val in self.final_dma_pool:
                    eng.wait_ge(sem, val)

        with nc.Block() as block:
            @block.tensor
            def _(eng):
                run("pe", eng)

            @block.scalar
            def _(eng):
                run("act", eng)

            @block.vector
            def _(eng):
                run("dve", eng)

            @block.gpsimd
            def _(eng):
                run("pool", eng)

            @block.sync
            def _(eng):
                run("sp", eng)
        st.close()


def bf16(a):
    return np.asarray(a, dtype=np.float32).astype(ml_dtypes.bfloat16)


def make_consts(S):
    pos = np.arange(S)
    a = (pos // 128).astype(np.float32)
    b = (pos % 128).astype(np.float32)
    c = {}
    c["ident"] = bf16(np.eye(128))
    c["kaug"] = bf16(np.stack([128 * a, b, np.ones(S), np.ones(S)]))
    sl_a = 2.0 ** (-8.0 * np.arange(1, 9) / 8)
    sl_b = 2.0 ** (-8.0 * np.arange(1, 5) / 4)
    def qaug(sl):
        return bf16(np.stack([np.stack([8 * s * np.ones(S), 8 * s * np.ones(S), -8 * s * 128 * a, -8 * s * b])
                              for s in sl]))
    c["qaug_a"] = qaug(sl_a)
    c["qaug_b"] = qaug(sl_b)
    i = np.arange(128)
    adm = (i[:, None] // 64) <= (i[None, :] // 64)
    fut = np.maximum(i[:, None] - i[None, :], 0).astype(np.float32)
    c["dfix_a"] = bf16(np.stack([-16 * s * fut for s in sl_a], 1))
    c["dfix_b"] = bf16(np.stack([np.where(adm, -16 * s * fut, -30000.0) for s in sl_b], 1))
    c["dmask"] = np.where(adm.T, 0.0, NEG).astype(np.float32)
    c["posrow"] = np.broadcast_to(pos.astype(np.float32)[None, :], (128, S)).copy()
    c["qpos"] = pos.astype(np.float32).reshape(S // 128, 128).T.copy()
    c["slope8"] = np.broadcast_to((8 * sl_a).astype(np.float32)[None, :], (128, 8)).copy()
    c["pow2"] = np.broadcast_to((2.0 ** -(np.arange(NBIS) + 1.0)).astype(np.float32)[None, :], (128, NBIS)).copy()
    c["ones_bf"] = bf16(np.ones((128, 128)))
    c["ltri"] = bf16(i[:, None] < i[None, :])
    c["iota64"] = np.broadcast_to(np.arange(64, dtype=np.float32)[None, :], (128, 64)).copy()
    c["ones_f"] = np.ones((128, 128), np.float32)
    return c


CONST_SPECS = None


def build_program(S, stop_after=None, nlayers=DEPTH, debug_out=False):
    nc = bass.Bass("TRN2", target_bir_lowering=False)
    P = Prog(nc)
    NT = S // 512
    NS = S // 128
    TOPK = min(256, S // 4)

    def ext(name, shape, dt=F32):
        return P.dram(name, shape, dt, kind="ExternalInput")

    x = ext("x", [S, D])
    mem = ext("mem", [256, D])
    ln_in_g = ext("ln_in_g", [1, D]); ln_in_b = ext("ln_in_b", [1, D])
    w_in = ext("w_in", [DEPTH, D, WIN])
    a_kv_norm = ext("a_kv_norm", [DEPTH, 128])
    a_w_uk = ext("a_w_uk", [DEPTH, 8, 128, 64]); a_w_uv = ext("a_w_uv", [DEPTH, 8, 128, 64])
    b_l = [ext(n, [DEPTH, 64]) for n in ("b_lq1", "b_lk1", "b_lq2", "b_lk2")]
    b_subln = ext("b_subln", [DEPTH, 128])
    w_o = ext("w_o", [DEPTH, D, D])
    ln_g = [ext(f"ln{i}_g", [DEPTH, D]) for i in (1, 2, 3)]
    ln_b = [ext(f"ln{i}_b", [DEPTH, D]) for i in (1, 2, 3)]
    m_wq = ext("m_wq", [DEPTH, D, D]); m_wkv = ext("m_wkv", [DEPTH, D, 2 * D]); m_wo = ext("m_wo", [DEPTH, D, D])
    router_w = ext("router_w", [DEPTH, D, NE]); router_bias = ext("router_bias", [DEPTH, NE])
    need_moe = stop_after is None or stop_after.startswith("p6")
    if need_moe:
        e_w_gate = ext("e_w_gate", [DEPTH, NE, D, 256]); e_w_up = ext("e_w_up", [DEPTH, NE, D, 256])
        e_w_down = ext("e_w_down", [DEPTH, NE, 256, D])
    s_w_gate = ext("s_w_gate", [DEPTH, D, 256]); s_w_up = ext("s_w_up", [DEPTH, D, 256])
    s_w_down = ext("s_w_down", [DEPTH, 256, D])
    cst = make_consts(S)
    cd = {k: ext("c_" + k, list(v.shape), BF16 if v.dtype == ml_dtypes.bfloat16 else F32) for k, v in cst.items()}
    out = P.dram("out", [S, D], F32, kind="ExternalOutput")

    def scratch(name, shape, dt, ntile):
        v = P.dram(name, shape, dt, kind="ExternalOutput" if debug_out else "Internal")
        hs = [H(f"{name}_{i}") for i in range(ntile)]
        return v.ap, hs
    h_tm, h_tm_h = scratch("h_tm", [S, D], F32, NT)
    hT, hT_h = scratch("hT", [D, S], BF16, NT)
    projT, projT_h = scratch("projT", [33, 64, S], BF16, NT)
    kaT, kaT_h = scratch("kaT", [8, 64, S], BF16, NT)
    va, va_h = scratch("va", [S, 512], BF16, NT)
    vb, vb_h = scratch("vb", [S, 512], BF16, NT)
    widx, widx_h = scratch("widx", [S, 8], F32, NT)
    catT, catT_h = scratch("catT", [D, S], BF16, NT)
    h_bf, h_bf_h = scratch("h_bf", [S, D], BF16, NS)
    C_ = S // 4
    Xg = P.dram("Xg", [NE * C_ + 128, D], BF16).ap
    Yg = P.dram("Yg", [NE * C_ + 128, D], BF16).ap
    TRASH = float(NE * C_)
    I32 = mybir.dt.int32
    U32 = mybir.dt.uint32
    off_t = [P.raw([128, 1], I32, f"off_t{i}") for i in range(16)]
    xs_t = [P.raw([128, D], BF16, f"xs_t{i}") for i in range(2)]
    gt_t = [P.raw([128, D], BF16, f"gt_t{i}") for i in range(8)]
    dbg = {}

    ident = P.sb([128, 128], BF16, "ident"); P.dma(ident, cd["ident"])
    ones_bf = P.sb([128, 128], BF16, "ones_bf"); P.dma(ones_bf, cd["ones_bf"])
    ones_f = P.sb([128, 128], F32, "ones_f"); P.dma(ones_f, cd["ones_f"])

    pbank = [P.ps([128, 512], F32, f"pb{i}") for i in range(8)]

    cnt = {"ln": 0}
    eps_ln = P.sb([128, 2], F32, "eps_ln")
    P.op("pool", "memset", ap=eps_ln[:, 0:1], constant=LN_EPS)
    P.op("pool", "memset", ap=eps_ln[:, 1:2], constant=RMS_EPS)
    LB = {}

    def ln_alloc():
        LB["g_bc"] = P.sb([128, D], F32, "g_bc"); LB["b_bc"] = P.sb([128, D], F32, "b_bc")
        LB["z"] = [P.sb([128, D], F32, f"ln_z{i}") for i in range(2)]
        LB["hb"] = [P.sb([128, D], BF16, f"ln_hb{i}") for i in range(2)]
        LB["st"] = [P.sb([128, 8], F32, f"ln_st{i}") for i in range(2)]
        LB["hTs"] = [P.sb([128, 8, 512], BF16, f"hTs{i}") for i in range(2)]

    def load_ln_params(g_ap, b_ap):
        P.dma(LB["g_bc"], V(g_ap.hs, g_ap.ap.partition_broadcast(128)))
        P.dma(LB["b_bc"], V(b_ap.hs, b_ap.ap.partition_broadcast(128)))

    def ln_tile(z, s128, final):
        i = cnt["ln"]; cnt["ln"] += 1
        stt = LB["st"][i % 2]
        hb = LB["hb"][i % 2]
        ln_junk = hb; g_bc = LB["g_bc"]; b_bc = LB["b_bc"]; hTs = LB["hTs"]
        P.op("act", "activation", out=ln_junk, in_=z, func=AF.Identity, accum_out=stt[:, 0:1])
        P.op("act", "activation", out=ln_junk, in_=z, func=AF.Square, accum_out=stt[:, 1:2])
        P.op("dve", "tensor_scalar", out=stt[:, 2:3], in0=stt[:, 0:1], scalar1=1.0 / D, scalar2=None, op0=ALU.mult)
        P.op("dve", "tensor_tensor", out=stt[:, 3:4], in0=stt[:, 2:3], in1=stt[:, 2:3], op=ALU.mult)
        P.op("dve", "scalar_tensor_tensor", out=stt[:, 4:5], in0=stt[:, 1:2], scalar=1.0 / D, in1=stt[:, 3:4],
             op0=ALU.mult, op1=ALU.subtract)
        P.op("act", "activation", out=stt[:, 6:7], in_=stt[:, 4:5], func=AF.Sqrt, bias=eps_ln[:, 0:1])
        P.op("dve", "reciprocal", out=stt[:, 5:6], in_=stt[:, 6:7])
        P.op("dve", "tensor_scalar", out=z, in0=z, scalar1=stt[:, 2:3], scalar2=stt[:, 5:6],
             op0=ALU.subtract, op1=ALU.mult)
        P.op("pool", "tensor_tensor", out=z, in0=z, in1=g_bc, op=ALU.mult)
        P.op("pool", "tensor_tensor", out=z, in0=z, in1=b_bc, op=ALU.add)
        tt = s128 // 4
        if final:
            P.dma(out[s128 * 128:(s128 + 1) * 128, :], z, q="pool")
        else:
            P.dma(V([h_tm_h[tt]], h_tm[s128 * 128:(s128 + 1) * 128, :]), z, q="pool")
        if final:
            return
        P.op("act", "activation", out=hb, in_=z, func=AF.Copy)
        P.dma(V([h_bf_h[s128]], h_bf[s128 * 128:(s128 + 1) * 128, :]), hb, q="pool")
        pst = pbank[7].bc(BF16)
        hs = hTs[tt % 2]
        for k in range(8):
            P.tr(pst[:, k * 128:(k + 1) * 128], hb[:, k * 128:(k + 1) * 128], ident)
        P.op("act", "activation", out=hs[:, :, (s128 % 4) * 128:(s128 % 4 + 1) * 128],
             in_=pst.re("p (k t) -> p k t", k=8), func=AF.Copy)
        if s128 % 4 == 3:
            P.dma(V([hT_h[tt]], hT.rearrange("(k p) s -> p k s", p=128)[:, :, tt * 512:(tt + 1) * 512]), hs, q="pool")

    p0_mark = P.mark()
    ln_alloc()
    load_ln_params(ln_in_g, ln_in_b)
    for s128 in range(NS):
        z = LB["z"][s128 % 2]
        P.dma(z, x[s128 * 128:(s128 + 1) * 128, :])
        ln_tile(z, s128, False)
    P.release(p0_mark)
    if stop_after == "p0":
        return finish(P, nc)

    bank_i = [0]

    rot = [[0, 1, 2, 3, 4, 5, 6]]

    def nb():
        r = rot[0]
        b = pbank[r[bank_i[0] % len(r)]]
        bank_i[0] += 1
        return b

    ev_i = [0]

    def evac(out, in_, act_only=False):
        ev_i[0] += 1
        if ev_i[0] % 2 or act_only:
            P.op("act", "activation", out=out, in_=in_, func=AF.Copy)
        else:
            P.op("dve", "tensor_copy", out=out, in_=in_)

    def wload(dst, src_ap, nk):
        srcv = src_ap.rearrange("(k p) c -> p k c", p=128)
        for k in range(nk):
            P.dma(dst[:, k, :], V([wH], srcv[:, k, :]), q="pool")
    wH = H("weights")

    def residual_ln(s128, ysrc, final):
        z = LB["z"][cnt["ln"] % 2]
        tt = s128 // 4
        P.dma(z, V([h_tm_h[tt]], h_tm[s128 * 128:(s128 + 1) * 128, :]))
        for hf in range(2):
            P.op("dve", "scalar_tensor_tensor", out=z[:, hf * 512:(hf + 1) * 512], in0=z[:, hf * 512:(hf + 1) * 512],
                 scalar=ALPHA, in1=ysrc[hf], op0=ALU.mult, op1=ALU.add)
        ln_tile(z, s128, final)

    base_mark = P.mark()
    OFF = dict(qa=0, ckv=512, qidx=640, kidx=1152, widx=1216, qb=1224, kb=1736, vb=2248)
    gcols = ([OFF["qa"] + 64 * h for h in range(8)] + [OFF["qidx"] + 64 * h for h in range(8)] + [OFF["kidx"]]
             + [OFF["qb"] + 64 * i for i in range(8)] + [OFF["kb"] + 64 * i for i in range(8)])
    projT_gh = [[H(f"projT_{b}_{t}") for t in range(NT)] for b in range(9)]
    kaT_gh = [[H(f"kaT_{b}_{t}") for t in range(NT)] for b in range(2)]
    catT_gh = [[H(f"catT_{b}_{t}") for t in range(NT)] for b in range(6)]

    for l in range(nlayers):
        lam_init = 0.8 - 0.6 * math.exp(-0.3 * l)
        w_in_sb = P.sb([128, 8, WIN], BF16, "w_in_sb"); wload(w_in_sb, w_in.ap[l], 8)
        wuk_sb = P.sb([128, 8, 64], BF16, "wuk_sb")
        P.dma(wuk_sb, V([wH], a_w_uk.ap[l].rearrange("h c d -> c h d")), q="pool")
        wuv_sb = P.sb([128, 8, 64], BF16, "wuv_sb")
        P.dma(wuv_sb, V([wH], a_w_uv.ap[l].rearrange("h c d -> c h d")), q="pool")
        kvn = P.sb([128, 1], F32, "kvn")
        P.dma(kvn, V([wH], a_kv_norm.ap[l:l + 1, :].rearrange("o c -> c o")))
        hTt = [P.sb([128, 8, 512], BF16, f"hTt{i}") for i in range(2)]
        stg64 = [P.sb([64, 4, 512], BF16, f"stg64_{i}") for i in range(3)]
        stg128 = [P.sb([128, 4, 512], BF16, f"stg128_{i}") for i in range(2)]
        stgw = [P.sb([128, 4, 8], F32, f"stgw_{i}") for i in range(2)]
        csq = P.sb([128, 512], F32, "csq"); rs = P.sb([128, 512], F32, "rs")
        cnT = P.sb([128, 512], BF16, "cnT")
        si = 0
        for tt in range(NT):
            ts = slice(tt * 512, (tt + 1) * 512)
            ht = hTt[tt % 2]
            P.dma(ht, V([hT_h[tt]], hT.rearrange("(k p) s -> p k s", p=128)[:, :, ts]))
            for b in range(9):
                gs = list(range(b * 4, min(b * 4 + 4, 33)))
                stg = stg64[si % 3]; si += 1
                for j, g in enumerate(gs):
                    ps = nb()
                    for k in range(8):
                        P.mm(ps[0:64, :], w_in_sb[:, k, gcols[g]:gcols[g] + 64], ht[:, k, :], k == 0, k == 7)
                    evac(stg[:, j, :], ps[0:64, :])
                P.dma(V([projT_gh[b][tt]], projT[gs[0]:gs[-1] + 1, :, ts].rearrange("g p s -> p g s")),
                      stg[:, 0:len(gs), :], q="pool")
            pc = nb()
            for k in range(8):
                P.mm(pc, w_in_sb[:, k, OFF["ckv"]:OFF["ckv"] + 128], ht[:, k, :], k == 0, k == 7)
            P.op("act", "activation", out=csq, in_=pc, func=AF.Square)
            pss = nb()
            P.mm(pss, ones_f, csq, True, True)
            P.op("act", "activation", out=rs, in_=pss, func=AF.Sqrt, scale=1.0 / 128, bias=eps_ln[:, 1:2])
            P.op("dve", "reciprocal", out=rs, in_=rs)
            P.op("dve", "scalar_tensor_tensor", out=cnT, in0=pc, scalar=kvn[:, 0:1], in1=rs, op0=ALU.mult, op1=ALU.mult)
            for b in range(2):
                stg = stg64[si % 3]; si += 1
                for j in range(4):
                    ps = nb()
                    P.mm(ps[0:64, :], wuk_sb[:, b * 4 + j, :], cnT, True, True)
                    evac(stg[:, j, :], ps[0:64, :])
                P.dma(V([kaT_gh[b][tt]], kaT[b * 4:b * 4 + 4, :, ts].rearrange("g p s -> p g s")), stg, q="pool")
            sg = stg128[0]
            for st_ in range(4):
                ps = nb()
                P.mm(ps, cnT[:, st_ * 128:(st_ + 1) * 128], wuv_sb.re("p h d -> p (h d)"), True, True)
                evac(sg[:, st_, :], ps)
            P.dma(V([va_h[tt]], va[ts, :].rearrange("(t p) c -> p t c", p=128)), sg, q="pool")
            sg = stg128[1]; sw = stgw[tt % 2]
            for st_ in range(4):
                ps = nb()
                for k in range(8):
                    P.mm(ps, ht[:, k, st_ * 128:(st_ + 1) * 128], w_in_sb[:, k, OFF["vb"]:OFF["vb"] + 512], k == 0, k == 7)
                evac(sg[:, st_, :], ps)
                ps = nb()
                for k in range(8):
                    P.mm(ps[:, 0:8], ht[:, k, st_ * 128:(st_ + 1) * 128], w_in_sb[:, k, OFF["widx"]:OFF["widx"] + 8], k == 0, k == 7)
                evac(sw[:, st_, :], ps[:, 0:8])
            P.dma(V([vb_h[tt]], vb[ts, :].rearrange("(t p) c -> p t c", p=128)), sg, q="pool")
            P.dma(V([widx_h[tt]], widx[ts, :].rearrange("(t p) c -> p t c", p=128)), sw, q="pool")
        P.release(base_mark)
        if stop_after == f"p1_{l}":
            return finish(P, nc)
        cq = {}
        kidx_sb = P.sb([64, S], BF16, "kidx_sb")
        P.dma(kidx_sb, V(projT_gh[4], projT[16, :, :]))
        ka_ring = [P.sb([69, S], BF16, f"ka{i}") for i in range(2)]
        for kr in ka_ring:
            P.dma(kr[64:65, :], cd["kaug"][2:3, :])
            P.dma(kr[65:69, :], cd["kaug"][0:4, :])
        va_sb = P.sb([128, NS, 8, 65], BF16, "va_sb")
        P.op("pool", "memset", ap=va_sb[:, :, :, 64:65], constant=1.0)
        for t in range(NS):
            P.dma(va_sb[:, t, :, 0:64], V([va_h[t // 4]], va[t * 128:(t + 1) * 128, :].rearrange("p (h d) -> p h d", h=8)))
        widx_sb = P.sb([128, NS, 8], F32, "widx_sb")
        P.dma(widx_sb, V(widx_h, widx.rearrange("(t p) c -> p t c", p=128)))
        isc = P.sb([128, S], F32, "isc")
        junk = P.sb([128, S], BF16, "junkb")
        maskq = [P.sb([128, S], BF16, f"maskq{i}") for i in range(1)]
        rot[0] = [0, 1, 2, 3, 4]
        maskT = P.sb([128, NS, 512], BF16, "maskT")
        posrow = P.sb([128, S], BF16, "posrow"); P.dma(posrow, cd["posrow"], q="pool")
        qpos = P.sb([128, NS], F32, "qpos"); P.dma(qpos, cd["qpos"])
        slope8 = P.sb([128, 8], F32, "slope8"); P.dma(slope8, cd["slope8"])
        pow2 = P.sb([128, NBIS], F32, "pow2"); P.dma(pow2, cd["pow2"])
        dmask = P.sb([128, 128], F32, "dmask"); P.dma(dmask, cd["dmask"])
        dfix_a = P.sb([128, 8, 128], BF16, "dfix_a"); P.dma(dfix_a, cd["dfix_a"])
        zero_c = P.sb([128, 1], F32, "zero_c"); P.op("pool", "memset", ap=zero_c, constant=0.0)
        negc = P.sb([128, 1], F32, "negc"); P.op("pool", "memset", ap=negc, constant=-30000.0)
        qi_ring = [P.sb([64, 8, 512], BF16, f"qi{i}") for i in range(1)]
        q_ring = [P.sb([69, 8, 512], BF16, f"qa{i}") for i in range(1)]
        rl_ring = [P.sb([128, 512], F32, f"rl{i}") for i in range(2)]
        e_ring = [P.sb([128, 512], BF16, f"e{i}") for i in range(4)]
        bst = [P.sb([128, 8], F32, f"bst{i}") for i in range(2)]
        steps = P.sb([128, NBIS], F32, "steps")
        shq = P.sb([128, 8], BF16, "shq")
        shT = P.sb([8, 512], BF16, "shT")
        osb = P.sb([65, 512], F32, "osb")
        oa_stg = [P.sb([64, 4, 512], BF16, f"oa_stg{i}") for i in range(2)]
        ei = 0; hh = 0; sti = 0
        for qt in range(NT):
            ts = slice(qt * 512, (qt + 1) * 512)
            qi = qi_ring[0]; qs = q_ring[0]
            P.dma(qi, V(projT_gh[2] + projT_gh[3], projT[8:16, :, ts].rearrange("g p s -> p g s")))
            P.dma(qs[0:64, :, :], V(projT_gh[0] + projT_gh[1], projT[0:8, :, ts].rearrange("g p s -> p g s")))
            P.dma(qs[65:69, :, :], V(cd["qaug_a"].hs, cd["qaug_a"].ap[:, :, ts].rearrange("h r s -> r h s")))
            for st_ in range(4):
                blk = qt * 4 + st_
                q0 = blk * 128
                nkeys = q0 + 128
                mq = maskq[0]
                b_ = bst[blk % 2]
                for h in range(8):
                    for kc in range((nkeys + 511) // 512):
                        w = min(512, nkeys - kc * 512)
                        ps = nb()
                        P.mm(ps[:, 0:w], qi[:, h, st_ * 128:(st_ + 1) * 128], kidx_sb[:, kc * 512:kc * 512 + w], True, True)
                        rl = rl_ring[ei % 2]; ei += 1
                        P.op("act", "activation", out=rl[:, 0:w], in_=ps[:, 0:w], func=AF.Relu)
                        dst = isc[:, kc * 512:kc * 512 + w]
                        if h == 0:
                            P.op("dve", "tensor_scalar", out=dst, in0=rl[:, 0:w], scalar1=widx_sb[:, blk, 0:1], scalar2=None, op0=ALU.mult)
                        else:
                            P.op("dve", "scalar_tensor_tensor", out=dst, in0=rl[:, 0:w], scalar=widx_sb[:, blk, h:h + 1], in1=dst,
                                 op0=ALU.mult, op1=ALU.add)
                P.op("dve", "tensor_tensor", out=isc[:, q0:q0 + 128], in0=isc[:, q0:q0 + 128], in1=dmask, op=ALU.add)
                tcol = b_[:, 4:5]
                if q0 < TOPK:
                    P.op("dve", "memset", ap=tcol, constant=-1.0e29)
                else:
                    P.op("dve", "tensor_reduce", out=b_[:, 0:1], in_=isc[:, 0:nkeys], axis=AX.X, op=ALU.max)
                    P.op("dve", "tensor_reduce", out=b_[:, 1:2], in_=isc[:, 0:q0], axis=AX.X, op=ALU.min)
                    P.op("dve", "tensor_tensor", out=b_[:, 2:3], in0=b_[:, 0:1], in1=b_[:, 1:2], op=ALU.subtract)
                    P.op("dve", "tensor_tensor", out=b_[:, 3:4], in0=b_[:, 0:1], in1=b_[:, 1:2], op=ALU.add)
                    P.op("dve", "tensor_scalar", out=tcol, in0=b_[:, 3:4], scalar1=0.5, scalar2=None, op0=ALU.mult)
                    P.op("dve", "tensor_scalar", out=steps, in0=pow2, scalar1=b_[:, 2:3], scalar2=None, op0=ALU.mult)
                    for n in range(NBIS):
                        P.op("dve", "tensor_scalar", out=junk[:, 0:nkeys], in0=isc[:, 0:nkeys], scalar1=tcol, scalar2=zero_c[:, 0:1],
                             op0=ALU.is_ge, op1=ALU.add, accum_out=b_[:, 5:6])
                        P.op("dve", "tensor_scalar", out=b_[:, 6:7], in0=b_[:, 5:6], scalar1=TOPK - 0.5, scalar2=0.5,
                             op0=ALU.is_gt, op1=ALU.subtract)
                        P.op("dve", "scalar_tensor_tensor", out=tcol, in0=b_[:, 6:7], scalar=steps[:, n:n + 1], in1=tcol,
                             op0=ALU.mult, op1=ALU.add)
                P.op("dve", "tensor_scalar", out=mq[:, 0:nkeys], in0=isc[:, 0:nkeys], scalar1=tcol, scalar2=None, op0=ALU.is_ge)
                P.op("dve", "tensor_tensor", out=junk[:, 0:nkeys], in0=mq[:, 0:nkeys], in1=posrow[:, 0:nkeys], op=ALU.mult)
                P.op("dve", "tensor_reduce", out=b_[:, 7:8], in_=junk[:, 0:nkeys], axis=AX.X, op=ALU.max)
                P.op("dve", "tensor_tensor", out=b_[:, 3:4], in0=qpos[:, blk:blk + 1], in1=b_[:, 7:8], op=ALU.subtract)
                P.op("dve", "tensor_scalar", out=shq, in0=slope8, scalar1=b_[:, 3:4], scalar2=None, op0=ALU.mult)
                pst = nb().bc(BF16)
                P.tr(pst[0:8, 0:128], shq, ident)
                evac(shT[:, st_ * 128:(st_ + 1) * 128], pst[0:8, 0:128], True)
                for kb0 in range(0, blk + 1, 8):
                    n_ = min(8, blk + 1 - kb0)
                    pst = nb().bc(BF16)
                    for j in range(n_):
                        P.tr(pst[:, j * 128:(j + 1) * 128], mq[:, (kb0 + j) * 128:(kb0 + j + 1) * 128], ident)
                    P.op("act", "activation", out=maskT[:, kb0:kb0 + n_, st_ * 128:(st_ + 1) * 128],
                         in_=pst[:, 0:n_ * 128].re("p (k t) -> p k t", k=n_), func=AF.Identity, scale=30000.0, bias=negc[:, 0:1])
            for h in range(8):
                P.dma(qs[64:65, h, :], shT[h:h + 1, :])
            nkb = 4 * (qt + 1)
            plist = []
            for h in range(8):
                for kb in range(nkb):
                    plist.append((h, kb))
            st8 = {}

            def s_part(h, kb, qt=qt, nkb=nkb, qs=qs, ts=ts):
                if kb == 0:
                    ka = ka_ring[st8["hh"] % 2]; st8["hh"] += 1
                    P.dma(ka[0:64, 0:nkb * 128], V(kaT_gh[h // 4], kaT[h, :, 0:nkb * 128]))
                    st8[("ka", h)] = ka
                ka = st8[("ka", h)]
                c0 = max(0, kb - 4 * qt) * 128
                ps = nb()
                kslc = ka[0:69, kb * 128:(kb + 1) * 128]
                if kb >= 4 * qt:
                    P.mm(ps[:, c0:c0 + 128], kslc, qs[0:69, h, c0:c0 + 128], True, False)
                    P.mm(ps[:, c0:c0 + 128], ident, dfix_a[:, h, :], False, False)
                    P.mm(ps[:, c0:c0 + 128], ident, maskT[:, kb, c0:c0 + 128], False, True)
                    if c0 + 128 < 512:
                        P.mm(ps[:, c0 + 128:512], kslc, qs[0:69, h, c0 + 128:512], True, False)
                        P.mm(ps[:, c0 + 128:512], ident, maskT[:, kb, c0 + 128:512], False, True)
                else:
                    P.mm(ps[:, c0:512], kslc, qs[0:69, h, c0:512], True, False)
                    P.mm(ps[:, c0:512], ident, maskT[:, kb, c0:512], False, True)
                E = e_ring[st8["sti"] % 4]; st8["sti"] += 1
                P.op("act", "activation", out=E[:, c0:512], in_=ps[:, c0:512], func=AF.Exp, scale=0.125)
                st8[("E", h, kb)] = E

            def r_part(h, kb, qt=qt, nkb=nkb, ts=ts):
                c0 = max(0, kb - 4 * qt) * 128
                E = st8.pop(("E", h, kb))
                po = pbank[5 + (h % 2)]
                P.mm(po[0:65, c0:512], va_sb[:, kb, h, :], E[:, c0:512], kb == 0, kb == nkb - 1)
                if kb == nkb - 1:
                    P.op("act", "activation", out=osb, in_=po[0:65, :], func=AF.Copy)
                    P.op("dve", "reciprocal", out=osb[64:65, :], in_=osb[64:65, :])
                    pb_ = pbank[7]
                    P.mm(pb_[0:64, :], ones_f[64:65, 0:64], osb[64:65, :], True, True)
                    og = oa_stg[(h // 4) % 2]
                    P.op("dve", "tensor_tensor", out=og[:, h % 4, :], in0=osb[0:64, :], in1=pb_[0:64, :], op=ALU.mult)
                    if h % 4 == 3:
                        h0 = h - 3
                        P.dma(V([catT_gh[h // 4][qt]], catT[h0 * 64:(h0 + 4) * 64, ts].rearrange("(g p) s -> p g s", p=64)), og, q="pool")
            st8["hh"] = hh; st8["sti"] = sti
            LA = 2
            for i in range(len(plist) + LA):
                if i < len(plist):
                    s_part(*plist[i])
                if i - LA >= 0:
                    r_part(*plist[i - LA])
            hh = st8["hh"]; sti = st8["sti"]
        P.release(base_mark)
        if stop_after == f"p2_{l}":
            return finish(P, nc)
        rot[0] = [0, 1, 2]
        kb_ring = [P.sb([68, S], BF16, f"kbr{i}") for i in range(4)]
        for kr in kb_ring:
            P.dma(kr[64:68, :], cd["kaug"][0:4, :])
        vb_sb = P.sb([128, NS, 512], BF16, "vb_sb")
        P.dma(vb_sb, V(vb_h, vb.rearrange("(t p) c -> p t c", p=128)))
        qb = P.sb([68, 8, 512], BF16, "qb")
        dfix_b = P.sb([128, 4, 128], BF16, "dfix_b"); P.dma(dfix_b, cd["dfix_b"])
        lt = [P.sb([128, 64], F32, f"lt{i}") for i in range(4)]
        for i in range(4):
            P.dma(lt[i], V(b_l[i].hs, b_l[i].ap[l:l + 1, :].partition_broadcast(128)))
        lsc = P.sb([128, 8], F32, "lsc")
        for j in range(2):
            P.op("dve", "tensor_tensor", out=lt[2 * j], in0=lt[2 * j], in1=lt[2 * j + 1], op=ALU.mult)
            P.op("dve", "tensor_reduce", out=lsc[:, j:j + 1], in_=lt[2 * j], axis=AX.X, op=ALU.add)
            P.op("act", "activation", out=lsc[:, 2 + j:3 + j], in_=lsc[:, j:j + 1], func=AF.Exp)
        P.op("dve", "tensor_tensor", out=lsc[:, 4:5], in0=lsc[:, 3:4], in1=lsc[:, 2:3], op=ALU.subtract)
        P.op("dve", "tensor_scalar", out=lsc[:, 5:6], in0=lsc[:, 4:5], scalar1=-lam_init, scalar2=None, op0=ALU.add)
        subc = P.sb([128, 1], F32, "subc")
        P.dma(subc, V([wH], b_subln.ap[l:l + 1, :].rearrange("o c -> c o")))
        P.op("dve", "tensor_scalar", out=subc, in0=subc, scalar1=1.0 - lam_init, scalar2=None, op0=ALU.mult)
        e_ring = [P.sb([128, 512], BF16, f"eb{i}") for i in range(4)]
        r_ = [P.sb([128, 512], F32, f"rr{i}") for i in range(4)]
        ob_stg = [P.sb([128, 512], BF16, f"ob_stg{i}") for i in range(2)]
        ki = 0; sti = 0
        for qt in range(NT):
            ts = slice(qt * 512, (qt + 1) * 512)
            nkb = 4 * (qt + 1)
            P.dma(qb[0:64, :, :], V(projT_gh[4] + projT_gh[5] + projT_gh[6], projT[17:25, :, ts].rearrange("g p s -> p g s")))
            for g in range(8):
                P.dma(qb[64:68, g, :], V(cd["qaug_b"].hs, cd["qaug_b"].ap[g // 2, :, ts]))
            steps = []
            for h in range(4):
                for kb in range(nkb):
                    for j in range(2):
                        steps.append((h, kb, j))
            st8 = {"ki": ki, "sti": sti}

            def s_part(h, kb, j, qt=qt, nkb=nkb):
                if kb == 0:
                    K_ = kb_ring[st8["ki"] % 4]; st8["ki"] += 1
                    P.dma(K_[0:64, 0:nkb * 128], V(projT_gh[6] + projT_gh[7] + projT_gh[8], projT[25 + 2 * h + j, :, 0:nkb * 128]))
                    st8[("K", h, j)] = K_
                K_ = st8[("K", h, j)]
                c0 = max(0, kb - 4 * qt) * 128
                ps = nb()
                kslc = K_[0:68, kb * 128:(kb + 1) * 128]
                if kb >= 4 * qt:
                    P.mm(ps[:, c0:c0 + 128], kslc, qb[0:68, 2 * h + j, c0:c0 + 128], True, False)
                    P.mm(ps[:, c0:c0 + 128], ident, dfix_b[:, h, :], False, True)
                    if c0 + 128 < 512:
                        P.mm(ps[:, c0 + 128:512], kslc, qb[0:68, 2 * h + j, c0 + 128:512], True, True)
                else:
                    P.mm(ps[:, c0:512], kslc, qb[0:68, 2 * h + j, c0:512], True, True)
                E = e_ring[st8["sti"] % 4]; st8["sti"] += 1
                P.op("act", "activation", out=E[:, c0:512], in_=ps[:, c0:512], func=AF.Exp, scale=0.125)
                st8[("E", h, kb, j)] = E

            def r_part(h, kb, j, qt=qt, nkb=nkb, ts=ts):
                c0 = max(0, kb - 4 * qt) * 128
                E = st8.pop(("E", h, kb, j))
                O_ = [pbank[3], pbank[4]]; Dn = [pbank[5], pbank[6]]
                P.mm(O_[j][:, c0:512], vb_sb[:, kb, h * 128:(h + 1) * 128], E[:, c0:512], kb == 0, kb == nkb - 1)
                P.mm(Dn[j][:, c0:512], ones_bf, E[:, c0:512], kb == 0, kb == nkb - 1)
                if kb == nkb - 1 and j == 1:
                    for jj in range(2):
                        P.op("dve", "reciprocal", out=r_[jj], in_=Dn[jj])
                        P.op("dve", "tensor_tensor", out=r_[jj], in0=O_[jj], in1=r_[jj], op=ALU.mult)
                    P.op("dve", "scalar_tensor_tensor", out=r_[2], in0=r_[1], scalar=lsc[:, 5:6], in1=r_[0], op0=ALU.mult, op1=ALU.add)
                    P.op("act", "activation", out=r_[3], in_=r_[2], func=AF.Square)
                    pss = pbank[7]
                    P.mm(pss, ones_f, r_[3], True, True)
                    P.op("act", "activation", out=r_[3], in_=pss, func=AF.Sqrt, scale=1.0 / 128, bias=eps_ln[:, 1:2])
                    P.op("dve", "reciprocal", out=r_[3], in_=r_[3])
                    og = ob_stg[h % 2]
                    P.op("dve", "scalar_tensor_tensor", out=og, in0=r_[2], scalar=subc[:, 0:1], in1=r_[3], op0=ALU.mult, op1=ALU.mult)
                    P.dma(V([catT_gh[2 + h][qt]], catT[512 + h * 128:512 + (h + 1) * 128, ts]), og, q="pool")
            LA = 2
            for i in range(len(steps) + LA):
                if i < len(steps):
                    s_part(*steps[i])
                if i - LA >= 0:
                    r_part(*steps[i - LA])
            ki = st8["ki"]; sti = st8["sti"]
        P.release(base_mark)
        if stop_after == f"p3_{l}":
            return finish(P, nc)
        rot[0] = [0, 1, 2, 3, 4, 5, 6]
        ln_alloc()
        load_ln_params(V(ln_g[0].hs, ln_g[0].ap[l:l + 1, :]), V(ln_b[0].hs, ln_b[0].ap[l:l + 1, :]))
        wo_sb = P.sb([128, 8, D], BF16, "wo_sb"); wload(wo_sb, w_o.ap[l], 8)
        ct_ring = [P.sb([128, 8, 512], BF16, f"ct{i}") for i in range(2)]
        for tt in range(NT):
            ts = slice(tt * 512, (tt + 1) * 512)
            ct = ct_ring[tt % 2]
            P.dma(ct, V([g_[tt] for g_ in catT_gh], catT.rearrange("(k p) s -> p k s", p=128)[:, :, ts]))
            for st_ in range(4):
                ys = [nb(), nb()]
                for hf in range(2):
                    for k in range(8):
                        P.mm(ys[hf], ct[:, k, st_ * 128:(st_ + 1) * 128], wo_sb[:, k, hf * 512:(hf + 1) * 512], k == 0, k == 7)
                residual_ln(tt * 4 + st_, ys, False)
        P.release(base_mark)
        if stop_after == f"p4_{l}":
            return finish(P, nc)
        rot[0] = [0, 1, 2, 3, 4]
        ln_alloc()
        load_ln_params(V(ln_g[1].hs, ln_g[1].ap[l:l + 1, :]), V(ln_b[1].hs, ln_b[1].ap[l:l + 1, :]))
        wq_sb = P.sb([128, 8, D], BF16, "wq_sb"); wload(wq_sb, m_wq.ap[l], 8)
        wkv_sb = P.sb([128, 8, 2 * D], BF16, "wkv_sb"); wload(wkv_sb, m_wkv.ap[l], 8)
        wo2_sb = P.sb([128, 8, D], BF16, "wo2_sb"); wload(wo2_sb, m_wo.ap[l], 8)
        mem_b = P.sb([128, 2, D], BF16, "mem_b")
        P.dma(mem_b, V(mem.hs, mem.ap.rearrange("(t p) d -> p t d", p=128)), q="pool")
        memT = P.sb([128, 8, 256], BF16, "memT")
        for t in range(2):
            pst = nb().bc(BF16)
            for k in range(8):
                P.tr(pst[:, k * 128:(k + 1) * 128], mem_b[:, t, k * 128:(k + 1) * 128], ident)
            evac(memT[:, :, t * 128:(t + 1) * 128], pst.re("p (k t) -> p k t", k=8))
        KT = P.sb([128, 8, 256], BF16, "KT")
        for dc in range(8):
            ps = nb()
            for k in range(8):
                P.mm(ps[:, 0:256], wkv_sb[:, k, dc * 128:(dc + 1) * 128], memT[:, k, :], k == 0, k == 7)
            evac(KT[:, dc, :], ps[:, 0:256])
        Vm = P.sb([128, 2, D], BF16, "Vm")
        for t in range(2):
            for hf in range(2):
                ps = nb()
                for k in range(8):
                    P.mm(ps, memT[:, k, t * 128:(t + 1) * 128], wkv_sb[:, k, D + hf * 512:D + (hf + 1) * 512], k == 0, k == 7)
                evac(Vm[:, t, hf * 512:(hf + 1) * 512], ps)
        ht_ring = [P.sb([128, 8, 512], BF16, f"ht5_{i}") for i in range(2)]
        QT = P.sb([128, 8, 512], BF16, "QT"); OT = P.sb([128, 8, 512], BF16, "OT")
        e5 = [P.sb([128, 2, 512], BF16, f"e5_{i}") for i in range(2)]
        rd = P.sb([128, 512], F32, "rd5")
        for tt in range(NT):
            ts = slice(tt * 512, (tt + 1) * 512)
            ht = ht_ring[tt % 2]
            P.dma(ht, V([hT_h[tt]], hT.rearrange("(k p) s -> p k s", p=128)[:, :, ts]))
            for dc in range(8):
                ps = nb()
                for k in range(8):
                    P.mm(ps, wq_sb[:, k, dc * 128:(dc + 1) * 128], ht[:, k, :], k == 0, k == 7)
                evac(QT[:, dc, :], ps)
            for h in range(4):
                E = e5[h % 2]
                for mb in range(2):
                    ps = nb()
                    for dc in range(2):
                        P.mm(ps, KT[:, 2 * h + dc, mb * 128:(mb + 1) * 128], QT[:, 2 * h + dc, :], dc == 0, dc == 1)
                    P.op("act", "activation", out=E[:, mb, :], in_=ps, func=AF.Exp, scale=1.0 / 16)
                den = pbank[5]
                for mb in range(2):
                    P.mm(den, ones_bf, E[:, mb, :], mb == 0, mb == 1)
                P.op("dve", "reciprocal", out=rd, in_=den)
                for dvc in range(2):
                    po = pbank[6] if dvc == 0 else nb()
                    for mb in range(2):
                        P.mm(po, Vm[:, mb, h * 256 + dvc * 128:h * 256 + (dvc + 1) * 128], E[:, mb, :], mb == 0, mb == 1)
                    P.op("dve", "tensor_tensor", out=OT[:, 2 * h + dvc, :], in0=po, in1=rd, op=ALU.mult)
            for st_ in range(4):
                ys = [nb(), nb()]
                for hf in range(2):
                    for k in range(8):
                        P.mm(ys[hf], OT[:, k, st_ * 128:(st_ + 1) * 128], wo2_sb[:, k, hf * 512:(hf + 1) * 512], k == 0, k == 7)
                residual_ln(tt * 4 + st_, ys, False)
        P.release(base_mark)
        if stop_after == f"p5_{l}":
            return finish(P, nc)
        rot[0] = [0, 1, 2, 3, 4, 5, 6]
        last = (l == nlayers - 1)
        ln_alloc()
        load_ln_params(V(ln_g[2].hs, ln_g[2].ap[l:l + 1, :]), V(ln_b[2].hs, ln_b[2].ap[l:l + 1, :]))
        NCS = C_ // 128
        WS = min(512, C_)
        rw_sb = P.sb([128, 8, NE], BF16, "rw_sb"); wload(rw_sb, router_w.ap[l], 8)
        rbias = P.sb([128, NE], F32, "rbias")
        P.dma(rbias, V(router_bias.hs, router_bias.ap[l:l + 1, :].partition_broadcast(128)))
        iota64 = P.sb([128, NE], F32, "iota64"); P.dma(iota64, cd["iota64"])
        ltri = P.sb([128, 128], BF16, "ltri"); P.dma(ltri, cd["ltri"])
        M_bf = P.sb([128, NS, NE], BF16, "M_bf")
        gk = P.sb([128, NS, 8], F32, "gk")
        offf = P.sb([128, NS, 8], F32, "offf")
        rt = [P.sb([128, NE], F32, f"rt{i}") for i in range(6)]
        rsm = P.sb([128, 16], F32, "rsm")
        idx8 = P.sb([128, 8], U32, "idx8"); idxf = P.sb([128, 8], F32, "idxf"); slotf = P.sb([128, 8], F32, "slotf")
        ovf = P.sb([128, 8], F32, "ovf")
        ht_ring = [P.sb([128, 8, 512], BF16, f"ht6_{i}") for i in range(2)]
        Xg_hs = []
        Yg_hs = [[H(f"Yg_{e}_{t}") for t in range(NCS)] for e in range(NE)]
        ztile = P.sb([128, D], BF16, "ztile")
        P.op("pool", "memset", ap=ztile, constant=0.0)
        yg_trash_h = H("Yg_trash")
        P.dma(V([yg_trash_h], Yg[NE * C_:NE * C_ + 128, :]), ztile, q="pool")
        oi = 0
        for tt in range(NT):
            ht = ht_ring[tt % 2]
            P.dma(ht, V([hT_h[tt]], hT.rearrange("(k p) s -> p k s", p=128)[:, :, tt * 512:(tt + 1) * 512]))
            for st_ in range(4):
                s_ = tt * 4 + st_
                ps = nb()
                for k in range(8):
                    P.mm(ps[:, 0:NE], ht[:, k, st_ * 128:(st_ + 1) * 128], rw_sb[:, k, :], k == 0, k == 7)
                P.op("act", "activation", out=rt[0], in_=ps[:, 0:NE], func=AF.Sigmoid)
                P.op("dve", "tensor_tensor", out=rt[1], in0=rt[0], in1=rbias, op=ALU.add)
                P.op("dve", "max", out=rsm[:, 0:8], in_=rt[1])
                P.op("dve", "max_index", out=idx8, in_max=rsm[:, 0:8], in_values=rt[1])
                P.op("dve", "tensor_copy", out=idxf, in_=idx8)
                P.op("dve", "tensor_scalar", out=rt[2], in0=rt[1], scalar1=rsm[:, 7:8], scalar2=None, op0=ALU.is_ge)
                P.op("dve", "tensor_copy", out=M_bf[:, s_, :], in_=rt[2])
                P.op("dve", "tensor_tensor", out=rt[2], in0=rt[2], in1=rt[0], op=ALU.mult)
                P.op("dve", "tensor_reduce", out=rsm[:, 8:9], in_=rt[2], axis=AX.X, op=ALU.add)
                P.op("dve", "reciprocal", out=rsm[:, 9:10], in_=rsm[:, 8:9])
                P.op("dve", "tensor_scalar", out=rt[2], in0=rt[2], scalar1=rsm[:, 9:10], scalar2=None, op0=ALU.mult)
                P.op("dve", "tensor_scalar", out=rt[3], in0=rt[2], scalar1=2.5, scalar2=None, op0=ALU.mult)
                pc = nb()
                P.mm(pc[:, 0:NE], ltri, M_bf[:, s_, :], True, s_ == 0)
                for j in range(s_):
                    P.mm(pc[:, 0:NE], ones_bf, M_bf[:, j, :], False, j == s_ - 1)
                P.op("act", "activation", out=rt[4], in_=pc[:, 0:NE], func=AF.Copy)
                for k in range(8):
                    P.op("dve", "tensor_scalar", out=rt[5], in0=iota64, scalar1=idxf[:, k:k + 1], scalar2=None, op0=ALU.is_equal)
                    P.op("dve", "tensor_tensor", out=rt[1], in0=rt[5], in1=rt[4], op=ALU.mult)
                    P.op("dve", "tensor_reduce", out=slotf[:, k:k + 1], in_=rt[1], axis=AX.X, op=ALU.add)
                    P.op("dve", "tensor_tensor", out=rt[1], in0=rt[5], in1=rt[3], op=ALU.mult)
                    P.op("dve", "tensor_reduce", out=gk[:, s_, k:k + 1], in_=rt[1], axis=AX.X, op=ALU.add)
                P.op("dve", "tensor_scalar", out=ovf, in0=slotf, scalar1=C_ - 0.5, scalar2=None, op0=ALU.is_lt)
                P.op("dve", "tensor_tensor", out=gk[:, s_, :], in0=gk[:, s_, :], in1=ovf, op=ALU.mult)
                P.op("dve", "scalar_tensor_tensor", out=offf[:, s_, :], in0=idxf, scalar=float(C_), in1=slotf, op0=ALU.mult, op1=ALU.add)
                P.op("dve", "tensor_scalar", out=offf[:, s_, :], in0=offf[:, s_, :], scalar1=-TRASH, scalar2=None, op0=ALU.add)
                P.op("dve", "tensor_tensor", out=offf[:, s_, :], in0=offf[:, s_, :], in1=ovf, op=ALU.mult)
                P.op("dve", "tensor_scalar", out=offf[:, s_, :], in0=offf[:, s_, :], scalar1=TRASH, scalar2=None, op0=ALU.add)
                xs = xs_t[s_ % 2]
                P.dma(xs, V([h_bf_h[s_]], h_bf[s_ * 128:(s_ + 1) * 128, :]))
                for k in range(8):
                    ot = off_t[oi % 16]; oi += 1
                    P.op("dve", "tensor_copy", out=ot, in_=offf[:, s_, k:k + 1])
                    hx = H(f"Xg_{s_}_{k}"); Xg_hs.append(hx)
                    P.add("pool", (lambda e, ot=ot, xs=xs: e.indirect_dma_start(
                        out=Xg[:, :], out_offset=bass.IndirectOffsetOnAxis(ap=ot.ap[:, 0:1], axis=0),
                        in_=xs.ap[:, :], in_offset=None, bounds_check=None)),
                        [xs, ot], [V([hx], Xg)], dma=True)
        wg_r = [P.sb([128, 8, 256], BF16, f"wg{i}") for i in range(2)]
        wu_r = [P.sb([128, 8, 256], BF16, f"wu{i}") for i in range(2)]
        wd_r = [P.sb([128, 2, D], BF16, f"wd{i}") for i in range(2)]
        sg_r = [P.sb([128, 512], F32, f"sg{i}") for i in range(2)]
        at_r = [P.sb([128, 2, 512], BF16, f"at{i}") for i in range(2)]
        stageb_mark = P.mark()
        xe_r = [P.sb([128, NCS, D], BF16, f"xe{i}") for i in range(2)]
        xT_r = [P.sb([128, 8, C_], BF16, f"xT{i}") for i in range(2)]
        ysb_r = [P.sb([128, D], BF16, f"ysb{i}") for i in range(3)]
        yi = 0

        def expert_block(wg, wu, wd, rhs_of, n_sub, emit_out, ai0):
            ai = ai0
            c0 = 0
            while c0 < n_sub * 128:
                w = min(512, n_sub * 128 - c0)
                gps = [pbank[0 + 2 * (ai % 2)], pbank[1 + 2 * (ai % 2)]]
                ups = [pbank[4], pbank[5]]
                for m in range(2):
                    for k in range(8):
                        P.mm(gps[m][:, 0:w], wg[:, k, m * 128:(m + 1) * 128], rhs_of(k, c0, w), k == 0, k == 7)
                for m in range(2):
                    for k in range(8):
                        P.mm(ups[m][:, 0:w], wu[:, k, m * 128:(m + 1) * 128], rhs_of(k, c0, w), k == 0, k == 7)
                at = at_r[ai % 2]
                for m in range(2):
                    sg = sg_r[m]
                    P.op("act", "activation", out=sg[:, 0:w], in_=gps[m][:, 0:w], func=AF.Silu)
                    P.op("dve", "tensor_tensor", out=at[:, m, 0:w], in0=sg[:, 0:w], in1=ups[m][:, 0:w], op=ALU.mult)
                for st_ in range(w // 128):
                    pys = [pbank[6], pbank[7]]
                    for hf in range(2):
                        for m in range(2):
                            P.mm(pys[hf], at[:, m, st_ * 128:(st_ + 1) * 128], wd[:, m, hf * 512:(hf + 1) * 512], m == 0, m == 1)
                    emit_out(c0 // 128 + st_, pys)
                ai += 1
                c0 += w
            return ai

        ai = 0
        for e in range(NE):
            wg = wg_r[e % 2]; wu = wu_r[e % 2]; wd = wd_r[e % 2]
            wload(wg, e_w_gate.ap[l, e], 8); wload(wu, e_w_up.ap[l, e], 8); wload(wd, e_w_down.ap[l, e], 2)
            xe = xe_r[e % 2]; xT = xT_r[e % 2]
            P.dma(xe, V(Xg_hs, Xg[e * C_:(e + 1) * C_, :].rearrange("(t p) d -> p t d", p=128)))
            for t in range(NCS):
                pst = nb().bc(BF16)
                for k in range(8):
                    P.tr(pst[:, k * 128:(k + 1) * 128], xe[:, t, k * 128:(k + 1) * 128], ident)
                evac(xT[:, :, t * 128:(t + 1) * 128], pst.re("p (k t) -> p k t", k=8))

            def emit_out(sub, pys, e=e):
                nonlocal yi
                ysb = ysb_r[yi % 3]; yi += 1
                P.op("act", "activation", out=ysb[:, 0:512], in_=pys[0], func=AF.Copy)
                P.op("dve", "tensor_copy", out=ysb[:, 512:1024], in_=pys[1])
                P.dma(V([Yg_hs[e][sub]], Yg[e * C_ + sub * 128:e * C_ + (sub + 1) * 128, :]), ysb, q="pool")
            ai = expert_block(wg, wu, wd, lambda k, c0, w, xT=xT: xT[:, k, c0:c0 + w], NCS, emit_out, ai)
        P.release(stageb_mark)
        wg = wg_r[0]; wu = wu_r[0]; wd = wd_r[0]
        wload(wg, s_w_gate.ap[l], 8); wload(wu, s_w_up.ap[l], 8); wload(wd, s_w_down.ap[l], 2)
        yacc_r = [P.sb([128, D], F32, f"yacc{i}") for i in range(3)]
        allY = [h_ for row in Yg_hs for h_ in row] + [yg_trash_h]
        gi = 0
        for tt in range(NT):
            ht = ht_ring[tt % 2]
            P.dma(ht, V([hT_h[tt]], hT.rearrange("(k p) s -> p k s", p=128)[:, :, tt * 512:(tt + 1) * 512]))

            def emit_out(sub, pys, tt=tt):
                nonlocal gi, oi, yi
                s_ = tt * 4 + sub
                ya = yacc_r[yi % 3]; yi += 1
                P.op("act", "activation", out=ya[:, 0:512], in_=pys[0], func=AF.Copy)
                P.op("act", "activation", out=ya[:, 512:1024], in_=pys[1], func=AF.Copy)
                for k in range(8):
                    ot = off_t[oi % 16]; oi += 1
                    P.op("dve", "tensor_copy", out=ot, in_=offf[:, s_, k:k + 1])
                    gt = gt_t[gi % 8]; gi += 1
                    P.add("pool", (lambda e, ot=ot, gt=gt: e.indirect_dma_start(
                        out=gt.ap[:, :], out_offset=None, in_=Yg[:, :],
                        in_offset=bass.IndirectOffsetOnAxis(ap=ot.ap[:, 0:1], axis=0), bounds_check=None)),
                        [V(allY, Yg), ot], [gt], dma=True)
                    P.op("dve", "scalar_tensor_tensor", out=ya, in0=gt, scalar=gk[:, s_, k:k + 1], in1=ya, op0=ALU.mult, op1=ALU.add)
                residual_ln(s_, [ya[:, 0:512], ya[:, 512:1024]], last)
            ai = expert_block(wg, wu, wd, lambda k, c0, w, ht=ht: ht[:, k, c0:c0 + w], 4, emit_out, ai)
        P.release(base_mark)
        if stop_after == f"p6_{l}":
            return finish(P, nc)
    return finish(P, nc)


def finish(P, nc):
    P.emit()
    return nc


def core_inputs(inp, core, S, need_moe=True):
    m = {}
    m["x"] = np.ascontiguousarray(inp["x"][core])
    m["mem"] = np.ascontiguousarray(inp["mem"][core])
    for k, v in inp.items():
        if k in ("x", "mem") or (not need_moe and k.startswith("e_w_")):
            continue
        if k in ("ln_in_g", "ln_in_b"):
            v = v.reshape(1, -1)
        m[k] = np.ascontiguousarray(v)
    for k, v in make_consts(S).items():
        m["c_" + k] = v
    return m


_CACHE = {}


def kernel(**inputs):
    S = inputs["x"].shape[1]
    nb = inputs["x"].shape[0]
    inp = {k: np.asarray(v) for k, v in inputs.items()}
    if S not in _CACHE:
        _CACHE[S] = build_program(S)
    nc = _CACHE[S]
    in_maps = [core_inputs(inp, c, S) for c in range(nb)]
    res = run_bass_kernel_spmd(nc, in_maps, core_ids=list(range(nb)))
    return np.stack([np.asarray(r["out"]) for r in res.results], axis=0).astype(np.float32)
```

```python
import math
from contextlib import ExitStack

import numpy as np
import ml_dtypes

import concourse.bass as bass
import concourse.mybir as mybir
from concourse.bass_utils import run_bass_kernel_spmd

F32 = mybir.dt.float32
BF16 = mybir.dt.bfloat16
ALU = mybir.AluOpType
AF = mybir.ActivationFunctionType
AX = mybir.AxisListType

D = 1024
DEPTH = 2
NE = 64
ALPHA = (2 * DEPTH) ** 0.25
LN_EPS = 1e-5
RMS_EPS = 1e-6
WIN = 2760
NBIS = 14
NEG = -1.0e30


class H:
    __slots__ = ("name", "last_w", "rd_eng", "rd_dma")

    def __init__(self, name=""):
        self.name = name
        self.last_w = None
        self.rd_eng = {}
        self.rd_dma = []


class V:
    def __init__(self, hs, ap):
        self.hs = hs
        self.ap = ap

    def __getitem__(self, k):
        return V(self.hs, self.ap[k])

    def re(self, s, **kw):
        return V(self.hs, self.ap.rearrange(s, **kw))

    def bc(self, dt):
        return V(self.hs, self.ap.bitcast(dt))


class Op:
    __slots__ = ("eng", "fn", "dma", "deps", "needs_inc", "sem", "val", "prev_val")

    def __init__(self, eng, fn, dma):
        self.eng = eng
        self.fn = fn
        self.dma = dma
        self.deps = ()
        self.needs_inc = False
        self.sem = None
        self.val = 0
        self.prev_val = 0


ENGS = ("pe", "act", "dve", "pool", "sp")
NDMASEM = 24


class Prog:
    def __init__(self, nc):
        self.nc = nc
        self.ops = {e: [] for e in ENGS}
        self.stack = ExitStack()
        self.nalloc = 0
        self.dma_scan = {}

    ARENA = 176 * 1024

    def sb(self, shape, dt, name=None):
        self.nalloc += 1
        name = name or f"sb{self.nalloc}"
        if not hasattr(self, "arena"):
            self.arena = self.stack.enter_context(
                self.nc.sbuf_tensor("arena", [128, self.ARENA], mybir.dt.uint8)).ap()
            self.off = 0
            self.barrier_deps = {e: None for e in ENGS}
        esz = 2 if dt == BF16 else 4
        n = int(np.prod(shape[1:]))
        nbytes = (n * esz + 63) // 64 * 64
        assert self.off + nbytes <= self.ARENA, f"SBUF arena overflow at {name}: {self.off}+{nbytes}"
        ap = self.arena[0:shape[0], self.off:self.off + n * esz].bitcast(dt)
        self.off += nbytes
        if len(shape) == 3:
            ap = ap.rearrange("p (a b) -> p a b", a=shape[1])
        elif len(shape) == 4:
            ap = ap.rearrange("p (a b c) -> p a b c", a=shape[1], b=shape[2])
        return V([H(name)], ap)

    def raw(self, shape, dt, name):
        t = self.stack.enter_context(self.nc.sbuf_tensor(name, list(shape), dt))
        return V([H(name)], t.ap())

    def mark(self):
        return self.off

    def release(self, mark):
        deps = set()
        for e in ENGS:
            last_c = None
            for op in reversed(self.ops[e]):
                if not op.dma:
                    last_c = op
                    break
            if last_c is not None:
                deps.add(last_c)
            for op in self.ops[e][self.dma_scan.get(e, 0):]:
                if op.dma:
                    deps.add(op)
            self.dma_scan[e] = len(self.ops[e])
        for d in deps:
            d.needs_inc = True
        for e in ENGS:
            prev = self.barrier_deps[e] or set()
            self.barrier_deps[e] = prev | deps
        self.off = mark

    def ps(self, shape, dt, name=None):
        self.nalloc += 1
        name = name or f"ps{self.nalloc}"
        t = self.stack.enter_context(self.nc.psum_tensor(name, list(shape), dt))
        return V([H(name)], t.ap())

    def dram(self, name, shape, dt, kind="Internal"):
        t = self.nc.dram_tensor(name, list(shape), dt, kind=kind)
        return V([H(name)], t.ap())

    def add(self, eng, fn, reads=(), writes=(), dma=False):
        op = Op(eng, fn, dma)
        deps = set()
        for v in reads:
            for h in v.hs:
                w = h.last_w
                if w is not None:
                    deps.add(w)
        for v in writes:
            for h in v.hs:
                w = h.last_w
                if w is not None and (w.dma or dma or w.eng != eng):
                    deps.add(w)
                for e, r in h.rd_eng.items():
                    if dma or e != eng:
                        deps.add(r)
                for r in h.rd_dma:
                    deps.add(r)
        if getattr(self, "barrier_deps", None) and self.barrier_deps[eng]:
            deps |= self.barrier_deps[eng]
            self.barrier_deps[eng] = None
        deps.discard(op)
        op.deps = tuple(deps)
        for d in deps:
            d.needs_inc = True
        for v in reads:
            for h in v.hs:
                if dma:
                    h.rd_dma.append(op)
                else:
                    h.rd_eng[eng] = op
        for v in writes:
            for h in v.hs:
                h.last_w = op
                h.rd_eng = {}
                h.rd_dma = []
        self.ops[eng].append(op)
        return op

    def op(self, eng, meth, **kw):
        reads, writes, real = [], [], {}
        kw.pop("_w", None)
        for k, v in kw.items():
            if isinstance(v, V):
                (writes if (k.startswith("out") or k in ("accum_out", "ap")) else reads).append(v)
                real[k] = v.ap
            else:
                real[k] = v
        return self.add(eng, lambda e: getattr(e, meth)(**real), reads, writes)

    def mm(self, out, lhsT, rhs, start, stop):
        return self.add("pe", lambda e: e.matmul(out.ap, lhsT.ap, rhs.ap, start=start, stop=stop),
                        [lhsT, rhs], [out])

    def tr(self, out, in_, ident):
        return self.add("pe", lambda e: e.transpose(out.ap, in_.ap, ident.ap), [in_, ident], [out])

    def dma(self, out, in_, q="sp"):
        return self.add(q, lambda e: e.dma_start(out=out.ap, in_=in_.ap), [in_], [out], dma=True)

    def emit(self):
        nc = self.nc
        st = self.stack
        esem = {e: st.enter_context(nc.semaphore(f"es_{e}")) for e in ENGS}
        dsem = {e: [st.enter_context(nc.semaphore(f"ds_{e}_{i}")) for i in range(NDMASEM)]
                for e in ("sp", "pool", "act")}
        for e in ENGS:
            cnt = 0
            duse = [0] * NDMASEM
            di = 0
            for op in self.ops[e]:
                if op.dma:
                    k = di % NDMASEM
                    di += 1
                    op.sem = dsem[e][k]
                    op.prev_val = duse[k]
                    duse[k] += 16
                    op.val = duse[k]
                elif op.needs_inc:
                    cnt += 1
                    op.sem = esem[e]
                    op.val = cnt
            if e == "sp":
                self.final_dma = [(dsem[e][k], duse[k]) for k in range(NDMASEM) if duse[k] > 0]
            if e == "pool":
                self.final_dma_pool = [(dsem[e][k], duse[k]) for k in range(NDMASEM) if duse[k] > 0]

        def run(ename, eng):
            waited = {}
            for op in self.ops[ename]:
                need = {}
                for d in op.deps:
                    key = id(d.sem)
                    if need.get(key, (None, -1))[1] < d.val:
                        need[key] = (d.sem, d.val)
                if op.dma and op.prev_val > 0:
                    key = id(op.sem)
                    if need.get(key, (None, -1))[1] < op.prev_val:
                        need[key] = (op.sem, op.prev_val)
                for key, (sem, val) in need.items():
                    if waited.get(key, 0) < val:
                        eng.wait_ge(sem, val)
                        waited[key] = val
                ins = op.fn(eng)
                if op.dma:
                    ins.then_inc(op.sem, 16)
                elif op.needs_inc:
                    ins.then_inc(op.sem, 1)
            if ename == "sp":
                for sem, val in self.final_dma:
                    eng.wait_ge(sem, val)
            if ename == "pool":
                for sem, val in self.final_dma_pool:
                    eng.wait_ge(sem, val)

        with nc.Block() as block:
            @block.tensor
            def _(eng):
                run("pe", eng)

            @block.scalar
            def _(eng):
                run("act", eng)

            @block.vector
            def _(eng):
                run("dve", eng)

            @block.gpsimd
            def _(eng):
                run("pool", eng)

            @block.sync
            def _(eng):
                run("sp", eng)
        st.close()


def bf16(a):
    return np.asarray(a, dtype=np.float32).astype(ml_dtypes.bfloat16)


def make_consts(S):
    pos = np.arange(S)
    a = (pos // 128).astype(np.float32)
    b = (pos % 128).astype(np.float32)
    c = {}
    c["ident"] = bf16(np.eye(128))
    c["kaug"] = bf16(np.stack([128 * a, b, np.ones(S), np.ones(S)]))
    sl_a = 2.0 ** (-8.0 * np.arange(1, 9) / 8)
    sl_b = 2.0 ** (-8.0 * np.arange(1, 5) / 4)
    def qaug(sl):
        return bf16(np.stack([np.stack([8 * s * np.ones(S), 8 * s * np.ones(S), -8 * s * 128 * a, -8 * s * b])
                              for s in sl]))
    c["qaug_a"] = qaug(sl_a)
    c["qaug_b"] = qaug(sl_b)
    i = np.arange(128)
    adm = (i[:, None] // 64) <= (i[None, :] // 64)
    fut = np.maximum(i[:, None] - i[None, :], 0).astype(np.float32)
    c["dfix_a"] = bf16(np.stack([-16 * s * fut for s in sl_a], 1))
    c["dfix_b"] = bf16(np.stack([np.where(adm, -16 * s * fut, -30000.0) for s in sl_b], 1))
    c["dmask"] = np.where(adm.T, 0.0, NEG).astype(np.float32)
    c["posrow"] = np.broadcast_to(pos.astype(np.float32)[None, :], (128, S)).copy()
    c["qpos"] = pos.astype(np.float32).reshape(S // 128, 128).T.copy()
    c["slope8"] = np.broadcast_to((8 * sl_a).astype(np.float32)[None, :], (128, 8)).copy()
    c["pow2"] = np.broadcast_to((2.0 ** -(np.arange(NBIS) + 1.0)).astype(np.float32)[None, :], (128, NBIS)).copy()
    c["ones_bf"] = bf16(np.ones((128, 128)))
    c["ltri"] = bf16(i[:, None] < i[None, :])
    c["iota64"] = np.broadcast_to(np.arange(64, dtype=np.float32)[None, :], (128, 64)).copy()
    c["ones_f"] = np.ones((128, 128), np.float32)
    return c


CONST_SPECS = None


def build_program(S, stop_after=None, nlayers=DEPTH, debug_out=False):
    nc = bass.Bass("TRN2", target_bir_lowering=False)
    P = Prog(nc)
    NT = S // 512
    NS = S // 128
    TOPK = min(256, S // 4)

    def ext(name, shape, dt=F32):
        return P.dram(name, shape, dt, kind="ExternalInput")

    x = ext("x", [S, D])
    mem = ext("mem", [256, D])
    ln_in_g = ext("ln_in_g", [1, D]); ln_in_b = ext("ln_in_b", [1, D])
    w_in = ext("w_in", [DEPTH, D, WIN])
    a_kv_norm = ext("a_kv_norm", [DEPTH, 128])
    a_w_uk = ext("a_w_uk", [DEPTH, 8, 128, 64]); a_w_uv = ext("a_w_uv", [DEPTH, 8, 128, 64])
    b_l = [ext(n, [DEPTH, 64]) for n in ("b_lq1", "b_lk1", "b_lq2", "b_lk2")]
    b_subln = ext("b_subln", [DEPTH, 128])
    w_o = ext("w_o", [DEPTH, D, D])
    ln_g = [ext(f"ln{i}_g", [DEPTH, D]) for i in (1, 2, 3)]
    ln_b = [ext(f"ln{i}_b", [DEPTH, D]) for i in (1, 2, 3)]
    m_wq = ext("m_wq", [DEPTH, D, D]); m_wkv = ext("m_wkv", [DEPTH, D, 2 * D]); m_wo = ext("m_wo", [DEPTH, D, D])
    router_w = ext("router_w", [DEPTH, D, NE]); router_bias = ext("router_bias", [DEPTH, NE])
    need_moe = stop_after is None or stop_after.startswith("p6")
    if need_moe:
        e_w_gate = ext("e_w_gate", [DEPTH, NE, D, 256]); e_w_up = ext("e_w_up", [DEPTH, NE, D, 256])
        e_w_down = ext("e_w_down", [DEPTH, NE, 256, D])
    s_w_gate = ext("s_w_gate", [DEPTH, D, 256]); s_w_up = ext("s_w_up", [DEPTH, D, 256])
    s_w_down = ext("s_w_down", [DEPTH, 256, D])
    cst = make_consts(S)
    cd = {k: ext("c_" + k, list(v.shape), BF16 if v.dtype == ml_dtypes.bfloat16 else F32) for k, v in cst.items()}
    out = P.dram("out", [S, D], F32, kind="ExternalOutput")

    def scratch(name, shape, dt, ntile):
        v = P.dram(name, shape, dt, kind="ExternalOutput" if debug_out else "Internal")
        hs = [H(f"{name}_{i}") for i in range(ntile)]
        return v.ap, hs
    h_tm, h_tm_h = scratch("h_tm", [S, D], F32, NT)
    hT, hT_h = scratch("hT", [D, S], BF16, NT)
    projT, projT_h = scratch("projT", [33, 64, S], BF16, NT)
    kaT, kaT_h = scratch("kaT", [8, 64, S], BF16, NT)
    va, va_h = scratch("va", [S, 512], BF16, NT)
    vb, vb_h = scratch("vb", [S, 512], BF16, NT)
    widx, widx_h = scratch("widx", [S, 8], F32, NT)
    catT, catT_h = scratch("catT", [D, S], BF16, NT)
    h_bf, h_bf_h = scratch("h_bf", [S, D], BF16, NS)
    C_ = S // 4
    Xg = P.dram("Xg", [NE * C_ + 128, D], BF16).ap
    Yg = P.dram("Yg", [NE * C_ + 128, D], BF16).ap
    TRASH = float(NE * C_)
    I32 = mybir.dt.int32
    U32 = mybir.dt.uint32
    off_t = [P.raw([128, 1], I32, f"off_t{i}") for i in range(16)]
    xs_t = [P.raw([128, D], BF16, f"xs_t{i}") for i in range(2)]
    gt_t = [P.raw([128, D], BF16, f"gt_t{i}") for i in range(8)]
    dbg = {}

    ident = P.sb([128, 128], BF16, "ident"); P.dma(ident, cd["ident"])
    ones_bf = P.sb([128, 128], BF16, "ones_bf"); P.dma(ones_bf, cd["ones_bf"])
    ones_f = P.sb([128, 128], F32, "ones_f"); P.dma(ones_f, cd["ones_f"])

    pbank = [P.ps([128, 512], F32, f"pb{i}") for i in range(8)]

    cnt = {"ln": 0}
    eps_ln = P.sb([128, 2], F32, "eps_ln")
    P.op("pool", "memset", ap=eps_ln[:, 0:1], constant=LN_EPS)
    P.op("pool", "memset", ap=eps_ln[:, 1:2], constant=RMS_EPS)
    LB = {}

    def ln_alloc():
        LB["g_bc"] = P.sb([128, D], F32, "g_bc"); LB["b_bc"] = P.sb([128, D], F32, "b_bc")
        LB["z"] = [P.sb([128, D], F32, f"ln_z{i}") for i in range(2)]
        LB["hb"] = [P.sb([128, D], BF16, f"ln_hb{i}") for i in range(2)]
        LB["st"] = [P.sb([128, 8], F32, f"ln_st{i}") for i in range(2)]
        LB["hTs"] = [P.sb([128, 8, 512], BF16, f"hTs{i}") for i in range(2)]

    def load_ln_params(g_ap, b_ap):
        P.dma(LB["g_bc"], V(g_ap.hs, g_ap.ap.partition_broadcast(128)))
        P.dma(LB["b_bc"], V(b_ap.hs, b_ap.ap.partition_broadcast(128)))

    def ln_tile(z, s128, final):
        i = cnt["ln"]; cnt["ln"] += 1
        stt = LB["st"][i % 2]
        hb = LB["hb"][i % 2]
        ln_junk = hb; g_bc = LB["g_bc"]; b_bc = LB["b_bc"]; hTs = LB["hTs"]
        P.op("act", "activation", out=ln_junk, in_=z, func=AF.Identity, accum_out=stt[:, 0:1])
        P.op("act", "activation", out=ln_junk, in_=z, func=AF.Square, accum_out=stt[:, 1:2])
        P.op("dve", "tensor_scalar", out=stt[:, 2:3], in0=stt[:, 0:1], scalar1=1.0 / D, scalar2=None, op0=ALU.mult)
        P.op("dve", "tensor_tensor", out=stt[:, 3:4], in0=stt[:, 2:3], in1=stt[:, 2:3], op=ALU.mult)
        P.op("dve", "scalar_tensor_tensor", out=stt[:, 4:5], in0=stt[:, 1:2], scalar=1.0 / D, in1=stt[:, 3:4],
             op0=ALU.mult, op1=ALU.subtract)
        P.op("act", "activation", out=stt[:, 6:7], in_=stt[:, 4:5], func=AF.Sqrt, bias=eps_ln[:, 0:1])
        P.op("dve", "reciprocal", out=stt[:, 5:6], in_=stt[:, 6:7])
        P.op("dve", "tensor_scalar", out=z, in0=z, scalar1=stt[:, 2:3], scalar2=stt[:, 5:6],
             op0=ALU.subtract, op1=ALU.mult)
        P.op("pool", "tensor_tensor", out=z, in0=z, in1=g_bc, op=ALU.mult)
        P.op("pool", "tensor_tensor", out=z, in0=z, in1=b_bc, op=ALU.add)
        tt = s128 // 4
        if final:
            P.dma(out[s128 * 128:(s128 + 1) * 128, :], z, q="pool")
        else:
            P.dma(V([h_tm_h[tt]], h_tm[s128 * 128:(s128 + 1) * 128, :]), z, q="pool")
        if final:
            return
        P.op("act", "activation", out=hb, in_=z, func=AF.Copy)
        P.dma(V([h_bf_h[s128]], h_bf[s128 * 128:(s128 + 1) * 128, :]), hb, q="pool")
        pst = pbank[7].bc(BF16)
        hs = hTs[tt % 2]
        for k in range(8):
            P.tr(pst[:, k * 128:(k + 1) * 128], hb[:, k * 128:(k + 1) * 128], ident)
        P.op("act", "activation", out=hs[:, :, (s128 % 4) * 128:(s128 % 4 + 1) * 128],
             in_=pst.re("p (k t) -> p k t", k=8), func=AF.Copy)
        if s128 % 4 == 3:
            P.dma(V([hT_h[tt]], hT.rearrange("(k p) s -> p k s", p=128)[:, :, tt * 512:(tt + 1) * 512]), hs, q="pool")

    p0_mark = P.mark()
    ln_alloc()
    load_ln_params(ln_in_g, ln_in_b)
    for s128 in range(NS):
        z = LB["z"][s128 % 2]
        P.dma(z, x[s128 * 128:(s128 + 1) * 128, :])
        ln_tile(z, s128, False)
    P.release(p0_mark)
    if stop_after == "p0":
        return finish(P, nc)

    bank_i = [0]

    rot = [[0, 1, 2, 3, 4, 5, 6]]

    def nb():
        r = rot[0]
        b = pbank[r[bank_i[0] % len(r)]]
        bank_i[0] += 1
        return b

    ev_i = [0]

    def evac(out, in_, act_only=False):
        ev_i[0] += 1
        if ev_i[0] % 2 or act_only:
            P.op("act", "activation", out=out, in_=in_, func=AF.Copy)
        else:
            P.op("dve", "tensor_copy", out=out, in_=in_)

    def wload(dst, src_ap, nk):
        srcv = src_ap.rearrange("(k p) c -> p k c", p=128)
        for k in range(nk):
            P.dma(dst[:, k, :], V([wH], srcv[:, k, :]), q="pool")
    wH = H("weights")

    def residual_ln(s128, ysrc, final):
        z = LB["z"][cnt["ln"] % 2]
        tt = s128 // 4
        P.dma(z, V([h_tm_h[tt]], h_tm[s128 * 128:(s128 + 1) * 128, :]))
        for hf in range(2):
            P.op("dve", "scalar_tensor_tensor", out=z[:, hf * 512:(hf + 1) * 512], in0=z[:, hf * 512:(hf + 1) * 512],
                 scalar=ALPHA, in1=ysrc[hf], op0=ALU.mult, op1=ALU.add)
        ln_tile(z, s128, final)

    base_mark = P.mark()
    OFF = dict(qa=0, ckv=512, qidx=640, kidx=1152, widx=1216, qb=1224, kb=1736, vb=2248)
    gcols = ([OFF["qa"] + 64 * h for h in range(8)] + [OFF["qidx"] + 64 * h for h in range(8)] + [OFF["kidx"]]
             + [OFF["qb"] + 64 * i for i in range(8)] + [OFF["kb"] + 64 * i for i in range(8)])
    projT_gh = [[H(f"projT_{b}_{t}") for t in range(NT)] for b in range(9)]
    kaT_gh = [[H(f"kaT_{b}_{t}") for t in range(NT)] for b in range(2)]
    catT_gh = [[H(f"catT_{b}_{t}") for t in range(NT)] for b in range(6)]

    for l in range(nlayers):
        lam_init = 0.8 - 0.6 * math.exp(-0.3 * l)
        w_in_sb = P.sb([128, 8, WIN], BF16, "w_in_sb"); wload(w_in_sb, w_in.ap[l], 8)
        wuk_sb = P.sb([128, 8, 64], BF16, "wuk_sb")
        P.dma(wuk_sb, V([wH], a_w_uk.ap[l].rearrange("h c d -> c h d")), q="pool")
        wuv_sb = P.sb([128, 8, 64], BF16, "wuv_sb")
        P.dma(wuv_sb, V([wH], a_w_uv.ap[l].rearrange("h c d -> c h d")), q="pool")
        kvn = P.sb([128, 1], F32, "kvn")
        P.dma(kvn, V([wH], a_kv_norm.ap[l:l + 1, :].rearrange("o c -> c o")))
        hTt = [P.sb([128, 8, 512], BF16, f"hTt{i}") for i in range(2)]
        stg64 = [P.sb([64, 4, 512], BF16, f"stg64_{i}") for i in range(3)]
        stg128 = [P.sb([128, 4, 512], BF16, f"stg128_{i}") for i in range(2)]
        stgw = [P.sb([128, 4, 8], F32, f"stgw_{i}") for i in range(2)]
        csq = P.sb([128, 512], F32, "csq"); rs = P.sb([128, 512], F32, "rs")
        cnT = P.sb([128, 512], BF16, "cnT")
        si = 0
        for tt in range(NT):
            ts = slice(tt * 512, (tt + 1) * 512)
            ht = hTt[tt % 2]
            P.dma(ht, V([hT_h[tt]], hT.rearrange("(k p) s -> p k s", p=128)[:, :, ts]))
            for b in range(9):
                gs = list(range(b * 4, min(b * 4 + 4, 33)))
                stg = stg64[si % 3]; si += 1
                for j, g in enumerate(gs):
                    ps = nb()
                    for k in range(8):
                        P.mm(ps[0:64, :], w_in_sb[:, k, gcols[g]:gcols[g] + 64], ht[:, k, :], k == 0, k == 7)
                    evac(stg[:, j, :], ps[0:64, :])
                P.dma(V([projT_gh[b][tt]], projT[gs[0]:gs[-1] + 1, :, ts].rearrange("g p s -> p g s")),
                      stg[:, 0:len(gs), :], q="pool")
            pc = nb()
            for k in range(8):
                P.mm(pc, w_in_sb[:, k, OFF["ckv"]:OFF["ckv"] + 128], ht[:, k, :], k == 0, k == 7)
            P.op("act", "activation", out=csq, in_=pc, func=AF.Square)
            pss = nb()
            P.mm(pss, ones_f, csq, True, True)
            P.op("act", "activation", out=rs, in_=pss, func=AF.Sqrt, scale=1.0 / 128, bias=eps_ln[:, 1:2])
            P.op("dve", "reciprocal", out=rs, in_=rs)
            P.op("dve", "scalar_tensor_tensor", out=cnT, in0=pc, scalar=kvn[:, 0:1], in1=rs, op0=ALU.mult, op1=ALU.mult)
            for b in range(2):
                stg = stg64[si % 3]; si += 1
                for j in range(4):
                    ps = nb()
                    P.mm(ps[0:64, :], wuk_sb[:, b * 4 + j, :], cnT, True, True)
                    evac(stg[:, j, :], ps[0:64, :])
                P.dma(V([kaT_gh[b][tt]], kaT[b * 4:b * 4 + 4, :, ts].rearrange("g p s -> p g s")), stg, q="pool")
            sg = stg128[0]
            for st_ in range(4):
                ps = nb()
                P.mm(ps, cnT[:, st_ * 128:(st_ + 1) * 128], wuv_sb.re("p h d -> p (h d)"), True, True)
                evac(sg[:, st_, :], ps)
            P.dma(V([va_h[tt]], va[ts, :].rearrange("(t p) c -> p t c", p=128)), sg, q="pool")
            sg = stg128[1]; sw = stgw[tt % 2]
            for st_ in range(4):
                ps = nb()
                for k in range(8):
                    P.mm(ps, ht[:, k, st_ * 128:(st_ + 1) * 128], w_in_sb[:, k, OFF["vb"]:OFF["vb"] + 512], k == 0, k == 7)
                evac(sg[:, st_, :], ps)
                ps = nb()
                for k in range(8):
                    P.mm(ps[:, 0:8], ht[:, k, st_ * 128:(st_ + 1) * 128], w_in_sb[:, k, OFF["widx"]:OFF["widx"] + 8], k == 0, k == 7)
                evac(sw[:, st_, :], ps[:, 0:8])
            P.dma(V([vb_h[tt]], vb[ts, :].rearrange("(t p) c -> p t c", p=128)), sg, q="pool")
            P.dma(V([widx_h[tt]], widx[ts, :].rearrange("(t p) c -> p t c", p=128)), sw, q="pool")
        P.release(base_mark)
        if stop_after == f"p1_{l}":
            return finish(P, nc)
        cq = {}
        kidx_sb = P.sb([64, S], BF16, "kidx_sb")
        P.dma(kidx_sb, V(projT_gh[4], projT[16, :, :]))
        ka_ring = [P.sb([69, S], BF16, f"ka{i}") for i in range(2)]
        for kr in ka_ring:
            P.dma(kr[64:65, :], cd["kaug"][2:3, :])
            P.dma(kr[65:69, :], cd["kaug"][0:4, :])
        va_sb = P.sb([128, NS, 8, 65], BF16, "va_sb")
        P.op("pool", "memset", ap=va_sb[:, :, :, 64:65], constant=1.0)
        for t in range(NS):
            P.dma(va_sb[:, t, :, 0:64], V([va_h[t // 4]], va[t * 128:(t + 1) * 128, :].rearrange("p (h d) -> p h d", h=8)))
        widx_sb = P.sb([128, NS, 8], F32, "widx_sb")
        P.dma(widx_sb, V(widx_h, widx.rearrange("(t p) c -> p t c", p=128)))
        isc = P.sb([128, S], F32, "isc")
        junk = P.sb([128, S], BF16, "junkb")
        maskq = [P.sb([128, S], BF16, f"maskq{i}") for i in range(1)]
        rot[0] = [0, 1, 2, 3, 4]
        maskT = P.sb([128, NS, 512], BF16, "maskT")
        posrow = P.sb([128, S], BF16, "posrow"); P.dma(posrow, cd["posrow"], q="pool")
        qpos = P.sb([128, NS], F32, "qpos"); P.dma(qpos, cd["qpos"])
        slope8 = P.sb([128, 8], F32, "slope8"); P.dma(slope8, cd["slope8"])
        pow2 = P.sb([128, NBIS], F32, "pow2"); P.dma(pow2, cd["pow2"])
        dmask = P.sb([128, 128], F32, "dmask"); P.dma(dmask, cd["dmask"])
        dfix_a = P.sb([128, 8, 128], BF16, "dfix_a"); P.dma(dfix_a, cd["dfix_a"])
        zero_c = P.sb([128, 1], F32, "zero_c"); P.op("pool", "memset", ap=zero_c, constant=0.0)
        negc = P.sb([128, 1], F32, "negc"); P.op("pool", "memset", ap=negc, constant=-30000.0)
        qi_ring = [P.sb([64, 8, 512], BF16, f"qi{i}") for i in range(1)]
        q_ring = [P.sb([69, 8, 512], BF16, f"qa{i}") for i in range(1)]
        rl_ring = [P.sb([128, 512], F32, f"rl{i}") for i in range(2)]
        e_ring = [P.sb([128, 512], BF16, f"e{i}") for i in range(4)]
        bst = [P.sb([128, 8], F32, f"bst{i}") for i in range(2)]
        steps = P.sb([128, NBIS], F32, "steps")
        shq = P.sb([128, 8], BF16, "shq")
        shT = P.sb([8, 512], BF16, "shT")
        osb = P.sb([65, 512], F32, "osb")
        oa_stg = [P.sb([64, 4, 512], BF16, f"oa_stg{i}") for i in range(2)]
        ei = 0; hh = 0; sti = 0
        for qt in range(NT):
            ts = slice(qt * 512, (qt + 1) * 512)
            qi = qi_ring[0]; qs = q_ring[0]
            P.dma(qi, V(projT_gh[2] + projT_gh[3], projT[8:16, :, ts].rearrange("g p s -> p g s")))
            P.dma(qs[0:64, :, :], V(projT_gh[0] + projT_gh[1], projT[0:8, :, ts].rearrange("g p s -> p g s")))
            P.dma(qs[65:69, :, :], V(cd["qaug_a"].hs, cd["qaug_a"].ap[:, :, ts].rearrange("h r s -> r h s")))
            for st_ in range(4):
                blk = qt * 4 + st_
                q0 = blk * 128
                nkeys = q0 + 128
                mq = maskq[0]
                b_ = bst[blk % 2]
                for h in range(8):
                    for kc in range((nkeys + 511) // 512):
                        w = min(512, nkeys - kc * 512)
                        ps = nb()
                        P.mm(ps[:, 0:w], qi[:, h, st_ * 128:(st_ + 1) * 128], kidx_sb[:, kc * 512:kc * 512 + w], True, True)
                        rl = rl_ring[ei % 2]; ei += 1
                        P.op("act", "activation", out=rl[:, 0:w], in_=ps[:, 0:w], func=AF.Relu)
                        dst = isc[:, kc * 512:kc * 512 + w]
                        if h == 0:
                            P.op("dve", "tensor_scalar", out=dst, in0=rl[:, 0:w], scalar1=widx_sb[:, blk, 0:1], scalar2=None, op0=ALU.mult)
                        else:
                            P.op("dve", "scalar_tensor_tensor", out=dst, in0=rl[:, 0:w], scalar=widx_sb[:, blk, h:h + 1], in1=dst,
                                 op0=ALU.mult, op1=ALU.add)
                P.op("dve", "tensor_tensor", out=isc[:, q0:q0 + 128], in0=isc[:, q0:q0 + 128], in1=dmask, op=ALU.add)
                tcol = b_[:, 4:5]
                if q0 < TOPK:
                    P.op("dve", "memset", ap=tcol, constant=-1.0e29)
                else:
                    P.op("dve", "tensor_reduce", out=b_[:, 0:1], in_=isc[:, 0:nkeys], axis=AX.X, op=ALU.max)
                    P.op("dve", "tensor_reduce", out=b_[:, 1:2], in_=isc[:, 0:q0], axis=AX.X, op=ALU.min)
                    P.op("dve", "tensor_tensor", out=b_[:, 2:3], in0=b_[:, 0:1], in1=b_[:, 1:2], op=ALU.subtract)
                    P.op("dve", "tensor_tensor", out=b_[:, 3:4], in0=b_[:, 0:1], in1=b_[:, 1:2], op=ALU.add)
                    P.op("dve", "tensor_scalar", out=tcol, in0=b_[:, 3:4], scalar1=0.5, scalar2=None, op0=ALU.mult)
                    P.op("dve", "tensor_scalar", out=steps, in0=pow2, scalar1=b_[:, 2:3], scalar2=None, op0=ALU.mult)
                    for n in range(NBIS):
                        P.op("dve", "tensor_scalar", out=junk[:, 0:nkeys], in0=isc[:, 0:nkeys], scalar1=tcol, scalar2=zero_c[:, 0:1],
                             op0=ALU.is_ge, op1=ALU.add, accum_out=b_[:, 5:6])
                        P.op("dve", "tensor_scalar", out=b_[:, 6:7], in0=b_[:, 5:6], scalar1=TOPK - 0.5, scalar2=0.5,
                             op0=ALU.is_gt, op1=ALU.subtract)
                        P.op("dve", "scalar_tensor_tensor", out=tcol, in0=b_[:, 6:7], scalar=steps[:, n:n + 1], in1=tcol,
                             op0=ALU.mult, op1=ALU.add)
                P.op("dve", "tensor_scalar", out=mq[:, 0:nkeys], in0=isc[:, 0:nkeys], scalar1=tcol, scalar2=None, op0=ALU.is_ge)
                P.op("dve", "tensor_tensor", out=junk[:, 0:nkeys], in0=mq[:, 0:nkeys], in1=posrow[:, 0:nkeys], op=ALU.mult)
                P.op("dve", "tensor_reduce", out=b_[:, 7:8], in_=junk[:, 0:nkeys], axis=AX.X, op=ALU.max)
                P.op("dve", "tensor_tensor", out=b_[:, 3:4], in0=qpos[:, blk:blk + 1], in1=b_[:, 7:8], op=ALU.subtract)
                P.op("dve", "tensor_scalar", out=shq, in0=slope8, scalar1=b_[:, 3:4], scalar2=None, op0=ALU.mult)
                pst = nb().bc(BF16)
                P.tr(pst[0:8, 0:128], shq, ident)
                evac(shT[:, st_ * 128:(st_ + 1) * 128], pst[0:8, 0:128], True)
                for kb0 in range(0, blk + 1, 8):
                    n_ = min(8, blk + 1 - kb0)
                    pst = nb().bc(BF16)
                    for j in range(n_):
                        P.tr(pst[:, j * 128:(j + 1) * 128], mq[:, (kb0 + j) * 128:(kb0 + j + 1) * 128], ident)
                    P.op("act", "activation", out=maskT[:, kb0:kb0 + n_, st_ * 128:(st_ + 1) * 128],
                         in_=pst[:, 0:n_ * 128].re("p (k t) -> p k t", k=n_), func=AF.Identity, scale=30000.0, bias=negc[:, 0:1])
            for h in range(8):
                P.dma(qs[64:65, h, :], shT[h:h + 1, :])
            nkb = 4 * (qt + 1)
            plist = []
            for h in range(8):
                for kb in range(nkb):
                    plist.append((h, kb))
            st8 = {}

            def s_part(h, kb, qt=qt, nkb=nkb, qs=qs, ts=ts):
                if kb == 0:
                    ka = ka_ring[st8["hh"] % 2]; st8["hh"] += 1
                    P.dma(ka[0:64, 0:nkb * 128], V(kaT_gh[h // 4], kaT[h, :, 0:nkb * 128]))
                    st8[("ka", h)] = ka
                ka = st8[("ka", h)]
                c0 = max(0, kb - 4 * qt) * 128
                ps = nb()
                kslc = ka[0:69, kb * 128:(kb + 1) * 128]
                if kb >= 4 * qt:
                    P.mm(ps[:, c0:c0 + 128], kslc, qs[0:69, h, c0:c0 + 128], True, False)
                    P.mm(ps[:, c0:c0 + 128], ident, dfix_a[:, h, :], False, False)
                    P.mm(ps[:, c0:c0 + 128], ident, maskT[:, kb, c0:c0 + 128], False, True)
                    if c0 + 128 < 512:
                        P.mm(ps[:, c0 + 128:512], kslc, qs[0:69, h, c0 + 128:512], True, False)
                        P.mm(ps[:, c0 + 128:512], ident, maskT[:, kb, c0 + 128:512], False, True)
                else:
                    P.mm(ps[:, c0:512], kslc, qs[0:69, h, c0:512], True, False)
                    P.mm(ps[:, c0:512], ident, maskT[:, kb, c0:512], False, True)
                E = e_ring[st8["sti"] % 4]; st8["sti"] += 1
                P.op("act", "activation", out=E[:, c0:512], in_=ps[:, c0:512], func=AF.Exp, scale=0.125)
                st8[("E", h, kb)] = E

            def r_part(h, kb, qt=qt, nkb=nkb, ts=ts):
                c0 = max(0, kb - 4 * qt) * 128
                E = st8.pop(("E", h, kb))
                po = pbank[5 + (h % 2)]
                P.mm(po[0:65, c0:512], va_sb[:, kb, h, :], E[:, c0:512], kb == 0, kb == nkb - 1)
                if kb == nkb - 1:
                    P.op("act", "activation", out=osb, in_=po[0:65, :], func=AF.Copy)
                    P.op("dve", "reciprocal", out=osb[64:65, :], in_=osb[64:65, :])
                    pb_ = pbank[7]
                    P.mm(pb_[0:64, :], ones_f[64:65, 0:64], osb[64:65, :], True, True)
                    og = oa_stg[(h // 4) % 2]
                    P.op("dve", "tensor_tensor", out=og[:, h % 4, :], in0=osb[0:64, :], in1=pb_[0:64, :], op=ALU.mult)
                    if h % 4 == 3:
                        h0 = h - 3
                        P.dma(V([catT_gh[h // 4][qt]], catT[h0 * 64:(h0 + 4) * 64, ts].rearrange("(g p) s -> p g s", p=64)), og, q="pool")
            st8["hh"] = hh; st8["sti"] = sti
            LA = 2
            for i in range(len(plist) + LA):
                if i < len(plist):
                    s_part(*plist[i])
                if i - LA >= 0:
                    r_part(*plist[i - LA])
            hh = st8["hh"]; sti = st8["sti"]
        P.release(base_mark)
        if stop_after == f"p2_{l}":
            return finish(P, nc)
        rot[0] = [0, 1, 2]
        kb_ring = [P.sb([68, S], BF16, f"kbr{i}") for i in range(4)]
        for kr in kb_ring:
            P.dma(kr[64:68, :], cd["kaug"][0:4, :])
        vb_sb = P.sb([128, NS, 512], BF16, "vb_sb")
        P.dma(vb_sb, V(vb_h, vb.rearrange("(t p) c -> p t c", p=128)))
        qb = P.sb([68, 8, 512], BF16, "qb")
        dfix_b = P.sb([128, 4, 128], BF16, "dfix_b"); P.dma(dfix_b, cd["dfix_b"])
        lt = [P.sb([128, 64], F32, f"lt{i}") for i in range(4)]
        for i in range(4):
            P.dma(lt[i], V(b_l[i].hs, b_l[i].ap[l:l + 1, :].partition_broadcast(128)))
        lsc = P.sb([128, 8], F32, "lsc")
        for j in range(2):
            P.op("dve", "tensor_tensor", out=lt[2 * j], in0=lt[2 * j], in1=lt[2 * j + 1], op=ALU.mult)
            P.op("dve", "tensor_reduce", out=lsc[:, j:j + 1], in_=lt[2 * j], axis=AX.X, op=ALU.add)
            P.op("act", "activation", out=lsc[:, 2 + j:3 + j], in_=lsc[:, j:j + 1], func=AF.Exp)
        P.op("dve", "tensor_tensor", out=lsc[:, 4:5], in0=lsc[:, 3:4], in1=lsc[:, 2:3], op=ALU.subtract)
        P.op("dve", "tensor_scalar", out=lsc[:, 5:6], in0=lsc[:, 4:5], scalar1=-lam_init, scalar2=None, op0=ALU.add)
        subc = P.sb([128, 1], F32, "subc")
        P.dma(subc, V([wH], b_subln.ap[l:l + 1, :].rearrange("o c -> c o")))
        P.op("dve", "tensor_scalar", out=subc, in0=subc, scalar1=1.0 - lam_init, scalar2=None, op0=ALU.mult)
        e_ring = [P.sb([128, 512], BF16, f"eb{i}") for i in range(4)]
        r_ = [P.sb([128, 512], F32, f"rr{i}") for i in range(4)]
        ob_stg = [P.sb([128, 512], BF16, f"ob_stg{i}") for i in range(2)]
        ki = 0; sti = 0
        for qt in range(NT):
            ts = slice(qt * 512, (qt + 1) * 512)
            nkb = 4 * (qt + 1)
            P.dma(qb[0:64, :, :], V(projT_gh[4] + projT_gh[5] + projT_gh[6], projT[17:25, :, ts].rearrange("g p s -> p g s")))
            for g in range(8):
                P.dma(qb[64:68, g, :], V(cd["qaug_b"].hs, cd["qaug_b"].ap[g // 2, :, ts]))
            steps = []
            for h in range(4):
                for kb in range(nkb):
                    for j in range(2):
                        steps.append((h, kb, j))
            st8 = {"ki": ki, "sti": sti}

            def s_part(h, kb, j, qt=qt, nkb=nkb):
                if kb == 0:
                    K_ = kb_ring[st8["ki"] % 4]; st8["ki"] += 1
                    P.dma(K_[0:64, 0:nkb * 128], V(projT_gh[6] + projT_gh[7] + projT_gh[8], projT[25 + 2 * h + j, :, 0:nkb * 128]))
                    st8[("K", h, j)] = K_
                K_ = st8[("K", h, j)]
                c0 = max(0, kb - 4 * qt) * 128
                ps = nb()
                kslc = K_[0:68, kb * 128:(kb + 1) * 128]
                if kb >= 4 * qt:
                    P.mm(ps[:, c0:c0 + 128], kslc, qb[0:68, 2 * h + j, c0:c0 + 128], True, False)
                    P.mm(ps[:, c0:c0 + 128], ident, dfix_b[:, h, :], False, True)
                    if c0 + 128 < 512:
                        P.mm(ps[:, c0 + 128:512], kslc, qb[0:68, 2 * h + j, c0 + 128:512], True, True)
                else:
                    P.mm(ps[:, c0:512], kslc, qb[0:68, 2 * h + j, c0:512], True, True)
                E = e_ring[st8["sti"] % 4]; st8["sti"] += 1
                P.op("act", "activation", out=E[:, c0:512], in_=ps[:, c0:512], func=AF.Exp, scale=0.125)
                st8[("E", h, kb, j)] = E

            def r_part(h, kb, j, qt=qt, nkb=nkb, ts=ts):
                c0 = max(0, kb - 4 * qt) * 128
                E = st8.pop(("E", h, kb, j))
                O_ = [pbank[3], pbank[4]]; Dn = [pbank[5], pbank[6]]
                P.mm(O_[j][:, c0:512], vb_sb[:, kb, h * 128:(h + 1) * 128], E[:, c0:512], kb == 0, kb == nkb - 1)
                P.mm(Dn[j][:, c0:512], ones_bf, E[:, c0:512], kb == 0, kb == nkb - 1)
                if kb == nkb - 1 and j == 1:
                    for jj in range(2):
                        P.op("dve", "reciprocal", out=r_[jj], in_=Dn[jj])
                        P.op("dve", "tensor_tensor", out=r_[jj], in0=O_[jj], in1=r_[jj], op=ALU.mult)
                    P.op("dve", "scalar_tensor_tensor", out=r_[2], in0=r_[1], scalar=lsc[:, 5:6], in1=r_[0], op0=ALU.mult, op1=ALU.add)
                    P.op("act", "activation", out=r_[3], in_=r_[2], func=AF.Square)
                    pss = pbank[7]
                    P.mm(pss, ones_f, r_[3], True, True)
                    P.op("act", "activation", out=r_[3], in_=pss, func=AF.Sqrt, scale=1.0 / 128, bias=eps_ln[:, 1:2])
                    P.op("dve", "reciprocal", out=r_[3], in_=r_[3])
                    og = ob_stg[h % 2]
                    P.op("dve", "scalar_tensor_tensor", out=og, in0=r_[2], scalar=subc[:, 0:1], in1=r_[3], op0=ALU.mult, op1=ALU.mult)
                    P.dma(V([catT_gh[2 + h][qt]], catT[512 + h * 128:512 + (h + 1) * 128, ts]), og, q="pool")
            LA = 2
            for i in range(len(steps) + LA):
                if i < len(steps):
                    s_part(*steps[i])
                if i - LA >= 0:
                    r_part(*steps[i - LA])
            ki = st8["ki"]; sti = st8["sti"]
        P.release(base_mark)
        if stop_after == f"p3_{l}":
            return finish(P, nc)
        rot[0] = [0, 1, 2, 3, 4, 5, 6]
        ln_alloc()
        load_ln_params(V(ln_g[0].hs, ln_g[0].ap[l:l + 1, :]), V(ln_b[0].hs, ln_b[0].ap[l:l + 1, :]))
        wo_sb = P.sb([128, 8, D], BF16, "wo_sb"); wload(wo_sb, w_o.ap[l], 8)
        ct_ring = [P.sb([128, 8, 512], BF16, f"ct{i}") for i in range(2)]
        for tt in range(NT):
            ts = slice(tt * 512, (tt + 1) * 512)
            ct = ct_ring[tt % 2]
            P.dma(ct, V([g_[tt] for g_ in catT_gh], catT.rearrange("(k p) s -> p k s", p=128)[:, :, ts]))
            for st_ in range(4):
                ys = [nb(), nb()]
                for hf in range(2):
                    for k in range(8):
                        P.mm(ys[hf], ct[:, k, st_ * 128:(st_ + 1) * 128], wo_sb[:, k, hf * 512:(hf + 1) * 512], k == 0, k == 7)
                residual_ln(tt * 4 + st_, ys, False)
        P.release(base_mark)
        if stop_after == f"p4_{l}":
            return finish(P, nc)
        rot[0] = [0, 1, 2, 3, 4]
        ln_alloc()
        load_ln_params(V(ln_g[1].hs, ln_g[1].ap[l:l + 1, :]), V(ln_b[1].hs, ln_b[1].ap[l:l + 1, :]))
        wq_sb = P.sb([128, 8, D], BF16, "wq_sb"); wload(wq_sb, m_wq.ap[l], 8)
        wkv_sb = P.sb([128, 8, 2 * D], BF16, "wkv_sb"); wload(wkv_sb, m_wkv.ap[l], 8)
        wo2_sb = P.sb([128, 8, D], BF16, "wo2_sb"); wload(wo2_sb, m_wo.ap[l], 8)
        mem_b = P.sb([128, 2, D], BF16, "mem_b")
        P.dma(mem_b, V(mem.hs, mem.ap.rearrange("(t p) d -> p t d", p=128)), q="pool")
        memT = P.sb([128, 8, 256], BF16, "memT")
        for t in range(2):
            pst = nb().bc(BF16)
            for k in range(8):
                P.tr(pst[:, k * 128:(k + 1) * 128], mem_b[:, t, k * 128:(k + 1) * 128], ident)
            evac(memT[:, :, t * 128:(t + 1) * 128], pst.re("p (k t) -> p k t", k=8))
        KT = P.sb([128, 8, 256], BF16, "KT")
        for dc in range(8):
            ps = nb()
            for k in range(8):
                P.mm(ps[:, 0:256], wkv_sb[:, k, dc * 128:(dc + 1) * 128], memT[:, k, :], k == 0, k == 7)
            evac(KT[:, dc, :], ps[:, 0:256])
        Vm = P.sb([128, 2, D], BF16, "Vm")
        for t in range(2):
            for hf in range(2):
                ps = nb()
                for k in range(8):
                    P.mm(ps, memT[:, k, t * 128:(t + 1) * 128], wkv_sb[:, k, D + hf * 512:D + (hf + 1) * 512], k == 0, k == 7)
                evac(Vm[:, t, hf * 512:(hf + 1) * 512], ps)
        ht_ring = [P.sb([128, 8, 512], BF16, f"ht5_{i}") for i in range(2)]
        QT = P.sb([128, 8, 512], BF16, "QT"); OT = P.sb([128, 8, 512], BF16, "OT")
        e5 = [P.sb([128, 2, 512], BF16, f"e5_{i}") for i in range(2)]
        rd = P.sb([128, 512], F32, "rd5")
        for tt in range(NT):
            ts = slice(tt * 512, (tt + 1) * 512)
            ht = ht_ring[tt % 2]
            P.dma(ht, V([hT_h[tt]], hT.rearrange("(k p) s -> p k s", p=128)[:, :, ts]))
            for dc in range(8):
                ps = nb()
                for k in range(8):
                    P.mm(ps, wq_sb[:, k, dc * 128:(dc + 1) * 128], ht[:, k, :], k == 0, k == 7)
                evac(QT[:, dc, :], ps)
            for h in range(4):
                E = e5[h % 2]
                for mb in range(2):
                    ps = nb()
                    for dc in range(2):
                        P.mm(ps, KT[:, 2 * h + dc, mb * 128:(mb + 1) * 128], QT[:, 2 * h + dc, :], dc == 0, dc == 1)
                    P.op("act", "activation", out=E[:, mb, :], in_=ps, func=AF.Exp, scale=1.0 / 16)
                den = pbank[5]
                for mb in range(2):
                    P.mm(den, ones_bf, E[:, mb, :], mb == 0, mb == 1)
                P.op("dve", "reciprocal", out=rd, in_=den)
                for dvc in range(2):
                    po = pbank[6] if dvc == 0 else nb()
                    for mb in range(2):
                        P.mm(po, Vm[:, mb, h * 256 + dvc * 128:h * 256 + (dvc + 1) * 128], E[:, mb, :], mb == 0, mb == 1)
                    P.op("dve", "tensor_tensor", out=OT[:, 2 * h + dvc, :], in0=po, in1=rd, op=ALU.mult)
            for st_ in range(4):
                ys = [nb(), nb()]
                for hf in range(2):
                    for k in range(8):
                        P.mm(ys[hf], OT[:, k, st_ * 128:(st_ + 1) * 128], wo2_sb[:, k, hf * 512:(hf + 1) * 512], k == 0, k == 7)
                residual_ln(tt * 4 + st_, ys, False)
        P.release(base_mark)
        if stop_after == f"p5_{l}":
            return finish(P, nc)
        rot[0] = [0, 1, 2, 3, 4, 5, 6]
        last = (l == nlayers - 1)
        ln_alloc()
        load_ln_params(V(ln_g[2].hs, ln_g[2].ap[l:l + 1, :]), V(ln_b[2].hs, ln_b[2].ap[l:l + 1, :]))
        NCS = C_ // 128
        WS = min(512, C_)
        rw_sb = P.sb([128, 8, NE], BF16, "rw_sb"); wload(rw_sb, router_w.ap[l], 8)
        rbias = P.sb([128, NE], F32, "rbias")
        P.dma(rbias, V(router_bias.hs, router_bias.ap[l:l + 1, :].partition_broadcast(128)))
        iota64 = P.sb([128, NE], F32, "iota64"); P.dma(iota64, cd["iota64"])
        ltri = P.sb([128, 128], BF16, "ltri"); P.dma(ltri, cd["ltri"])
        M_bf = P.sb([128, NS, NE], BF16, "M_bf")
        gk = P.sb([128, NS, 8], F32, "gk")
        offf = P.sb([128, NS, 8], F32, "offf")
        rt = [P.sb([128, NE], F32, f"rt{i}") for i in range(6)]
        rsm = P.sb([128, 16], F32, "rsm")
        idx8 = P.sb([128, 8], U32, "idx8"); idxf = P.sb([128, 8], F32, "idxf"); slotf = P.sb([128, 8], F32, "slotf")
        ovf = P.sb([128, 8], F32, "ovf")
        oh3 = P.sb([128, 8, NE], F32, "oh3"); oh3b = P.sb([128, 8, NE], F32, "oh3b")
        ht_ring = [P.sb([128, 8, 512], BF16, f"ht6_{i}") for i in range(2)]
        Xg_hs = []
        Yg_hs = [[H(f"Yg_{e}_{t}") for t in range(NCS)] for e in range(NE)]
        ztile = P.sb([128, D], BF16, "ztile")
        P.op("pool", "memset", ap=ztile, constant=0.0)
        yg_trash_h = H("Yg_trash")
        P.dma(V([yg_trash_h], Yg[NE * C_:NE * C_ + 128, :]), ztile, q="pool")
        oi = 0
        for tt in range(NT):
            ht = ht_ring[tt % 2]
            P.dma(ht, V([hT_h[tt]], hT.rearrange("(k p) s -> p k s", p=128)[:, :, tt * 512:(tt + 1) * 512]))
            for st_ in range(4):
                s_ = tt * 4 + st_
                ps = nb()
                for k in range(8):
                    P.mm(ps[:, 0:NE], ht[:, k, st_ * 128:(st_ + 1) * 128], rw_sb[:, k, :], k == 0, k == 7)
                P.op("act", "activation", out=rt[0], in_=ps[:, 0:NE], func=AF.Sigmoid)
                P.op("dve", "tensor_tensor", out=rt[1], in0=rt[0], in1=rbias, op=ALU.add)
                P.op("dve", "max", out=rsm[:, 0:8], in_=rt[1])
                P.op("dve", "max_index", out=idx8, in_max=rsm[:, 0:8], in_values=rt[1])
                P.op("dve", "tensor_copy", out=idxf, in_=idx8)
                P.op("dve", "tensor_scalar", out=rt[2], in0=rt[1], scalar1=rsm[:, 7:8], scalar2=None, op0=ALU.is_ge)
                P.op("dve", "tensor_copy", out=M_bf[:, s_, :], in_=rt[2])
                P.op("dve", "tensor_tensor", out=rt[2], in0=rt[2], in1=rt[0], op=ALU.mult)
                P.op("dve", "tensor_reduce", out=rsm[:, 8:9], in_=rt[2], axis=AX.X, op=ALU.add)
                P.op("dve", "reciprocal", out=rsm[:, 9:10], in_=rsm[:, 8:9])
                P.op("dve", "tensor_scalar", out=rt[2], in0=rt[2], scalar1=rsm[:, 9:10], scalar2=None, op0=ALU.mult)
                P.op("dve", "tensor_scalar", out=rt[3], in0=rt[2], scalar1=2.5, scalar2=None, op0=ALU.mult)
                pc = nb()
                P.mm(pc[:, 0:NE], ltri, M_bf[:, s_, :], True, s_ == 0)
                for j in range(s_):
                    P.mm(pc[:, 0:NE], ones_bf, M_bf[:, j, :], False, j == s_ - 1)
                P.op("act", "activation", out=rt[4], in_=pc[:, 0:NE], func=AF.Copy)
                io_b = V(iota64.hs, iota64.ap.unsqueeze(1).broadcast_to([128, 8, NE]))
                ix_b = V(idxf.hs, idxf.ap.unsqueeze(2).broadcast_to([128, 8, NE]))
                cu_b = V(rt[4].hs, rt[4].ap.unsqueeze(1).broadcast_to([128, 8, NE]))
                g_b = V(rt[3].hs, rt[3].ap.unsqueeze(1).broadcast_to([128, 8, NE]))
                P.op("dve", "tensor_tensor", out=oh3, in0=io_b, in1=ix_b, op=ALU.is_equal)
                P.op("dve", "tensor_tensor", out=oh3b, in0=oh3, in1=cu_b, op=ALU.mult)
                P.op("dve", "tensor_reduce", out=slotf, in_=oh3b, axis=AX.X, op=ALU.add)
                P.op("dve", "tensor_tensor", out=oh3b, in0=oh3, in1=g_b, op=ALU.mult)
                P.op("dve", "tensor_reduce", out=gk[:, s_, :], in_=oh3b, axis=AX.X, op=ALU.add)
                P.op("dve", "tensor_scalar", out=ovf, in0=slotf, scalar1=C_ - 0.5, scalar2=None, op0=ALU.is_lt)
                P.op("dve", "tensor_tensor", out=gk[:, s_, :], in0=gk[:, s_, :], in1=ovf, op=ALU.mult)
                P.op("dve", "scalar_tensor_tensor", out=offf[:, s_, :], in0=idxf, scalar=float(C_), in1=slotf, op0=ALU.mult, op1=ALU.add)
                P.op("dve", "tensor_scalar", out=offf[:, s_, :], in0=offf[:, s_, :], scalar1=-TRASH, scalar2=None, op0=ALU.add)
                P.op("dve", "tensor_tensor", out=offf[:, s_, :], in0=offf[:, s_, :], in1=ovf, op=ALU.mult)
                P.op("dve", "tensor_scalar", out=offf[:, s_, :], in0=offf[:, s_, :], scalar1=TRASH, scalar2=None, op0=ALU.add)
                xs = xs_t[s_ % 2]
                P.dma(xs, V([h_bf_h[s_]], h_bf[s_ * 128:(s_ + 1) * 128, :]))
                for k in range(8):
                    ot = off_t[oi % 16]; oi += 1
                    P.op("dve", "tensor_copy", out=ot, in_=offf[:, s_, k:k + 1])
                    hx = H(f"Xg_{s_}_{k}"); Xg_hs.append(hx)
                    P.add("pool", (lambda e, ot=ot, xs=xs: e.indirect_dma_start(
                        out=Xg[:, :], out_offset=bass.IndirectOffsetOnAxis(ap=ot.ap[:, 0:1], axis=0),
                        in_=xs.ap[:, :], in_offset=None, bounds_check=None)),
                        [xs, ot], [V([hx], Xg)], dma=True)
        wg_r = [P.sb([128, 8, 256], BF16, f"wg{i}") for i in range(2)]
        wu_r = [P.sb([128, 8, 256], BF16, f"wu{i}") for i in range(2)]
        wd_r = [P.sb([128, 2, D], BF16, f"wd{i}") for i in range(2)]
        sg_r = [P.sb([128, 512], F32, f"sg{i}") for i in range(2)]
        at_r = [P.sb([128, 2, 512], BF16, f"at{i}") for i in range(2)]
        stageb_mark = P.mark()
        xe_r = [P.sb([128, NCS, D], BF16, f"xe{i}") for i in range(2)]
        xT_r = [P.sb([128, 8, C_], BF16, f"xT{i}") for i in range(2)]
        ysb_r = [P.sb([128, D], BF16, f"ysb{i}") for i in range(4)]
        yi = 0

        def expert_block(wg, wu, wd, rhs_of, n_sub, emit_out, ai0):
            ai = ai0
            c0 = 0
            while c0 < n_sub * 128:
                w = min(512, n_sub * 128 - c0)
                gps = [pbank[0 + 2 * (ai % 2)], pbank[1 + 2 * (ai % 2)]]
                ups = [pbank[4], pbank[5]]
                for m in range(2):
                    for k in range(8):
                        P.mm(gps[m][:, 0:w], wg[:, k, m * 128:(m + 1) * 128], rhs_of(k, c0, w), k == 0, k == 7)
                for m in range(2):
                    for k in range(8):
                        P.mm(ups[m][:, 0:w], wu[:, k, m * 128:(m + 1) * 128], rhs_of(k, c0, w), k == 0, k == 7)
                at = at_r[ai % 2]
                for m in range(2):
                    sg = sg_r[m]
                    P.op("act", "activation", out=sg[:, 0:w], in_=gps[m][:, 0:w], func=AF.Silu)
                    P.op("dve", "tensor_tensor", out=at[:, m, 0:w], in0=sg[:, 0:w], in1=ups[m][:, 0:w], op=ALU.mult)
                for st_ in range(w // 128):
                    pys = [pbank[6], pbank[7]]
                    for hf in range(2):
                        for m in range(2):
                            P.mm(pys[hf], at[:, m, st_ * 128:(st_ + 1) * 128], wd[:, m, hf * 512:(hf + 1) * 512], m == 0, m == 1)
                    emit_out(c0 // 128 + st_, pys)
                ai += 1
                c0 += w
            return ai

        ai = 0
        for e in range(NE):
            wg = wg_r[e % 2]; wu = wu_r[e % 2]; wd = wd_r[e % 2]
            if e == 0:
                wload(wg, e_w_gate.ap[l, 0], 8); wload(wu, e_w_up.ap[l, 0], 8); wload(wd, e_w_down.ap[l, 0], 2)
            if e + 1 < NE:
                wload(wg_r[(e + 1) % 2], e_w_gate.ap[l, e + 1], 8); wload(wu_r[(e + 1) % 2], e_w_up.ap[l, e + 1], 8)
                wload(wd_r[(e + 1) % 2], e_w_down.ap[l, e + 1], 2)
            xe = xe_r[e % 2]; xT = xT_r[e % 2]
            if e == 0:
                P.dma(xe, V(Xg_hs, Xg[0:C_, :].rearrange("(t p) d -> p t d", p=128)))
            if e + 1 < NE:
                P.dma(xe_r[(e + 1) % 2], V(Xg_hs, Xg[(e + 1) * C_:(e + 2) * C_, :].rearrange("(t p) d -> p t d", p=128)))
            for t in range(NCS):
                pst = nb().bc(BF16)
                for k in range(8):
                    P.tr(pst[:, k * 128:(k + 1) * 128], xe[:, t, k * 128:(k + 1) * 128], ident)
                evac(xT[:, :, t * 128:(t + 1) * 128], pst.re("p (k t) -> p k t", k=8))

            def emit_out(sub, pys, e=e):
                nonlocal yi
                ysb = ysb_r[yi % 4]; yi += 1
                P.op("act", "activation", out=ysb[:, 0:512], in_=pys[0], func=AF.Copy)
                P.op("dve", "tensor_copy", out=ysb[:, 512:1024], in_=pys[1])
                P.dma(V([Yg_hs[e][sub]], Yg[e * C_ + sub * 128:e * C_ + (sub + 1) * 128, :]), ysb, q="sp")
            ai = expert_block(wg, wu, wd, lambda k, c0, w, xT=xT: xT[:, k, c0:c0 + w], NCS, emit_out, ai)
        P.release(stageb_mark)
        wg = wg_r[0]; wu = wu_r[0]; wd = wd_r[0]
        wload(wg, s_w_gate.ap[l], 8); wload(wu, s_w_up.ap[l], 8); wload(wd, s_w_down.ap[l], 2)
        yacc_r = [P.sb([128, D], F32, f"yacc{i}") for i in range(3)]
        allY = [h_ for row in Yg_hs for h_ in row] + [yg_trash_h]
        gi = 0
        for tt in range(NT):
            ht = ht_ring[tt % 2]
            P.dma(ht, V([hT_h[tt]], hT.rearrange("(k p) s -> p k s", p=128)[:, :, tt * 512:(tt + 1) * 512]))

            def emit_out(sub, pys, tt=tt):
                nonlocal gi, oi, yi
                s_ = tt * 4 + sub
                ya = yacc_r[yi % 3]; yi += 1
                P.op("act", "activation", out=ya[:, 0:512], in_=pys[0], func=AF.Copy)
                P.op("act", "activation", out=ya[:, 512:1024], in_=pys[1], func=AF.Copy)
                for k in range(8):
                    ot = off_t[oi % 16]; oi += 1
                    P.op("dve", "tensor_copy", out=ot, in_=offf[:, s_, k:k + 1])
                    gt = gt_t[gi % 8]; gi += 1
                    P.add("pool", (lambda e, ot=ot, gt=gt: e.indirect_dma_start(
                        out=gt.ap[:, :], out_offset=None, in_=Yg[:, :],
                        in_offset=bass.IndirectOffsetOnAxis(ap=ot.ap[:, 0:1], axis=0), bounds_check=None)),
                        [V(allY, Yg), ot], [gt], dma=True)
                    P.op("dve", "scalar_tensor_tensor", out=ya, in0=gt, scalar=gk[:, s_, k:k + 1], in1=ya, op0=ALU.mult, op1=ALU.add)
                residual_ln(s_, [ya[:, 0:512], ya[:, 512:1024]], last)
            ai = expert_block(wg, wu, wd, lambda k, c0, w, ht=ht: ht[:, k, c0:c0 + w], 4, emit_out, ai)
        P.release(base_mark)
        if stop_after == f"p6_{l}":
            return finish(P, nc)
    return finish(P, nc)


def finish(P, nc):
    P.emit()
    return nc


def core_inputs(inp, core, S, need_moe=True):
    m = {}
    m["x"] = np.ascontiguousarray(inp["x"][core])
    m["mem"] = np.ascontiguousarray(inp["mem"][core])
    for k, v in inp.items():
        if k in ("x", "mem") or (not need_moe and k.startswith("e_w_")):
            continue
        if k in ("ln_in_g", "ln_in_b"):
            v = v.reshape(1, -1)
        m[k] = np.ascontiguousarray(v)
    for k, v in make_consts(S).items():
        m["c_" + k] = v
    return m


_CACHE = {}


def kernel(**inputs):
    S = inputs["x"].shape[1]
    nb = inputs["x"].shape[0]
    inp = {k: np.asarray(v) for k, v in inputs.items()}
    if S not in _CACHE:
        _CACHE[S] = build_program(S)
    nc = _CACHE[S]
    in_maps = [core_inputs(inp, c, S) for c in range(nb)]
    res = run_bass_kernel_spmd(nc, in_maps, core_ids=list(range(nb)))
    return np.stack([np.asarray(r["out"]) for r in res.results], axis=0).astype(np.float32)
```

```python
import math
from contextlib import ExitStack

import numpy as np
import ml_dtypes

import concourse.bass as bass
import concourse.mybir as mybir
from concourse.bass_utils import run_bass_kernel_spmd

F32 = mybir.dt.float32
BF16 = mybir.dt.bfloat16
ALU = mybir.AluOpType
AF = mybir.ActivationFunctionType
AX = mybir.AxisListType

D = 1024
DEPTH = 2
NE = 64
ALPHA = (2 * DEPTH) ** 0.25
LN_EPS = 1e-5
RMS_EPS = 1e-6
WIN = 2760
NBIS = 14
NEG = -1.0e30


class H:
    __slots__ = ("name", "last_w", "rd_eng", "rd_dma")

    def __init__(self, name=""):
        self.name = name
        self.last_w = None
        self.rd_eng = {}
        self.rd_dma = []


class V:
    def __init__(self, hs, ap):
        self.hs = hs
        self.ap = ap

    def __getitem__(self, k):
        return V(self.hs, self.ap[k])

    def re(self, s, **kw):
        return V(self.hs, self.ap.rearrange(s, **kw))

    def bc(self, dt):
        return V(self.hs, self.ap.bitcast(dt))


class Op:
    __slots__ = ("eng", "fn", "dma", "deps", "needs_inc", "sem", "val", "prev_val")

    def __init__(self, eng, fn, dma):
        self.eng = eng
        self.fn = fn
        self.dma = dma
        self.deps = ()
        self.needs_inc = False
        self.sem = None
        self.val = 0
        self.prev_val = 0


ENGS = ("pe", "act", "dve", "pool", "sp")
NDMASEM = 24


class Prog:
    def __init__(self, nc):
        self.nc = nc
        self.ops = {e: [] for e in ENGS}
        self.stack = ExitStack()
        self.nalloc = 0
        self.dma_scan = {}

    ARENA = 176 * 1024

    def sb(self, shape, dt, name=None):
        self.nalloc += 1
        name = name or f"sb{self.nalloc}"
        if not hasattr(self, "arena"):
            self.arena = self.stack.enter_context(
                self.nc.sbuf_tensor("arena", [128, self.ARENA], mybir.dt.uint8)).ap()
            self.off = 0
            self.barrier_deps = {e: None for e in ENGS}
        esz = 2 if dt == BF16 else 4
        n = int(np.prod(shape[1:]))
        nbytes = (n * esz + 63) // 64 * 64
        assert self.off + nbytes <= self.ARENA, f"SBUF arena overflow at {name}: {self.off}+{nbytes}"
        ap = self.arena[0:shape[0], self.off:self.off + n * esz].bitcast(dt)
        self.off += nbytes
        if len(shape) == 3:
            ap = ap.rearrange("p (a b) -> p a b", a=shape[1])
        elif len(shape) == 4:
            ap = ap.rearrange("p (a b c) -> p a b c", a=shape[1], b=shape[2])
        return V([H(name)], ap)

    def raw(self, shape, dt, name):
        t = self.stack.enter_context(self.nc.sbuf_tensor(name, list(shape), dt))
        return V([H(name)], t.ap())

    def mark(self):
        return self.off

    def release(self, mark):
        deps = set()
        for e in ENGS:
            last_c = None
            for op in reversed(self.ops[e]):
                if not op.dma:
                    last_c = op
                    break
            if last_c is not None:
                deps.add(last_c)
            for op in self.ops[e][self.dma_scan.get(e, 0):]:
                if op.dma:
                    deps.add(op)
            self.dma_scan[e] = len(self.ops[e])
        for d in deps:
            d.needs_inc = True
        for e in ENGS:
            prev = self.barrier_deps[e] or set()
            self.barrier_deps[e] = prev | deps
        self.off = mark

    def ps(self, shape, dt, name=None):
        self.nalloc += 1
        name = name or f"ps{self.nalloc}"
        t = self.stack.enter_context(self.nc.psum_tensor(name, list(shape), dt))
        return V([H(name)], t.ap())

    def dram(self, name, shape, dt, kind="Internal"):
        t = self.nc.dram_tensor(name, list(shape), dt, kind=kind)
        return V([H(name)], t.ap())

    def add(self, eng, fn, reads=(), writes=(), dma=False):
        op = Op(eng, fn, dma)
        deps = set()
        for v in reads:
            for h in v.hs:
                w = h.last_w
                if w is not None:
                    deps.add(w)
        for v in writes:
            for h in v.hs:
                w = h.last_w
                if w is not None and (w.dma or dma or w.eng != eng):
                    deps.add(w)
                for e, r in h.rd_eng.items():
                    if dma or e != eng:
                        deps.add(r)
                for r in h.rd_dma:
                    deps.add(r)
        if getattr(self, "barrier_deps", None) and self.barrier_deps[eng]:
            deps |= self.barrier_deps[eng]
            self.barrier_deps[eng] = None
        deps.discard(op)
        op.deps = tuple(deps)
        for d in deps:
            d.needs_inc = True
        for v in reads:
            for h in v.hs:
                if dma:
                    h.rd_dma.append(op)
                else:
                    h.rd_eng[eng] = op
        for v in writes:
            for h in v.hs:
                h.last_w = op
                h.rd_eng = {}
                h.rd_dma = []
        self.ops[eng].append(op)
        return op

    def op(self, eng, meth, **kw):
        reads, writes, real = [], [], {}
        kw.pop("_w", None)
        for k, v in kw.items():
            if isinstance(v, V):
                (writes if (k.startswith("out") or k in ("accum_out", "ap")) else reads).append(v)
                real[k] = v.ap
            else:
                real[k] = v
        return self.add(eng, lambda e: getattr(e, meth)(**real), reads, writes)

    def mm(self, out, lhsT, rhs, start, stop):
        return self.add("pe", lambda e: e.matmul(out.ap, lhsT.ap, rhs.ap, start=start, stop=stop),
                        [lhsT, rhs], [out])

    def tr(self, out, in_, ident):
        return self.add("pe", lambda e: e.transpose(out.ap, in_.ap, ident.ap), [in_, ident], [out])

    def dma(self, out, in_, q="sp"):
        return self.add(q, lambda e: e.dma_start(out=out.ap, in_=in_.ap), [in_], [out], dma=True)

    def emit(self):
        nc = self.nc
        st = self.stack
        esem = {e: st.enter_context(nc.semaphore(f"es_{e}")) for e in ENGS}
        dsem = {e: [st.enter_context(nc.semaphore(f"ds_{e}_{i}")) for i in range(NDMASEM)]
                for e in ("sp", "pool", "act")}
        for e in ENGS:
            cnt = 0
            duse = [0] * NDMASEM
            di = 0
            for op in self.ops[e]:
                if op.dma:
                    k = di % NDMASEM
                    di += 1
                    op.sem = dsem[e][k]
                    op.prev_val = duse[k]
                    duse[k] += 16
                    op.val = duse[k]
                elif op.needs_inc:
                    cnt += 1
                    op.sem = esem[e]
                    op.val = cnt
            if e == "sp":
                self.final_dma = [(dsem[e][k], duse[k]) for k in range(NDMASEM) if duse[k] > 0]
            if e == "pool":
                self.final_dma_pool = [(dsem[e][k], duse[k]) for k in range(NDMASEM) if duse[k] > 0]

        def run(ename, eng):
            waited = {}
            for op in self.ops[ename]:
                need = {}
                for d in op.deps:
                    key = id(d.sem)
                    if need.get(key, (None, -1))[1] < d.val:
                        need[key] = (d.sem, d.val)
                if op.dma and op.prev_val > 0:
                    key = id(op.sem)
                    if need.get(key, (None, -1))[1] < op.prev_val:
                        need[key] = (op.sem, op.prev_val)
                for key, (sem, val) in need.items():
                    if waited.get(key, 0) < val:
                        eng.wait_ge(sem, val)
                        waited[key] = val
                ins = op.fn(eng)
                if op.dma:
                    ins.then_inc(op.sem, 16)
                elif op.needs_inc:
                    ins.then_inc(op.sem, 1)
            if ename == "sp":
                for sem, val in self.final_dma:
                    eng.wait_ge(sem, val)
            if ename == "pool":
                for sem, val in self.final_dma_pool:
                    eng.wait_ge(sem, val)

        with nc.Block() as block:
            @block.tensor
            def _(eng):
                run("pe", eng)

            @block.scalar
            def _(eng):
                run("act", eng)

            @block.vector
            def _(eng):
                run("dve", eng)

            @block.gpsimd
            def _(eng):
                run("pool", eng)

            @block.sync
            def _(eng):
                run("sp", eng)
        st.close()


def bf16(a):
    return np.asarray(a, dtype=np.float32).astype(ml_dtypes.bfloat16)


def make_consts(S):
    pos = np.arange(S)
    a = (pos // 128).astype(np.float32)
    b = (pos % 128).astype(np.float32)
    c = {}
    c["ident"] = bf16(np.eye(128))
    c["kaug"] = bf16(np.stack([128 * a, b, np.ones(S), np.ones(S)]))
    sl_a = 2.0 ** (-8.0 * np.arange(1, 9) / 8)
    sl_b = 2.0 ** (-8.0 * np.arange(1, 5) / 4)
    def qaug(sl):
        return bf16(np.stack([np.stack([8 * s * np.ones(S), 8 * s * np.ones(S), -8 * s * 128 * a, -8 * s * b])
                              for s in sl]))
    c["qaug_a"] = qaug(sl_a)
    c["qaug_b"] = qaug(sl_b)
    i = np.arange(128)
    adm = (i[:, None] // 64) <= (i[None, :] // 64)
    fut = np.maximum(i[:, None] - i[None, :], 0).astype(np.float32)
    c["dfix_a"] = bf16(np.stack([-16 * s * fut for s in sl_a], 1))
    c["dfix_b"] = bf16(np.stack([np.where(adm, -16 * s * fut, -30000.0) for s in sl_b], 1))
    c["dmask"] = np.where(adm.T, 0.0, NEG).astype(np.float32)
    c["posrow"] = np.broadcast_to(pos.astype(np.float32)[None, :], (128, S)).copy()
    c["qpos"] = pos.astype(np.float32).reshape(S // 128, 128).T.copy()
    c["slope8"] = np.broadcast_to((8 * sl_a).astype(np.float32)[None, :], (128, 8)).copy()
    c["pow2"] = np.broadcast_to((2.0 ** -(np.arange(NBIS) + 1.0)).astype(np.float32)[None, :], (128, NBIS)).copy()
    c["ones_bf"] = bf16(np.ones((128, 128)))
    c["ltri"] = bf16(i[:, None] < i[None, :])
    c["iota64"] = np.broadcast_to(np.arange(64, dtype=np.float32)[None, :], (128, 64)).copy()
    c["ones_f"] = np.ones((128, 128), np.float32)
    return c


CONST_SPECS = None


def build_program(S, stop_after=None, nlayers=DEPTH, debug_out=False):
    nc = bass.Bass("TRN2", target_bir_lowering=False)
    P = Prog(nc)
    NT = S // 512
    NS = S // 128
    TOPK = min(256, S // 4)

    def ext(name, shape, dt=F32):
        return P.dram(name, shape, dt, kind="ExternalInput")

    x = ext("x", [S, D])
    mem = ext("mem", [256, D])
    ln_in_g = ext("ln_in_g", [1, D]); ln_in_b = ext("ln_in_b", [1, D])
    w_in = ext("w_in", [DEPTH, D, WIN])
    a_kv_norm = ext("a_kv_norm", [DEPTH, 128])
    a_w_uk = ext("a_w_uk", [DEPTH, 8, 128, 64]); a_w_uv = ext("a_w_uv", [DEPTH, 8, 128, 64])
    b_l = [ext(n, [DEPTH, 64]) for n in ("b_lq1", "b_lk1", "b_lq2", "b_lk2")]
    b_subln = ext("b_subln", [DEPTH, 128])
    w_o = ext("w_o", [DEPTH, D, D])
    ln_g = [ext(f"ln{i}_g", [DEPTH, D]) for i in (1, 2, 3)]
    ln_b = [ext(f"ln{i}_b", [DEPTH, D]) for i in (1, 2, 3)]
    m_wq = ext("m_wq", [DEPTH, D, D]); m_wkv = ext("m_wkv", [DEPTH, D, 2 * D]); m_wo = ext("m_wo", [DEPTH, D, D])
    router_w = ext("router_w", [DEPTH, D, NE]); router_bias = ext("router_bias", [DEPTH, NE])
    need_moe = stop_after is None or stop_after.startswith("p6")
    if need_moe:
        e_w_gate = ext("e_w_gate", [DEPTH, NE, D, 256]); e_w_up = ext("e_w_up", [DEPTH, NE, D, 256])
        e_w_down = ext("e_w_down", [DEPTH, NE, 256, D])
    s_w_gate = ext("s_w_gate", [DEPTH, D, 256]); s_w_up = ext("s_w_up", [DEPTH, D, 256])
    s_w_down = ext("s_w_down", [DEPTH, 256, D])
    cst = make_consts(S)
    cd = {k: ext("c_" + k, list(v.shape), BF16 if v.dtype == ml_dtypes.bfloat16 else F32) for k, v in cst.items()}
    out = P.dram("out", [S, D], F32, kind="ExternalOutput")

    def scratch(name, shape, dt, ntile):
        v = P.dram(name, shape, dt, kind="ExternalOutput" if debug_out else "Internal")
        hs = [H(f"{name}_{i}") for i in range(ntile)]
        return v.ap, hs
    h_tm, h_tm_h = scratch("h_tm", [S, D], F32, NT)
    hT, hT_h = scratch("hT", [D, S], BF16, NT)
    projT, projT_h = scratch("projT", [33, 64, S], BF16, NT)
    kaT, kaT_h = scratch("kaT", [8, 64, S], BF16, NT)
    va, va_h = scratch("va", [S, 512], BF16, NT)
    vb, vb_h = scratch("vb", [S, 512], BF16, NT)
    widx, widx_h = scratch("widx", [S, 8], F32, NT)
    catT, catT_h = scratch("catT", [D, S], BF16, NT)
    h_bf, h_bf_h = scratch("h_bf", [S, D], BF16, NS)
    C_ = S // 4
    Xg = P.dram("Xg", [NE * C_ + 128, D], BF16).ap
    Yg = P.dram("Yg", [NE * C_ + 128, D], BF16).ap
    TRASH = float(NE * C_)
    I32 = mybir.dt.int32
    U32 = mybir.dt.uint32
    off_t = [P.raw([128, 1], I32, f"off_t{i}") for i in range(16)]
    xs_t = [P.raw([128, D], BF16, f"xs_t{i}") for i in range(2)]
    gt_t = [P.raw([128, D], BF16, f"gt_t{i}") for i in range(8)]
    dbg = {}

    ident = P.sb([128, 128], BF16, "ident"); P.dma(ident, cd["ident"])
    ones_bf = P.sb([128, 128], BF16, "ones_bf"); P.dma(ones_bf, cd["ones_bf"])
    ones_f = P.sb([128, 128], F32, "ones_f"); P.dma(ones_f, cd["ones_f"])

    pbank = [P.ps([128, 512], F32, f"pb{i}") for i in range(8)]

    cnt = {"ln": 0}
    eps_ln = P.sb([128, 2], F32, "eps_ln")
    P.op("pool", "memset", ap=eps_ln[:, 0:1], constant=LN_EPS)
    P.op("pool", "memset", ap=eps_ln[:, 1:2], constant=RMS_EPS)
    LB = {}

    def ln_alloc():
        LB["g_bc"] = P.sb([128, D], F32, "g_bc"); LB["b_bc"] = P.sb([128, D], F32, "b_bc")
        LB["z"] = [P.sb([128, D], F32, f"ln_z{i}") for i in range(2)]
        LB["hb"] = [P.sb([128, D], BF16, f"ln_hb{i}") for i in range(2)]
        LB["st"] = [P.sb([128, 8], F32, f"ln_st{i}") for i in range(2)]
        LB["hTs"] = [P.sb([128, 8, 512], BF16, f"hTs{i}") for i in range(2)]

    def load_ln_params(g_ap, b_ap):
        P.dma(LB["g_bc"], V(g_ap.hs, g_ap.ap.partition_broadcast(128)))
        P.dma(LB["b_bc"], V(b_ap.hs, b_ap.ap.partition_broadcast(128)))

    def ln_tile(z, s128, final):
        i = cnt["ln"]; cnt["ln"] += 1
        stt = LB["st"][i % 2]
        hb = LB["hb"][i % 2]
        ln_junk = hb; g_bc = LB["g_bc"]; b_bc = LB["b_bc"]; hTs = LB["hTs"]
        P.op("act", "activation", out=ln_junk, in_=z, func=AF.Identity, accum_out=stt[:, 0:1])
        P.op("act", "activation", out=ln_junk, in_=z, func=AF.Square, accum_out=stt[:, 1:2])
        P.op("dve", "tensor_scalar", out=stt[:, 2:3], in0=stt[:, 0:1], scalar1=1.0 / D, scalar2=None, op0=ALU.mult)
        P.op("dve", "tensor_tensor", out=stt[:, 3:4], in0=stt[:, 2:3], in1=stt[:, 2:3], op=ALU.mult)
        P.op("dve", "scalar_tensor_tensor", out=stt[:, 4:5], in0=stt[:, 1:2], scalar=1.0 / D, in1=stt[:, 3:4],
             op0=ALU.mult, op1=ALU.subtract)
        P.op("act", "activation", out=stt[:, 6:7], in_=stt[:, 4:5], func=AF.Sqrt, bias=eps_ln[:, 0:1])
        P.op("dve", "reciprocal", out=stt[:, 5:6], in_=stt[:, 6:7])
        P.op("dve", "tensor_scalar", out=z, in0=z, scalar1=stt[:, 2:3], scalar2=stt[:, 5:6],
             op0=ALU.subtract, op1=ALU.mult)
        P.op("pool", "tensor_tensor", out=z, in0=z, in1=g_bc, op=ALU.mult)
        P.op("pool", "tensor_tensor", out=z, in0=z, in1=b_bc, op=ALU.add)
        tt = s128 // 4
        if final:
            P.dma(out[s128 * 128:(s128 + 1) * 128, :], z, q="pool")
        else:
            P.dma(V([h_tm_h[tt]], h_tm[s128 * 128:(s128 + 1) * 128, :]), z, q="pool")
        if final:
            return
        P.op("act", "activation", out=hb, in_=z, func=AF.Copy)
        P.dma(V([h_bf_h[s128]], h_bf[s128 * 128:(s128 + 1) * 128, :]), hb, q="pool")
        pst = pbank[7].bc(BF16)
        hs = hTs[tt % 2]
        for k in range(8):
            P.tr(pst[:, k * 128:(k + 1) * 128], hb[:, k * 128:(k + 1) * 128], ident)
        P.op("act", "activation", out=hs[:, :, (s128 % 4) * 128:(s128 % 4 + 1) * 128],
             in_=pst.re("p (k t) -> p k t", k=8), func=AF.Copy)
        if s128 % 4 == 3:
            P.dma(V([hT_h[tt]], hT.rearrange("(k p) s -> p k s", p=128)[:, :, tt * 512:(tt + 1) * 512]), hs, q="pool")

    p0_mark = P.mark()
    ln_alloc()
    load_ln_params(ln_in_g, ln_in_b)
    for s128 in range(NS):
        z = LB["z"][s128 % 2]
        P.dma(z, x[s128 * 128:(s128 + 1) * 128, :])
        ln_tile(z, s128, False)
    P.release(p0_mark)
    if stop_after == "p0":
        return finish(P, nc)

    bank_i = [0]

    rot = [[0, 1, 2, 3, 4, 5, 6]]

    def nb():
        r = rot[0]
        b = pbank[r[bank_i[0] % len(r)]]
        bank_i[0] += 1
        return b

    ev_i = [0]

    def evac(out, in_, act_only=False):
        ev_i[0] += 1
        if ev_i[0] % 2 or act_only:
            P.op("act", "activation", out=out, in_=in_, func=AF.Copy)
        else:
            P.op("dve", "tensor_copy", out=out, in_=in_)

    def wload(dst, src_ap, nk):
        srcv = src_ap.rearrange("(k p) c -> p k c", p=128)
        for k in range(nk):
            P.dma(dst[:, k, :], V([wH], srcv[:, k, :]), q="pool")
    wH = H("weights")

    def residual_ln(s128, ysrc, final):
        z = LB["z"][cnt["ln"] % 2]
        tt = s128 // 4
        P.dma(z, V([h_tm_h[tt]], h_tm[s128 * 128:(s128 + 1) * 128, :]))
        for hf in range(2):
            P.op("dve", "scalar_tensor_tensor", out=z[:, hf * 512:(hf + 1) * 512], in0=z[:, hf * 512:(hf + 1) * 512],
                 scalar=ALPHA, in1=ysrc[hf], op0=ALU.mult, op1=ALU.add)
        ln_tile(z, s128, final)

    base_mark = P.mark()
    OFF = dict(qa=0, ckv=512, qidx=640, kidx=1152, widx=1216, qb=1224, kb=1736, vb=2248)
    gcols = ([OFF["qa"] + 64 * h for h in range(8)] + [OFF["qidx"] + 64 * h for h in range(8)] + [OFF["kidx"]]
             + [OFF["qb"] + 64 * i for i in range(8)] + [OFF["kb"] + 64 * i for i in range(8)])
    projT_gh = [[H(f"projT_{b}_{t}") for t in range(NT)] for b in range(9)]
    kaT_gh = [[H(f"kaT_{b}_{t}") for t in range(NT)] for b in range(2)]
    catT_gh = [[H(f"catT_{b}_{t}") for t in range(NT)] for b in range(6)]

    for l in range(nlayers):
        lam_init = 0.8 - 0.6 * math.exp(-0.3 * l)
        w_in_sb = P.sb([128, 8, WIN], BF16, "w_in_sb"); wload(w_in_sb, w_in.ap[l], 8)
        wuk_sb = P.sb([128, 8, 64], BF16, "wuk_sb")
        P.dma(wuk_sb, V([wH], a_w_uk.ap[l].rearrange("h c d -> c h d")), q="pool")
        wuv_sb = P.sb([128, 8, 64], BF16, "wuv_sb")
        P.dma(wuv_sb, V([wH], a_w_uv.ap[l].rearrange("h c d -> c h d")), q="pool")
        kvn = P.sb([128, 1], F32, "kvn")
        P.dma(kvn, V([wH], a_kv_norm.ap[l:l + 1, :].rearrange("o c -> c o")))
        hTt = [P.sb([128, 8, 512], BF16, f"hTt{i}") for i in range(2)]
        stg64 = [P.sb([64, 4, 512], BF16, f"stg64_{i}") for i in range(3)]
        stg128 = [P.sb([128, 4, 512], BF16, f"stg128_{i}") for i in range(2)]
        stgw = [P.sb([128, 4, 8], F32, f"stgw_{i}") for i in range(2)]
        csq = P.sb([128, 512], F32, "csq"); rs = P.sb([128, 512], F32, "rs")
        cnT = P.sb([128, 512], BF16, "cnT")
        si = 0
        for tt in range(NT):
            ts = slice(tt * 512, (tt + 1) * 512)
            ht = hTt[tt % 2]
            P.dma(ht, V([hT_h[tt]], hT.rearrange("(k p) s -> p k s", p=128)[:, :, ts]))
            for b in range(9):
                gs = list(range(b * 4, min(b * 4 + 4, 33)))
                stg = stg64[si % 3]; si += 1
                for j, g in enumerate(gs):
                    ps = nb()
                    for k in range(8):
                        P.mm(ps[0:64, :], w_in_sb[:, k, gcols[g]:gcols[g] + 64], ht[:, k, :], k == 0, k == 7)
                    evac(stg[:, j, :], ps[0:64, :])
                P.dma(V([projT_gh[b][tt]], projT[gs[0]:gs[-1] + 1, :, ts].rearrange("g p s -> p g s")),
                      stg[:, 0:len(gs), :], q="pool")
            pc = nb()
            for k in range(8):
                P.mm(pc, w_in_sb[:, k, OFF["ckv"]:OFF["ckv"] + 128], ht[:, k, :], k == 0, k == 7)
            P.op("act", "activation", out=csq, in_=pc, func=AF.Square)
            pss = nb()
            P.mm(pss, ones_f, csq, True, True)
            P.op("act", "activation", out=rs, in_=pss, func=AF.Sqrt, scale=1.0 / 128, bias=eps_ln[:, 1:2])
            P.op("dve", "reciprocal", out=rs, in_=rs)
            P.op("dve", "scalar_tensor_tensor", out=cnT, in0=pc, scalar=kvn[:, 0:1], in1=rs, op0=ALU.mult, op1=ALU.mult)
            for b in range(2):
                stg = stg64[si % 3]; si += 1
                for j in range(4):
                    ps = nb()
                    P.mm(ps[0:64, :], wuk_sb[:, b * 4 + j, :], cnT, True, True)
                    evac(stg[:, j, :], ps[0:64, :])
                P.dma(V([kaT_gh[b][tt]], kaT[b * 4:b * 4 + 4, :, ts].rearrange("g p s -> p g s")), stg, q="pool")
            sg = stg128[0]
            for st_ in range(4):
                ps = nb()
                P.mm(ps, cnT[:, st_ * 128:(st_ + 1) * 128], wuv_sb.re("p h d -> p (h d)"), True, True)
                evac(sg[:, st_, :], ps)
            P.dma(V([va_h[tt]], va[ts, :].rearrange("(t p) c -> p t c", p=128)), sg, q="pool")
            sg = stg128[1]; sw = stgw[tt % 2]
            for st_ in range(4):
                ps = nb()
                for k in range(8):
                    P.mm(ps, ht[:, k, st_ * 128:(st_ + 1) * 128], w_in_sb[:, k, OFF["vb"]:OFF["vb"] + 512], k == 0, k == 7)
                evac(sg[:, st_, :], ps)
                ps = nb()
                for k in range(8):
                    P.mm(ps[:, 0:8], ht[:, k, st_ * 128:(st_ + 1) * 128], w_in_sb[:, k, OFF["widx"]:OFF["widx"] + 8], k == 0, k == 7)
                evac(sw[:, st_, :], ps[:, 0:8])
            P.dma(V([vb_h[tt]], vb[ts, :].rearrange("(t p) c -> p t c", p=128)), sg, q="pool")
            P.dma(V([widx_h[tt]], widx[ts, :].rearrange("(t p) c -> p t c", p=128)), sw, q="pool")
        P.release(base_mark)
        if stop_after == f"p1_{l}":
            return finish(P, nc)
        cq = {}
        kidx_sb = P.sb([64, S], BF16, "kidx_sb")
        P.dma(kidx_sb, V(projT_gh[4], projT[16, :, :]))
        ka_ring = [P.sb([69, S], BF16, f"ka{i}") for i in range(2)]
        for kr in ka_ring:
            P.dma(kr[64:65, :], cd["kaug"][2:3, :])
            P.dma(kr[65:69, :], cd["kaug"][0:4, :])
        va_sb = P.sb([128, NS, 8, 65], BF16, "va_sb")
        P.op("pool", "memset", ap=va_sb[:, :, :, 64:65], constant=1.0)
        for t in range(NS):
            P.dma(va_sb[:, t, :, 0:64], V([va_h[t // 4]], va[t * 128:(t + 1) * 128, :].rearrange("p (h d) -> p h d", h=8)))
        widx_sb = P.sb([128, NS, 8], F32, "widx_sb")
        P.dma(widx_sb, V(widx_h, widx.rearrange("(t p) c -> p t c", p=128)))
        isc = P.sb([128, S], F32, "isc")
        junk = P.sb([128, S], BF16, "junkb")
        maskq = [P.sb([128, S], BF16, f"maskq{i}") for i in range(1)]
        rot[0] = [0, 1, 2, 3, 4]
        maskT = P.sb([128, NS, 512], BF16, "maskT")
        posrow = P.sb([128, S], BF16, "posrow"); P.dma(posrow, cd["posrow"], q="pool")
        qpos = P.sb([128, NS], F32, "qpos"); P.dma(qpos, cd["qpos"])
        slope8 = P.sb([128, 8], F32, "slope8"); P.dma(slope8, cd["slope8"])
        pow2 = P.sb([128, NBIS], F32, "pow2"); P.dma(pow2, cd["pow2"])
        dmask = P.sb([128, 128], F32, "dmask"); P.dma(dmask, cd["dmask"])
        dfix_a = P.sb([128, 8, 128], BF16, "dfix_a"); P.dma(dfix_a, cd["dfix_a"])
        zero_c = P.sb([128, 1], F32, "zero_c"); P.op("pool", "memset", ap=zero_c, constant=0.0)
        negc = P.sb([128, 1], F32, "negc"); P.op("pool", "memset", ap=negc, constant=-30000.0)
        qi_ring = [P.sb([64, 8, 512], BF16, f"qi{i}") for i in range(1)]
        q_ring = [P.sb([69, 8, 512], BF16, f"qa{i}") for i in range(1)]
        rl_ring = [P.sb([128, 512], F32, f"rl{i}") for i in range(2)]
        e_ring = [P.sb([128, 512], BF16, f"e{i}") for i in range(4)]
        bst = [P.sb([128, 8], F32, f"bst{i}") for i in range(2)]
        steps = P.sb([128, NBIS], F32, "steps")
        shq = P.sb([128, 8], BF16, "shq")
        shT = P.sb([8, 512], BF16, "shT")
        osb = P.sb([65, 512], F32, "osb")
        oa_stg = [P.sb([64, 4, 512], BF16, f"oa_stg{i}") for i in range(2)]
        ei = 0; hh = 0; sti = 0
        for qt in range(NT):
            ts = slice(qt * 512, (qt + 1) * 512)
            qi = qi_ring[0]; qs = q_ring[0]
            P.dma(qi, V(projT_gh[2] + projT_gh[3], projT[8:16, :, ts].rearrange("g p s -> p g s")))
            P.dma(qs[0:64, :, :], V(projT_gh[0] + projT_gh[1], projT[0:8, :, ts].rearrange("g p s -> p g s")))
            P.dma(qs[65:69, :, :], V(cd["qaug_a"].hs, cd["qaug_a"].ap[:, :, ts].rearrange("h r s -> r h s")))
            for st_ in range(4):
                blk = qt * 4 + st_
                q0 = blk * 128
                nkeys = q0 + 128
                mq = maskq[0]
                b_ = bst[blk % 2]
                for h in range(8):
                    for kc in range((nkeys + 511) // 512):
                        w = min(512, nkeys - kc * 512)
                        ps = nb()
                        P.mm(ps[:, 0:w], qi[:, h, st_ * 128:(st_ + 1) * 128], kidx_sb[:, kc * 512:kc * 512 + w], True, True)
                        rl = rl_ring[ei % 2]; ei += 1
                        P.op("act", "activation", out=rl[:, 0:w], in_=ps[:, 0:w], func=AF.Relu)
                        dst = isc[:, kc * 512:kc * 512 + w]
                        if h == 0:
                            P.op("dve", "tensor_scalar", out=dst, in0=rl[:, 0:w], scalar1=widx_sb[:, blk, 0:1], scalar2=None, op0=ALU.mult)
                        else:
                            P.op("dve", "scalar_tensor_tensor", out=dst, in0=rl[:, 0:w], scalar=widx_sb[:, blk, h:h + 1], in1=dst,
                                 op0=ALU.mult, op1=ALU.add)
                P.op("dve", "tensor_tensor", out=isc[:, q0:q0 + 128], in0=isc[:, q0:q0 + 128], in1=dmask, op=ALU.add)
                tcol = b_[:, 4:5]
                if q0 < TOPK:
                    P.op("dve", "memset", ap=tcol, constant=-1.0e29)
                else:
                    P.op("dve", "tensor_reduce", out=b_[:, 0:1], in_=isc[:, 0:nkeys], axis=AX.X, op=ALU.max)
                    P.op("dve", "tensor_reduce", out=b_[:, 1:2], in_=isc[:, 0:q0], axis=AX.X, op=ALU.min)
                    P.op("dve", "tensor_tensor", out=b_[:, 2:3], in0=b_[:, 0:1], in1=b_[:, 1:2], op=ALU.subtract)
                    P.op("dve", "tensor_tensor", out=b_[:, 3:4], in0=b_[:, 0:1], in1=b_[:, 1:2], op=ALU.add)
                    P.op("dve", "tensor_scalar", out=tcol, in0=b_[:, 3:4], scalar1=0.5, scalar2=None, op0=ALU.mult)
                    P.op("dve", "tensor_scalar", out=steps, in0=pow2, scalar1=b_[:, 2:3], scalar2=None, op0=ALU.mult)
                    for n in range(NBIS):
                        P.op("dve", "tensor_scalar", out=junk[:, 0:nkeys], in0=isc[:, 0:nkeys], scalar1=tcol, scalar2=zero_c[:, 0:1],
                             op0=ALU.is_ge, op1=ALU.add, accum_out=b_[:, 5:6])
                        P.op("dve", "tensor_scalar", out=b_[:, 6:7], in0=b_[:, 5:6], scalar1=TOPK - 0.5, scalar2=0.5,
                             op0=ALU.is_gt, op1=ALU.subtract)
                        P.op("dve", "scalar_tensor_tensor", out=tcol, in0=b_[:, 6:7], scalar=steps[:, n:n + 1], in1=tcol,
                             op0=ALU.mult, op1=ALU.add)
                P.op("dve", "tensor_scalar", out=mq[:, 0:nkeys], in0=isc[:, 0:nkeys], scalar1=tcol, scalar2=None, op0=ALU.is_ge)
                P.op("dve", "tensor_tensor", out=junk[:, 0:nkeys], in0=mq[:, 0:nkeys], in1=posrow[:, 0:nkeys], op=ALU.mult)
                P.op("dve", "tensor_reduce", out=b_[:, 7:8], in_=junk[:, 0:nkeys], axis=AX.X, op=ALU.max)
                P.op("dve", "tensor_tensor", out=b_[:, 3:4], in0=qpos[:, blk:blk + 1], in1=b_[:, 7:8], op=ALU.subtract)
                P.op("dve", "tensor_scalar", out=shq, in0=slope8, scalar1=b_[:, 3:4], scalar2=None, op0=ALU.mult)
                pst = nb().bc(BF16)
                P.tr(pst[0:8, 0:128], shq, ident)
                evac(shT[:, st_ * 128:(st_ + 1) * 128], pst[0:8, 0:128], True)
                for kb0 in range(0, blk + 1, 8):
                    n_ = min(8, blk + 1 - kb0)
                    pst = nb().bc(BF16)
                    for j in range(n_):
                        P.tr(pst[:, j * 128:(j + 1) * 128], mq[:, (kb0 + j) * 128:(kb0 + j + 1) * 128], ident)
                    P.op("act", "activation", out=maskT[:, kb0:kb0 + n_, st_ * 128:(st_ + 1) * 128],
                         in_=pst[:, 0:n_ * 128].re("p (k t) -> p k t", k=n_), func=AF.Identity, scale=30000.0, bias=negc[:, 0:1])
            for h in range(8):
                P.dma(qs[64:65, h, :], shT[h:h + 1, :])
            nkb = 4 * (qt + 1)
            plist = []
            for h in range(8):
                for kb in range(nkb):
                    plist.append((h, kb))
            st8 = {}

            def s_part(h, kb, qt=qt, nkb=nkb, qs=qs, ts=ts):
                if kb == 0:
                    ka = ka_ring[st8["hh"] % 2]; st8["hh"] += 1
                    P.dma(ka[0:64, 0:nkb * 128], V(kaT_gh[h // 4], kaT[h, :, 0:nkb * 128]))
                    st8[("ka", h)] = ka
                ka = st8[("ka", h)]
                c0 = max(0, kb - 4 * qt) * 128
                ps = nb()
                kslc = ka[0:69, kb * 128:(kb + 1) * 128]
                if kb >= 4 * qt:
                    P.mm(ps[:, c0:c0 + 128], kslc, qs[0:69, h, c0:c0 + 128], True, False)
                    P.mm(ps[:, c0:c0 + 128], ident, dfix_a[:, h, :], False, False)
                    P.mm(ps[:, c0:c0 + 128], ident, maskT[:, kb, c0:c0 + 128], False, True)
                    if c0 + 128 < 512:
                        P.mm(ps[:, c0 + 128:512], kslc, qs[0:69, h, c0 + 128:512], True, False)
                        P.mm(ps[:, c0 + 128:512], ident, maskT[:, kb, c0 + 128:512], False, True)
                else:
                    P.mm(ps[:, c0:512], kslc, qs[0:69, h, c0:512], True, False)
                    P.mm(ps[:, c0:512], ident, maskT[:, kb, c0:512], False, True)
                E = e_ring[st8["sti"] % 4]; st8["sti"] += 1
                P.op("act", "activation", out=E[:, c0:512], in_=ps[:, c0:512], func=AF.Exp, scale=0.125)
                st8[("E", h, kb)] = E

            def r_part(h, kb, qt=qt, nkb=nkb, ts=ts):
                c0 = max(0, kb - 4 * qt) * 128
                E = st8.pop(("E", h, kb))
                po = pbank[5 + (h % 2)]
                P.mm(po[0:65, c0:512], va_sb[:, kb, h, :], E[:, c0:512], kb == 0, kb == nkb - 1)
                if kb == nkb - 1:
                    P.op("act", "activation", out=osb, in_=po[0:65, :], func=AF.Copy)
                    P.op("dve", "reciprocal", out=osb[64:65, :], in_=osb[64:65, :])
                    pb_ = pbank[7]
                    P.mm(pb_[0:64, :], ones_f[64:65, 0:64], osb[64:65, :], True, True)
                    og = oa_stg[(h // 4) % 2]
                    P.op("dve", "tensor_tensor", out=og[:, h % 4, :], in0=osb[0:64, :], in1=pb_[0:64, :], op=ALU.mult)
                    if h % 4 == 3:
                        h0 = h - 3
                        P.dma(V([catT_gh[h // 4][qt]], catT[h0 * 64:(h0 + 4) * 64, ts].rearrange("(g p) s -> p g s", p=64)), og, q="pool")
            st8["hh"] = hh; st8["sti"] = sti
            LA = 2
            for i in range(len(plist) + LA):
                if i < len(plist):
                    s_part(*plist[i])
                if i - LA >= 0:
                    r_part(*plist[i - LA])
            hh = st8["hh"]; sti = st8["sti"]
        P.release(base_mark)
        if stop_after == f"p2_{l}":
            return finish(P, nc)
        rot[0] = [0, 1, 2]
        kb_ring = [P.sb([68, S], BF16, f"kbr{i}") for i in range(4)]
        for kr in kb_ring:
            P.dma(kr[64:68, :], cd["kaug"][0:4, :])
        vb_sb = P.sb([128, NS, 512], BF16, "vb_sb")
        P.dma(vb_sb, V(vb_h, vb.rearrange("(t p) c -> p t c", p=128)))
        qb = P.sb([68, 8, 512], BF16, "qb")
        dfix_b = P.sb([128, 4, 128], BF16, "dfix_b"); P.dma(dfix_b, cd["dfix_b"])
        lt = [P.sb([128, 64], F32, f"lt{i}") for i in range(4)]
        for i in range(4):
            P.dma(lt[i], V(b_l[i].hs, b_l[i].ap[l:l + 1, :].partition_broadcast(128)))
        lsc = P.sb([128, 8], F32, "lsc")
        for j in range(2):
            P.op("dve", "tensor_tensor", out=lt[2 * j], in0=lt[2 * j], in1=lt[2 * j + 1], op=ALU.mult)
            P.op("dve", "tensor_reduce", out=lsc[:, j:j + 1], in_=lt[2 * j], axis=AX.X, op=ALU.add)
            P.op("act", "activation", out=lsc[:, 2 + j:3 + j], in_=lsc[:, j:j + 1], func=AF.Exp)
        P.op("dve", "tensor_tensor", out=lsc[:, 4:5], in0=lsc[:, 3:4], in1=lsc[:, 2:3], op=ALU.subtract)
        P.op("dve", "tensor_scalar", out=lsc[:, 5:6], in0=lsc[:, 4:5], scalar1=-lam_init, scalar2=None, op0=ALU.add)
        subc = P.sb([128, 1], F32, "subc")
        P.dma(subc, V([wH], b_subln.ap[l:l + 1, :].rearrange("o c -> c o")))
        P.op("dve", "tensor_scalar", out=subc, in0=subc, scalar1=1.0 - lam_init, scalar2=None, op0=ALU.mult)
        e_ring = [P.sb([128, 512], BF16, f"eb{i}") for i in range(4)]
        r_ = [P.sb([128, 512], F32, f"rr{i}") for i in range(4)]
        ob_stg = [P.sb([128, 512], BF16, f"ob_stg{i}") for i in range(2)]
        ki = 0; sti = 0
        for qt in range(NT):
            ts = slice(qt * 512, (qt + 1) * 512)
            nkb = 4 * (qt + 1)
            P.dma(qb[0:64, :, :], V(projT_gh[4] + projT_gh[5] + projT_gh[6], projT[17:25, :, ts].rearrange("g p s -> p g s")))
            for g in range(8):
                P.dma(qb[64:68, g, :], V(cd["qaug_b"].hs, cd["qaug_b"].ap[g // 2, :, ts]))
            steps = []
            for h in range(4):
                for kb in range(nkb):
                    for j in range(2):
                        steps.append((h, kb, j))
            st8 = {"ki": ki, "sti": sti}

            def s_part(h, kb, j, qt=qt, nkb=nkb):
                if kb == 0:
                    K_ = kb_ring[st8["ki"] % 4]; st8["ki"] += 1
                    P.dma(K_[0:64, 0:nkb * 128], V(projT_gh[6] + projT_gh[7] + projT_gh[8], projT[25 + 2 * h + j, :, 0:nkb * 128]))
                    st8[("K", h, j)] = K_
                K_ = st8[("K", h, j)]
                c0 = max(0, kb - 4 * qt) * 128
                ps = nb()
                kslc = K_[0:68, kb * 128:(kb + 1) * 128]
                if kb >= 4 * qt:
                    P.mm(ps[:, c0:c0 + 128], kslc, qb[0:68, 2 * h + j, c0:c0 + 128], True, False)
                    P.mm(ps[:, c0:c0 + 128], ident, dfix_b[:, h, :], False, True)
                    if c0 + 128 < 512:
                        P.mm(ps[:, c0 + 128:512], kslc, qb[0:68, 2 * h + j, c0 + 128:512], True, True)
                else:
                    P.mm(ps[:, c0:512], kslc, qb[0:68, 2 * h + j, c0:512], True, True)
                E = e_ring[st8["sti"] % 4]; st8["sti"] += 1
                P.op("act", "activation", out=E[:, c0:512], in_=ps[:, c0:512], func=AF.Exp, scale=0.125)
                st8[("E", h, kb, j)] = E

            def r_part(h, kb, j, qt=qt, nkb=nkb, ts=ts):
                c0 = max(0, kb - 4 * qt) * 128
                E = st8.pop(("E", h, kb, j))
                O_ = [pbank[3], pbank[4]]; Dn = [pbank[5], pbank[6]]
                P.mm(O_[j][:, c0:512], vb_sb[:, kb, h * 128:(h + 1) * 128], E[:, c0:512], kb == 0, kb == nkb - 1)
                P.mm(Dn[j][:, c0:512], ones_bf, E[:, c0:512], kb == 0, kb == nkb - 1)
                if kb == nkb - 1 and j == 1:
                    for jj in range(2):
                        P.op("dve", "reciprocal", out=r_[jj], in_=Dn[jj])
                        P.op("dve", "tensor_tensor", out=r_[jj], in0=O_[jj], in1=r_[jj], op=ALU.mult)
                    P.op("dve", "scalar_tensor_tensor", out=r_[2], in0=r_[1], scalar=lsc[:, 5:6], in1=r_[0], op0=ALU.mult, op1=ALU.add)
                    P.op("act", "activation", out=r_[3], in_=r_[2], func=AF.Square)
                    pss = pbank[7]
                    P.mm(pss, ones_f, r_[3], True, True)
                    P.op("act", "activation", out=r_[3], in_=pss, func=AF.Sqrt, scale=1.0 / 128, bias=eps_ln[:, 1:2])
                    P.op("dve", "reciprocal", out=r_[3], in_=r_[3])
                    og = ob_stg[h % 2]
                    P.op("dve", "scalar_tensor_tensor", out=og, in0=r_[2], scalar=subc[:, 0:1], in1=r_[3], op0=ALU.mult, op1=ALU.mult)
                    P.dma(V([catT_gh[2 + h][qt]], catT[512 + h * 128:512 + (h + 1) * 128, ts]), og, q="pool")
            LA = 2
            for i in range(len(steps) + LA):
                if i < len(steps):
                    s_part(*steps[i])
                if i - LA >= 0:
                    r_part(*steps[i - LA])
            ki = st8["ki"]; sti = st8["sti"]
        P.release(base_mark)
        if stop_after == f"p3_{l}":
            return finish(P, nc)
        rot[0] = [0, 1, 2, 3, 4, 5, 6]
        ln_alloc()
        load_ln_params(V(ln_g[0].hs, ln_g[0].ap[l:l + 1, :]), V(ln_b[0].hs, ln_b[0].ap[l:l + 1, :]))
        wo_sb = P.sb([128, 8, D], BF16, "wo_sb"); wload(wo_sb, w_o.ap[l], 8)
        ct_ring = [P.sb([128, 8, 512], BF16, f"ct{i}") for i in range(2)]
        for tt in range(NT):
            ts = slice(tt * 512, (tt + 1) * 512)
            ct = ct_ring[tt % 2]
            P.dma(ct, V([g_[tt] for g_ in catT_gh], catT.rearrange("(k p) s -> p k s", p=128)[:, :, ts]))
            for st_ in range(4):
                ys = [nb(), nb()]
                for hf in range(2):
                    for k in range(8):
                        P.mm(ys[hf], ct[:, k, st_ * 128:(st_ + 1) * 128], wo_sb[:, k, hf * 512:(hf + 1) * 512], k == 0, k == 7)
                residual_ln(tt * 4 + st_, ys, False)
        P.release(base_mark)
        if stop_after == f"p4_{l}":
            return finish(P, nc)
        rot[0] = [0, 1, 2, 3, 4]
        ln_alloc()
        load_ln_params(V(ln_g[1].hs, ln_g[1].ap[l:l + 1, :]), V(ln_b[1].hs, ln_b[1].ap[l:l + 1, :]))
        wq_sb = P.sb([128, 8, D], BF16, "wq_sb"); wload(wq_sb, m_wq.ap[l], 8)
        wkv_sb = P.sb([128, 8, 2 * D], BF16, "wkv_sb"); wload(wkv_sb, m_wkv.ap[l], 8)
        wo2_sb = P.sb([128, 8, D], BF16, "wo2_sb"); wload(wo2_sb, m_wo.ap[l], 8)
        mem_b = P.sb([128, 2, D], BF16, "mem_b")
        P.dma(mem_b, V(mem.hs, mem.ap.rearrange("(t p) d -> p t d", p=128)), q="pool")
        memT = P.sb([128, 8, 256], BF16, "memT")
        for t in range(2):
            pst = nb().bc(BF16)
            for k in range(8):
                P.tr(pst[:, k * 128:(k + 1) * 128], mem_b[:, t, k * 128:(k + 1) * 128], ident)
            evac(memT[:, :, t * 128:(t + 1) * 128], pst.re("p (k t) -> p k t", k=8))
        KT = P.sb([128, 8, 256], BF16, "KT")
        for dc in range(8):
            ps = nb()
            for k in range(8):
                P.mm(ps[:, 0:256], wkv_sb[:, k, dc * 128:(dc + 1) * 128], memT[:, k, :], k == 0, k == 7)
            evac(KT[:, dc, :], ps[:, 0:256])
        Vm = P.sb([128, 2, D], BF16, "Vm")
        for t in range(2):
            for hf in range(2):
                ps = nb()
                for k in range(8):
                    P.mm(ps, memT[:, k, t * 128:(t + 1) * 128], wkv_sb[:, k, D + hf * 512:D + (hf + 1) * 512], k == 0, k == 7)
                evac(Vm[:, t, hf * 512:(hf + 1) * 512], ps)
        ht_ring = [P.sb([128, 8, 512], BF16, f"ht5_{i}") for i in range(2)]
        QT = P.sb([128, 8, 512], BF16, "QT"); OT = P.sb([128, 8, 512], BF16, "OT")
        e5 = [P.sb([128, 2, 512], BF16, f"e5_{i}") for i in range(2)]
        rd = P.sb([128, 512], F32, "rd5")
        for tt in range(NT):
            ts = slice(tt * 512, (tt + 1) * 512)
            ht = ht_ring[tt % 2]
            P.dma(ht, V([hT_h[tt]], hT.rearrange("(k p) s -> p k s", p=128)[:, :, ts]))
            for dc in range(8):
                ps = nb()
                for k in range(8):
                    P.mm(ps, wq_sb[:, k, dc * 128:(dc + 1) * 128], ht[:, k, :], k == 0, k == 7)
                evac(QT[:, dc, :], ps)
            for h in range(4):
                E = e5[h % 2]
                for mb in range(2):
                    ps = nb()
                    for dc in range(2):
                        P.mm(ps, KT[:, 2 * h + dc, mb * 128:(mb + 1) * 128], QT[:, 2 * h + dc, :], dc == 0, dc == 1)
                    P.op("act", "activation", out=E[:, mb, :], in_=ps, func=AF.Exp, scale=1.0 / 16)
                den = pbank[5]
                for mb in range(2):
                    P.mm(den, ones_bf, E[:, mb, :], mb == 0, mb == 1)
                P.op("dve", "reciprocal", out=rd, in_=den)
                for dvc in range(2):
                    po = pbank[6] if dvc == 0 else nb()
                    for mb in range(2):
                        P.mm(po, Vm[:, mb, h * 256 + dvc * 128:h * 256 + (dvc + 1) * 128], E[:, mb, :], mb == 0, mb == 1)
                    P.op("dve", "tensor_tensor", out=OT[:, 2 * h + dvc, :], in0=po, in1=rd, op=ALU.mult)
            for st_ in range(4):
                ys = [nb(), nb()]
                for hf in range(2):
                    for k in range(8):
                        P.mm(ys[hf], OT[:, k, st_ * 128:(st_ + 1) * 128], wo2_sb[:, k, hf * 512:(hf + 1) * 512], k == 0, k == 7)
                residual_ln(tt * 4 + st_, ys, False)
        P.release(base_mark)
        if stop_after == f"p5_{l}":
            return finish(P, nc)
        rot[0] = [0, 1, 2, 3, 4, 5, 6]
        last = (l == nlayers - 1)
        ln_alloc()
        load_ln_params(V(ln_g[2].hs, ln_g[2].ap[l:l + 1, :]), V(ln_b[2].hs, ln_b[2].ap[l:l + 1, :]))
        NCS = C_ // 128
        WS = min(512, C_)
        rw_sb = P.sb([128, 8, NE], BF16, "rw_sb"); wload(rw_sb, router_w.ap[l], 8)
        rbias = P.sb([128, NE], F32, "rbias")
        P.dma(rbias, V(router_bias.hs, router_bias.ap[l:l + 1, :].partition_broadcast(128)))
        iota64 = P.sb([128, NE], F32, "iota64"); P.dma(iota64, cd["iota64"])
        ltri = P.sb([128, 128], BF16, "ltri"); P.dma(ltri, cd["ltri"])
        M_bf = P.sb([128, NS, NE], BF16, "M_bf")
        gk = P.sb([128, NS, 8], F32, "gk")
        offf = P.sb([128, NS, 8], F32, "offf")
        rt = [P.sb([128, NE], F32, f"rt{i}") for i in range(6)]
        rsm = P.sb([128, 16], F32, "rsm")
        idx8 = P.sb([128, 8], U32, "idx8"); idxf = P.sb([128, 8], F32, "idxf"); slotf = P.sb([128, 8], F32, "slotf")
        ovf = P.sb([128, 8], F32, "ovf")
        oh3 = P.sb([128, 8, NE], F32, "oh3"); oh3b = P.sb([128, 8, NE], F32, "oh3b")
        ht_ring = [P.sb([128, 8, 512], BF16, f"ht6_{i}") for i in range(2)]
        Xg_hs = []
        Yg_hs = [[H(f"Yg_{e}_{t}") for t in range(NCS)] for e in range(NE)]
        ztile = P.sb([128, D], BF16, "ztile")
        P.op("pool", "memset", ap=ztile, constant=0.0)
        yg_trash_h = H("Yg_trash")
        P.dma(V([yg_trash_h], Yg[NE * C_:NE * C_ + 128, :]), ztile, q="pool")
        oi = 0
        for tt in range(NT):
            ht = ht_ring[tt % 2]
            P.dma(ht, V([hT_h[tt]], hT.rearrange("(k p) s -> p k s", p=128)[:, :, tt * 512:(tt + 1) * 512]))
            for st_ in range(4):
                s_ = tt * 4 + st_
                ps = nb()
                for k in range(8):
                    P.mm(ps[:, 0:NE], ht[:, k, st_ * 128:(st_ + 1) * 128], rw_sb[:, k, :], k == 0, k == 7)
                P.op("act", "activation", out=rt[0], in_=ps[:, 0:NE], func=AF.Sigmoid)
                P.op("dve", "tensor_tensor", out=rt[1], in0=rt[0], in1=rbias, op=ALU.add)
                P.op("dve", "max", out=rsm[:, 0:8], in_=rt[1])
                P.op("dve", "max_index", out=idx8, in_max=rsm[:, 0:8], in_values=rt[1])
                P.op("dve", "tensor_copy", out=idxf, in_=idx8)
                P.op("dve", "tensor_scalar", out=rt[2], in0=rt[1], scalar1=rsm[:, 7:8], scalar2=None, op0=ALU.is_ge)
                P.op("dve", "tensor_copy", out=M_bf[:, s_, :], in_=rt[2])
                P.op("dve", "tensor_tensor", out=rt[2], in0=rt[2], in1=rt[0], op=ALU.mult)
                P.op("dve", "tensor_reduce", out=rsm[:, 8:9], in_=rt[2], axis=AX.X, op=ALU.add)
                P.op("dve", "reciprocal", out=rsm[:, 9:10], in_=rsm[:, 8:9])
                P.op("dve", "tensor_scalar", out=rt[2], in0=rt[2], scalar1=rsm[:, 9:10], scalar2=None, op0=ALU.mult)
                P.op("dve", "tensor_scalar", out=rt[3], in0=rt[2], scalar1=2.5, scalar2=None, op0=ALU.mult)
                pc = nb()
                P.mm(pc[:, 0:NE], ltri, M_bf[:, s_, :], True, s_ == 0)
                for j in range(s_):
                    P.mm(pc[:, 0:NE], ones_bf, M_bf[:, j, :], False, j == s_ - 1)
                P.op("act", "activation", out=rt[4], in_=pc[:, 0:NE], func=AF.Copy)
                io_b = V(iota64.hs, iota64.ap.unsqueeze(1).broadcast_to([128, 8, NE]))
                ix_b = V(idxf.hs, idxf.ap.unsqueeze(2).broadcast_to([128, 8, NE]))
                cu_b = V(rt[4].hs, rt[4].ap.unsqueeze(1).broadcast_to([128, 8, NE]))
                g_b = V(rt[3].hs, rt[3].ap.unsqueeze(1).broadcast_to([128, 8, NE]))
                P.op("dve", "tensor_tensor", out=oh3, in0=io_b, in1=ix_b, op=ALU.is_equal)
                P.op("dve", "tensor_tensor", out=oh3b, in0=oh3, in1=cu_b, op=ALU.mult)
                P.op("dve", "tensor_reduce", out=slotf, in_=oh3b, axis=AX.X, op=ALU.add)
                P.op("dve", "tensor_tensor", out=oh3b, in0=oh3, in1=g_b, op=ALU.mult)
                P.op("dve", "tensor_reduce", out=gk[:, s_, :], in_=oh3b, axis=AX.X, op=ALU.add)
                P.op("dve", "tensor_scalar", out=ovf, in0=slotf, scalar1=C_ - 0.5, scalar2=None, op0=ALU.is_lt)
                P.op("dve", "tensor_tensor", out=gk[:, s_, :], in0=gk[:, s_, :], in1=ovf, op=ALU.mult)
                P.op("dve", "scalar_tensor_tensor", out=offf[:, s_, :], in0=idxf, scalar=float(C_), in1=slotf, op0=ALU.mult, op1=ALU.add)
                P.op("dve", "tensor_scalar", out=offf[:, s_, :], in0=offf[:, s_, :], scalar1=-TRASH, scalar2=None, op0=ALU.add)
                P.op("dve", "tensor_tensor", out=offf[:, s_, :], in0=offf[:, s_, :], in1=ovf, op=ALU.mult)
                P.op("dve", "tensor_scalar", out=offf[:, s_, :], in0=offf[:, s_, :], scalar1=TRASH, scalar2=None, op0=ALU.add)
                xs = xs_t[s_ % 2]
                P.dma(xs, V([h_bf_h[s_]], h_bf[s_ * 128:(s_ + 1) * 128, :]))
                for k in range(8):
                    ot = off_t[oi % 16]; oi += 1
                    P.op("dve", "tensor_copy", out=ot, in_=offf[:, s_, k:k + 1])
                    hx = H(f"Xg_{s_}_{k}"); Xg_hs.append(hx)
                    P.add("pool", (lambda e, ot=ot, xs=xs: e.indirect_dma_start(
                        out=Xg[:, :], out_offset=bass.IndirectOffsetOnAxis(ap=ot.ap[:, 0:1], axis=0),
                        in_=xs.ap[:, :], in_offset=None, bounds_check=None)),
                        [xs, ot], [V([hx], Xg)], dma=True)
        wg_r = [P.sb([128, 8, 256], BF16, f"wg{i}") for i in range(2)]
        wu_r = [P.sb([128, 8, 256], BF16, f"wu{i}") for i in range(2)]
        wd_r = [P.sb([128, 2, D], BF16, f"wd{i}") for i in range(2)]
        sg_r = [P.sb([128, 512], F32, f"sg{i}") for i in range(2)]
        at_r = [P.sb([128, 2, 512], BF16, f"at{i}") for i in range(2)]
        stageb_mark = P.mark()
        xe_r = [P.sb([128, NCS, D], BF16, f"xe{i}") for i in range(2)]
        xT_r = [P.sb([128, 8, C_], BF16, f"xT{i}") for i in range(2)]
        ysb_r = [P.sb([128, D], BF16, f"ysb{i}") for i in range(4)]
        yi = 0

        pyi = [0]
        rot[0] = [4, 5, 6, 7]

        def expert_block(wg, wu, wd, rhs_of, n_sub, emit_out, ai0):
            ai = ai0
            c0 = 0
            while c0 < n_sub * 128:
                w = min(512, n_sub * 128 - c0)
                gps = [pbank[0], pbank[1]]
                ups = [pbank[2], pbank[3]]
                for m in range(2):
                    for k in range(8):
                        P.mm(gps[m][:, 0:w], wg[:, k, m * 128:(m + 1) * 128], rhs_of(k, c0, w), k == 0, k == 7)
                for m in range(2):
                    for k in range(8):
                        P.mm(ups[m][:, 0:w], wu[:, k, m * 128:(m + 1) * 128], rhs_of(k, c0, w), k == 0, k == 7)
                at = at_r[ai % 2]
                for m in range(2):
                    sg = sg_r[m]
                    P.op("act", "activation", out=sg[:, 0:w], in_=gps[m][:, 0:w], func=AF.Silu)
                    P.op("dve", "tensor_tensor", out=at[:, m, 0:w], in0=sg[:, 0:w], in1=ups[m][:, 0:w], op=ALU.mult)
                for st_ in range(w // 128):
                    pyi[0] += 1
                    pys = [pbank[4 + 2 * (pyi[0] % 2)], pbank[5 + 2 * (pyi[0] % 2)]]
                    for hf in range(2):
                        for m in range(2):
                            P.mm(pys[hf], at[:, m, st_ * 128:(st_ + 1) * 128], wd[:, m, hf * 512:(hf + 1) * 512], m == 0, m == 1)
                    emit_out(c0 // 128 + st_, pys)
                ai += 1
                c0 += w
            return ai

        ai = 0
        for e in range(NE):
            wg = wg_r[e % 2]; wu = wu_r[e % 2]; wd = wd_r[e % 2]
            if e == 0:
                wload(wg, e_w_gate.ap[l, 0], 8); wload(wu, e_w_up.ap[l, 0], 8); wload(wd, e_w_down.ap[l, 0], 2)
            if e + 1 < NE:
                wload(wg_r[(e + 1) % 2], e_w_gate.ap[l, e + 1], 8); wload(wu_r[(e + 1) % 2], e_w_up.ap[l, e + 1], 8)
                wload(wd_r[(e + 1) % 2], e_w_down.ap[l, e + 1], 2)
            xe = xe_r[e % 2]; xT = xT_r[e % 2]
            if e == 0:
                P.dma(xe, V(Xg_hs, Xg[0:C_, :].rearrange("(t p) d -> p t d", p=128)))
            if e + 1 < NE:
                P.dma(xe_r[(e + 1) % 2], V(Xg_hs, Xg[(e + 1) * C_:(e + 2) * C_, :].rearrange("(t p) d -> p t d", p=128)))
            for t in range(NCS):
                pst = nb().bc(BF16)
                for k in range(8):
                    P.tr(pst[:, k * 128:(k + 1) * 128], xe[:, t, k * 128:(k + 1) * 128], ident)
                evac(xT[:, :, t * 128:(t + 1) * 128], pst.re("p (k t) -> p k t", k=8))

            def emit_out(sub, pys, e=e):
                nonlocal yi
                ysb = ysb_r[yi % 4]; yi += 1
                P.op("act", "activation", out=ysb[:, 0:512], in_=pys[0], func=AF.Copy)
                P.op("dve", "tensor_copy", out=ysb[:, 512:1024], in_=pys[1])
                P.dma(V([Yg_hs[e][sub]], Yg[e * C_ + sub * 128:e * C_ + (sub + 1) * 128, :]), ysb, q="sp")
            ai = expert_block(wg, wu, wd, lambda k, c0, w, xT=xT: xT[:, k, c0:c0 + w], NCS, emit_out, ai)
        P.release(stageb_mark)
        wg = wg_r[0]; wu = wu_r[0]; wd = wd_r[0]
        wload(wg, s_w_gate.ap[l], 8); wload(wu, s_w_up.ap[l], 8); wload(wd, s_w_down.ap[l], 2)
        yacc_r = [P.sb([128, D], F32, f"yacc{i}") for i in range(3)]
        allY = [h_ for row in Yg_hs for h_ in row] + [yg_trash_h]
        gi = 0
        for tt in range(NT):
            ht = ht_ring[tt % 2]
            P.dma(ht, V([hT_h[tt]], hT.rearrange("(k p) s -> p k s", p=128)[:, :, tt * 512:(tt + 1) * 512]))

            def emit_out(sub, pys, tt=tt):
                nonlocal gi, oi, yi
                s_ = tt * 4 + sub
                ya = yacc_r[yi % 3]; yi += 1
                P.op("act", "activation", out=ya[:, 0:512], in_=pys[0], func=AF.Copy)
                P.op("act", "activation", out=ya[:, 512:1024], in_=pys[1], func=AF.Copy)
                gts = []
                for k in range(8):
                    ot = off_t[oi % 16]; oi += 1
                    P.op("dve", "tensor_copy", out=ot, in_=offf[:, s_, k:k + 1])
                    gt = gt_t[gi % 8]; gi += 1
                    gts.append(gt)
                    P.add("pool", (lambda e, ot=ot, gt=gt: e.indirect_dma_start(
                        out=gt.ap[:, :], out_offset=None, in_=Yg[:, :],
                        in_offset=bass.IndirectOffsetOnAxis(ap=ot.ap[:, 0:1], axis=0), bounds_check=None)),
                        [V(allY, Yg), ot], [gt], dma=True)
                for k in range(8):
                    P.op("dve", "scalar_tensor_tensor", out=ya, in0=gts[k], scalar=gk[:, s_, k:k + 1], in1=ya, op0=ALU.mult, op1=ALU.add)
                residual_ln(s_, [ya[:, 0:512], ya[:, 512:1024]], last)
            ai = expert_block(wg, wu, wd, lambda k, c0, w, ht=ht: ht[:, k, c0:c0 + w], 4, emit_out, ai)
        P.release(base_mark)
        if stop_after == f"p6_{l}":
            return finish(P, nc)
    return finish(P, nc)


def finish(P, nc):
    P.emit()
    return nc


def core_inputs(inp, core, S, need_moe=True):
    m = {}
    m["x"] = np.ascontiguousarray(inp["x"][core])
    m["mem"] = np.ascontiguousarray(inp["mem"][core])
    for k, v in inp.items():
        if k in ("x", "mem") or (not need_moe and k.startswith("e_w_")):
            continue
        if k in ("ln_in_g", "ln_in_b"):
            v = v.reshape(1, -1)
        m[k] = np.ascontiguousarray(v)
    for k, v in make_consts(S).items():
        m["c_" + k] = v
    return m


_CACHE = {}


def kernel(**inputs):
    S = inputs["x"].shape[1]
    nb = inputs["x"].shape[0]
    inp = {k: np.asarray(v) for k, v in inputs.items()}
    if S not in _CACHE:
        _CACHE[S] = build_program(S)
    nc = _CACHE[S]
    in_maps = [core_inputs(inp, c, S) for c in range(nb)]
    res = run_bass_kernel_spmd(nc, in_maps, core_ids=list(range(nb)))
    return np.stack([np.asarray(r["out"]) for r in res.results], axis=0).astype(np.float32)
```
